# Optimizing a Trainium2 kernel written in Bass

```python
import math
import jax
import jax.numpy as jnp
from jax import lax
import numpy as np

D_MODEL = 1024
BATCH = 8
SEQ = 4096
DEPTH = 2

GRID_W = 64
CTX_LEN = 256
N_EVEN = (DEPTH + 1) // 2
N_ODD = DEPTH // 2
NORM_EPS = 1e-6
CONV_W = 3
CHUNK = 64
SC_WIDTH = 1024
SSD_HEADS = 16
SSD_HEAD_DIM = 64
SSD_WIDTH = SSD_HEADS * SSD_HEAD_DIM
SSD_GROUPS = 4
SSD_STATE = 128
SSD_CONV_DIM = SSD_WIDTH + 2 * SSD_GROUPS * SSD_STATE
EVEN_IN = 3 * SC_WIDTH + SSD_WIDTH + SSD_CONV_DIM + 2 * SSD_HEADS
EVEN_MIX = SC_WIDTH + SSD_WIDTH
HG_EXPAND = 128
HG_HEADS = D_MODEL // HG_EXPAND
HG_KDIM = HG_EXPAND
HG_VDIM = D_MODEL // HG_HEADS
HG_FWIDTH = HG_HEADS * HG_KDIM
HG_IWIDTH = HG_HEADS * HG_VDIM
ODD_IN = 3 * HG_FWIDTH + 2 * HG_IWIDTH
PEER_HEADS = 8
PEER_NKEYS = 128
PEER_EXPERTS = PEER_NKEYS * PEER_NKEYS
PEER_QDIM = 256
PEER_HALF = PEER_QDIM // 2
PEER_TOPK = 16
PEER_BLOCK = 128

kernel_name = "hybrid_conv_ssd_hgrn2_peer_dit"


def rmsnorm(x, w):
    xf = x.astype(jnp.float32)
    y = xf * lax.rsqrt(jnp.mean(xf * xf, axis=-1, keepdims=True) + NORM_EPS)
    return (y * w.astype(jnp.float32)).astype(x.dtype)


def group_rmsnorm(y, w, groups):
    shp = y.shape
    yg = y.astype(jnp.float32).reshape(*shp[:-1], groups, shp[-1] // groups)
    yg = yg * lax.rsqrt(jnp.mean(yg * yg, axis=-1, keepdims=True) + NORM_EPS)
    return yg.reshape(shp) * w.astype(jnp.float32)


def modulate(h, shift, scale):
    return h * (1 + scale) + shift


def dwconv(u, w):
    L = u.shape[1]
    pad = CONV_W // 2
    up = jnp.pad(u, ((0, 0), (pad, pad), (0, 0)))
    return sum(up[:, k:k + L] * w[k] for k in range(CONV_W))


def conv_grid(u, w):
    Bsz, S, C = u.shape
    rows = S // GRID_W
    return dwconv(u.reshape(Bsz * rows, GRID_W, C), w).reshape(Bsz, S, C)


def chunk_scan(decay, contrib, s0, keep_starts):
    def step(s, inp):
        d, u = inp
        return d * s + u, (s if keep_starts else None)
    s_fin, starts = lax.scan(step, s0, (jnp.moveaxis(decay, 1, 0), jnp.moveaxis(contrib, 1, 0)))
    return (jnp.moveaxis(starts, 0, 1) if keep_starts else None), s_fin


def ssd_scan(xh, dt, a, bm, cm, s0, with_out):
    Bsz, T, H, P = xh.shape
    G, N = bm.shape[2], bm.shape[3]
    hpg = H // G
    nc = T // CHUNK
    xdt = (xh * dt[..., None]).reshape(Bsz, nc, CHUNK, G, hpg, P)
    bm = bm.reshape(Bsz, nc, CHUNK, G, N)
    la = jnp.cumsum((dt * a).reshape(Bsz, nc, CHUNK, G, hpg), axis=2)
    to_end = jnp.exp(la[:, :, -1:] - la)
    contrib = jnp.einsum('bcsgn,bcsgh,bcsghp->bcghnp', bm, to_end, xdt).reshape(Bsz, nc, H, N, P)
    chunk_decay = jnp.exp(la[:, :, -1]).reshape(Bsz, nc, H, 1, 1)
    starts, s_fin = chunk_scan(chunk_decay, contrib, s0, with_out)
    if not with_out:
        return None, s_fin
    cm = cm.reshape(Bsz, nc, CHUNK, G, N)
    mask = jnp.tril(jnp.ones((CHUNK, CHUNK), dtype=bool))[:, :, None, None]
    seg = la[:, :, :, None] - la[:, :, None]
    lmat = jnp.exp(jnp.where(mask, seg, -jnp.inf))
    cb = jnp.einsum('bctgn,bcsgn->bctsg', cm, bm)
    y = jnp.einsum('bctsg,bctsgh,bcsghp->bctghp', cb, lmat, xdt)
    y = y + jnp.einsum('bctgn,bcghnp,bctgh->bctghp', cm, starts.reshape(Bsz, nc, G, hpg, N, P), jnp.exp(la))
    return y.reshape(Bsz, T, H, P), s_fin


def gla_scan(q, k, v, g, s0, with_out):
    Bsz, T, H, K = q.shape
    V = v.shape[-1]
    nc = T // CHUNK
    q = q.reshape(Bsz, nc, CHUNK, H, K)
    k = k.reshape(Bsz, nc, CHUNK, H, K)
    v = v.reshape(Bsz, nc, CHUNK, H, V)
    gc = jnp.cumsum(g.reshape(Bsz, nc, CHUNK, H, K), axis=2)
    g_end = gc[:, :, -1]
    contrib = jnp.einsum('bcshk,bcshv->bchkv', k * jnp.exp(g_end[:, :, None] - gc), v)
    starts, s_fin = chunk_scan(jnp.exp(g_end)[..., None], contrib, s0, with_out)
    if not with_out:
        return None, s_fin
    qg = q * jnp.exp(gc)
    kg = k * jnp.exp(-gc)
    mask = jnp.tril(jnp.ones((CHUNK, CHUNK), dtype=bool))
    att = jnp.where(mask, jnp.einsum('bcthk,bcshk->bchts', qg, kg), 0.0)
    o = jnp.einsum('bchts,bcshv->bcthv', att, v) + jnp.einsum('bcthk,bchkv->bcthv', qg, starts)
    return o.reshape(Bsz, T, H, V), s_fin


def _flipper(d):
    return (lambda t: jnp.flip(t, axis=1)) if d == 1 else (lambda t: t)


def even_mixer(hx, hc, w_in, w_out, sc_conv_w, ssd_conv_w, ssd_conv_b, dt_bias, a_log, d_skip,
               ssd_norm_w, need_ctx):
    a = -jnp.exp(a_log.astype(jnp.float32))
    dt_bias = dt_bias.astype(jnp.float32)

    def project(h, conv):
        Bsz, T, _ = h.shape
        sc_x, sc_b, sc_c, z, xbc, dt_raw = jnp.split(
            h @ w_in,
            [SC_WIDTH, 2 * SC_WIDTH, 3 * SC_WIDTH, 3 * SC_WIDTH + SSD_WIDTH,
             3 * SC_WIDTH + SSD_WIDTH + SSD_CONV_DIM], axis=-1)
        xbc = jax.nn.silu(conv(xbc, ssd_conv_w) + ssd_conv_b).astype(jnp.float32)
        xs, bm, cm = jnp.split(xbc, [SSD_WIDTH, SSD_WIDTH + SSD_GROUPS * SSD_STATE], axis=-1)
        ssd_in = (xs.reshape(Bsz, T, SSD_HEADS, SSD_HEAD_DIM),
                  bm.reshape(Bsz, T, SSD_GROUPS, SSD_STATE),
                  cm.reshape(Bsz, T, SSD_GROUPS, SSD_STATE),
                  dt_raw.astype(jnp.float32).reshape(Bsz, T, 2, SSD_HEADS))
        return (sc_x, sc_b, sc_c, z), ssd_in

    def run(ssd_in, d, s0, with_out):
        xs, bm, cm, dt_raw = ssd_in
        dt = jax.nn.softplus(dt_raw[:, :, d] + dt_bias[d])
        fl = _flipper(d)
        y, s = ssd_scan(fl(xs), fl(dt), a[d], fl(bm), fl(cm), s0, with_out)
        return (fl(y) if with_out else None), s

    def finish(gates, ssd_in, y_ssd, conv):
        sc_x, sc_b, sc_c, z = gates
        Bsz, T = z.shape[:2]
        y_a = sc_b * conv(sc_c * sc_x, sc_conv_w)
        y_b = (y_ssd + d_skip.astype(jnp.float32)[:, None] * ssd_in[0]).reshape(Bsz, T, SSD_WIDTH)
        y_b = group_rmsnorm(y_b * jax.nn.silu(z.astype(jnp.float32)), ssd_norm_w, SSD_GROUPS)
        return jnp.concatenate([y_a, y_b.astype(y_a.dtype)], axis=-1) @ w_out

    gates_x, ssd_x = project(hx, conv_grid)
    gates_c, ssd_c = project(hc, dwconv)
    Bsz = hx.shape[0]
    ys_x, ys_c = [], []
    for d in range(2):
        s0 = jnp.zeros((Bsz, SSD_HEADS, SSD_STATE, SSD_HEAD_DIM), jnp.float32)
        yc_d, s_ctx = run(ssd_c, d, s0, need_ctx)
        yx_d, _ = run(ssd_x, d, s_ctx, True)
        ys_x.append(yx_d)
        ys_c.append(yc_d)
    out_x = finish(gates_x, ssd_x, ys_x[0] + ys_x[1], conv_grid)
    out_c = finish(gates_c, ssd_c, ys_c[0] + ys_c[1], dwconv) if need_ctx else None
    return out_x, out_c


def odd_mixer(hx, hc, w_in, w_out, lower_bound, norm_w, need_ctx):
    lb = lower_bound.reshape(2, 1, 1, HG_HEADS, HG_KDIM)

    def project(h):
        Bsz, T, _ = h.shape
        q, f_fwd, f_bwd, i, g = jnp.split(
            h @ w_in, [HG_FWIDTH, 2 * HG_FWIDTH, 3 * HG_FWIDTH, 3 * HG_FWIDTH + HG_IWIDTH], axis=-1)
        heads = lambda t, dim: t.astype(jnp.float32).reshape(Bsz, T, HG_HEADS, dim)
        q = heads(jax.nn.silu(q), HG_KDIM) * (HG_KDIM ** -0.5)
        return q, (heads(f_fwd, HG_KDIM), heads(f_bwd, HG_KDIM)), heads(i, HG_VDIM), g

    def run(p, d, s0, with_out):
        q, fs, i, _ = p
        f = lb[d] + (1.0 - lb[d]) * jax.nn.sigmoid(fs[d])
        fl = _flipper(d)
        o, s = gla_scan(fl(q), fl(1.0 - f), fl(i), fl(jnp.log(f)), s0, with_out)
        return (fl(o) if with_out else None), s

    def finish(o, g):
        Bsz, T = g.shape[:2]
        o = group_rmsnorm(o.reshape(Bsz, T, HG_IWIDTH), norm_w, HG_HEADS) * jax.nn.silu(g.astype(jnp.float32))
        return o.astype(g.dtype) @ w_out

    lat, cp = project(hx), project(hc)
    Bsz = hx.shape[0]
    os_x, os_c = [], []
    for d in range(2):
        s0 = jnp.zeros((Bsz, HG_HEADS, HG_KDIM, HG_VDIM), jnp.float32)
        oc_d, s_ctx = run(cp, d, s0, need_ctx)
        ox_d, _ = run(lat, d, s_ctx, True)
        os_x.append(ox_d)
        os_c.append(oc_d)
    out_x = finish(os_x[0] + os_x[1], lat[3])
    out_c = finish(os_c[0] + os_c[1], cp[3]) if need_ctx else None
    return out_x, out_c


def peer_ffn(h, wq, keys, u_tab, v_tab):
    Bsz, T, D = h.shape
    n = Bsz * T
    hf = h.reshape(n, D)
    q = (hf @ wq).reshape(n, PEER_HEADS, 2, PEER_HALF)
    sub = jnp.einsum('nhad,hakd->nhak', q, keys)
    s_top, i_top = lax.top_k(sub, PEER_TOPK)
    cand_s = (s_top[:, :, 0, :, None] + s_top[:, :, 1, None, :]).reshape(n, PEER_HEADS, PEER_TOPK * PEER_TOPK)
    cand_i = (i_top[:, :, 0, :, None] * PEER_NKEYS + i_top[:, :, 1, None, :]).reshape(n, PEER_HEADS, PEER_TOPK * PEER_TOPK)
    best_s, pos = lax.top_k(cand_s, PEER_TOPK)
    idx = jnp.take_along_axis(cand_i, pos, axis=-1)
    gate = jax.nn.softmax(best_s.astype(jnp.float32), axis=-1).astype(h.dtype)
    nb = n // PEER_BLOCK
    idx = idx.reshape(nb, PEER_BLOCK, PEER_HEADS * PEER_TOPK)
    gate = gate.reshape(nb, PEER_BLOCK, PEER_HEADS * PEER_TOPK)

    def block(args):
        hb, ib, gb = args
        act = jax.nn.gelu(jnp.einsum('td,tkd->tk', hb, u_tab[ib]), approximate=False) * gb
        return jnp.einsum('tk,tkd->td', act, v_tab[ib])

    out = lax.map(block, (hf.reshape(nb, PEER_BLOCK, D), idx, gate))
    return out.reshape(Bsz, T, D)


def setup_inputs(seed: int = 0) -> dict:
    key = jax.random.key(seed)
    ks = iter(jax.random.split(key, 32))
    nrm = lambda shape, scale: jax.random.normal(next(ks), shape, jnp.float32) * scale
    gain = lambda shape: 1.0 + 0.02 * jax.random.normal(next(ks), shape, jnp.float32)
    D = D_MODEL
    x = nrm((BATCH, SEQ, D), 1.0)
    c = nrm((BATCH, D), 1.0)
    ctx = nrm((BATCH, CTX_LEN, D), 1.0)
    c_ctx = nrm((D,), 1.0)
    ada_w = nrm((DEPTH, D, 6 * D), 0.5 * D ** -0.5)
    ada_b = nrm((DEPTH, 6 * D), 0.02)
    norm_mix_w = gain((DEPTH, D))
    norm_ffn_w = gain((DEPTH, D))
    norm_f_w = gain((D,))
    ev_w_in = nrm((N_EVEN, D, EVEN_IN), D ** -0.5)
    ev_w_out = nrm((N_EVEN, EVEN_MIX, D), EVEN_MIX ** -0.5)
    sc_conv_w = nrm((N_EVEN, CONV_W, SC_WIDTH), CONV_W ** -0.5)
    ssd_conv_w = nrm((N_EVEN, CONV_W, SSD_CONV_DIM), CONV_W ** -0.5)
    ssd_conv_b = nrm((N_EVEN, SSD_CONV_DIM), 0.02)
    log_dt = jax.random.uniform(next(ks), (N_EVEN, 2, SSD_HEADS), jnp.float32, math.log(1e-3), math.log(1e-1))
    dt0 = jnp.exp(log_dt)
    ssd_dt_bias = dt0 + jnp.log(-jnp.expm1(-dt0))
    ssd_a_log = jnp.log(jax.random.uniform(next(ks), (N_EVEN, 2, SSD_HEADS), jnp.float32, 1.0, 16.0))
    ssd_d = gain((N_EVEN, SSD_HEADS))
    ssd_norm_w = gain((N_EVEN, SSD_WIDTH))
    od_w_in = nrm((N_ODD, D, ODD_IN), D ** -0.5)
    od_w_out = nrm((N_ODD, HG_IWIDTH, D), HG_IWIDTH ** -0.5)
    hg_lb_logits = 1.0 + nrm((DEPTH, 2, HG_FWIDTH), 0.1)
    hg_norm_w = gain((N_ODD, HG_IWIDTH))
    peer_wq = nrm((DEPTH, D, PEER_HEADS * PEER_QDIM), D ** -0.5)
    peer_keys = nrm((DEPTH, PEER_HEADS, 2, PEER_NKEYS, PEER_HALF), PEER_HALF ** -0.5)
    peer_u = nrm((DEPTH, PEER_EXPERTS, D), D ** -0.5)
    peer_v = nrm((DEPTH, PEER_EXPERTS, D), 0.5)
    return {"x": x, "c": c, "ctx": ctx, "c_ctx": c_ctx, "ada_w": ada_w, "ada_b": ada_b,
            "norm_mix_w": norm_mix_w, "norm_ffn_w": norm_ffn_w, "norm_f_w": norm_f_w,
            "ev_w_in": ev_w_in, "ev_w_out": ev_w_out, "sc_conv_w": sc_conv_w,
            "ssd_conv_w": ssd_conv_w, "ssd_conv_b": ssd_conv_b, "ssd_dt_bias": ssd_dt_bias,
            "ssd_a_log": ssd_a_log, "ssd_d": ssd_d, "ssd_norm_w": ssd_norm_w,
            "od_w_in": od_w_in, "od_w_out": od_w_out, "hg_lb_logits": hg_lb_logits,
            "hg_norm_w": hg_norm_w, "peer_wq": peer_wq, "peer_keys": peer_keys,
            "peer_u": peer_u, "peer_v": peer_v}


def reference(x, c, ctx, c_ctx, ada_w, ada_b, norm_mix_w, norm_ffn_w, norm_f_w, ev_w_in, ev_w_out,
              sc_conv_w, ssd_conv_w, ssd_conv_b, ssd_dt_bias, ssd_a_log, ssd_d, ssd_norm_w,
              od_w_in, od_w_out, hg_lb_logits, hg_norm_w, peer_wq, peer_keys, peer_u, peer_v):
    lb_sm = jax.nn.softmax(hg_lb_logits.astype(jnp.float32), axis=0)
    lower_bounds = jnp.cumsum(lb_sm, axis=0) - lb_sm[0]
    cond_x = jax.nn.silu(c)[:, None, :]
    cond_c = jax.nn.silu(c_ctx)
    for layer in range(DEPTH):
        last = layer == DEPTH - 1
        j = layer // 2
        mx = jnp.split(cond_x @ ada_w[layer] + ada_b[layer], 6, axis=-1)
        mc = jnp.split(cond_c @ ada_w[layer] + ada_b[layer], 6, axis=-1)
        hx = modulate(rmsnorm(x, norm_mix_w[layer]), mx[0], mx[1])
        hc = modulate(rmsnorm(ctx, norm_mix_w[layer]), mc[0], mc[1])
        if layer % 2 == 0:
            ox, oc = even_mixer(hx, hc, ev_w_in[j], ev_w_out[j], sc_conv_w[j], ssd_conv_w[j],
                                ssd_conv_b[j], ssd_dt_bias[j], ssd_a_log[j], ssd_d[j], ssd_norm_w[j],
                                not last)
        else:
            ox, oc = odd_mixer(hx, hc, od_w_in[j], od_w_out[j], lower_bounds[layer], hg_norm_w[j],
                               not last)
        x = x + mx[2] * ox
        x = x + mx[5] * peer_ffn(modulate(rmsnorm(x, norm_ffn_w[layer]), mx[3], mx[4]),
                                 peer_wq[layer], peer_keys[layer], peer_u[layer], peer_v[layer])
        if not last:
            ctx = ctx + mc[2] * oc
            ctx = ctx + mc[5] * peer_ffn(modulate(rmsnorm(ctx, norm_ffn_w[layer]), mc[3], mc[4]),
                                         peer_wq[layer], peer_keys[layer], peer_u[layer], peer_v[layer])
    return rmsnorm(x, norm_f_w)
```

```python
import contextlib
import numpy as np
import concourse.bass as bass
import concourse.mybir as mybir
from concourse.alu_op_type import AluOpType as ALU
from concourse.bass_utils import run_bass_kernel_spmd

AF = mybir.ActivationFunctionType
AX = mybir.AxisListType
F32 = mybir.dt.float32
BF16 = mybir.dt.bfloat16
U32 = mybir.dt.uint32
I32 = mybir.dt.int32
U16 = mybir.dt.uint16

ENGS = ("sync", "scalar", "vector", "gpsimd", "tensor")
SAME_ENGINE_WAITS = True


class Prog:
    def __init__(self, nc):
        self.nc = nc
        self.stack = contextlib.ExitStack()
        self.q = {e: [] for e in ENGS}
        self.cnt = {}
        self.waited = {e: {} for e in ENGS}
        self.last_w = {}
        self.reads = {}
        self.semkeys = []
        self.sems = {}
        self.nops = 0
        self.shared = set()

    def sb(self, name, shape, dt):
        return self.stack.enter_context(self.nc.sbuf_tensor(name, list(shape), dt))

    def ps(self, name, shape, dt):
        return self.stack.enter_context(self.nc.psum_tensor(name, list(shape), dt))

    def dram(self, name, shape, dt, kind="Internal"):
        return self.nc.dram_tensor(name, list(shape), dt, kind=kind)

    def _semkey(self, k):
        if k not in self.cnt:
            self.cnt[k] = 0
            self.semkeys.append(k)
        return k

    def _deps(self, eng, reads, writes):
        deps = {}
        def add(ev, war=False):
            if ev is None:
                return
            k, v = ev
            if k in self.shared:
                v = self.cnt[k]
            if k == ("E", eng):
                if eng == "tensor" or war or not SAME_ENGINE_WAITS:
                    return
            if deps.get(k, 0) < v:
                deps[k] = v
        for r in reads:
            add(self.last_w.get(r))
        for w in writes:
            add(self.last_w.get(w))
            for ev in self.reads.get(w, ()):
                add(ev, war=True)
        out = []
        wd = self.waited[eng]
        for k, v in deps.items():
            if wd.get(k, 0) >= v:
                continue
            wd[k] = v
            out.append((k, v))
        return out

    def _record(self, ev, reads, writes):
        for r in reads:
            self.reads.setdefault(r, []).append(ev)
        for w in writes:
            self.last_w[w] = ev
            self.reads[w] = []

    def op(self, eng, fn, reads=(), writes=()):
        reads = tuple(reads); writes = tuple(writes)
        waits = self._deps(eng, reads, writes)
        k = self._semkey(("E", eng))
        self.cnt[k] += 1
        v = self.cnt[k]
        self.q[eng].append((waits, fn, k, 1))
        self._record((k, v), reads, writes)
        self.nops += 1

    def dma(self, eng, fn, reads=(), writes=(), sem=None):
        reads = tuple(reads); writes = tuple(writes)
        waits = self._deps(eng, reads, writes)
        k = self._semkey(("D", sem if sem is not None else writes[0]))
        if sem == "misc":
            self.shared.add(k)
        self.cnt[k] += 16
        v = self.cnt[k]
        self.q[eng].append((waits, fn, k, 16))
        self._record((k, v), reads, writes)
        self.nops += 1

    def wait_all(self, eng, keys):
        waits = self._deps(eng, tuple(keys), ())
        self.q[eng].append((waits, None, None, 0))

    def build(self):
        nc = self.nc
        for i, k in enumerate(self.semkeys):
            self.sems[k] = self.stack.enter_context(nc.semaphore("s%d" % i))
        block = self.stack.enter_context(nc.Block())
        sems = self.sems

        def run(e, items):
            for waits, fn, k, inc in items:
                for wk, wv in waits:
                    e.wait_ge(sems[wk], wv)
                if fn is not None:
                    fn(e).then_inc(sems[k], inc)

        @block.sync
        def _(e):
            run(e, self.q["sync"])

        @block.scalar
        def _(e):
            run(e, self.q["scalar"])

        @block.vector
        def _(e):
            run(e, self.q["vector"])

        @block.gpsimd
        def _(e):
            run(e, self.q["gpsimd"])

        @block.tensor
        def _(e):
            run(e, self.q["tensor"])

    def close(self):
        self.stack.close()


def _barrier(P):
    allk = [(k, P.cnt[k]) for k in P.semkeys if P.cnt[k] > 0]
    for e in ENGS:
        waits = []
        for k, v in allk:
            if P.waited[e].get(k, 0) < v:
                P.waited[e][k] = v
                waits.append((k, v))
        if waits:
            P.q[e].append((waits, None, None, 0))


Prog.barrier = _barrier


class Arena:
    def __init__(self, P, nbytes):
        self.words = nbytes // 4
        self.t = P.sb("arena", [128, self.words], F32)
        self.off = 0
        self.n = 0

    def alloc(self, shape, dt=F32):
        n = 1
        for s in shape:
            n *= s
        esz = 2 if dt in (BF16, U16) else 4
        words = (n * esz + 3) // 4
        words = (words + 7) // 8 * 8
        assert self.off + words <= self.words, ("arena overflow", self.off, words)
        ap = self.t[:, self.off:self.off + words]
        self.off += words
        if dt != F32:
            ap = ap.bitcast(dt)
        ap = ap[:, 0:n]
        if len(shape) > 1:
            names = "abcd"[:len(shape)]
            pat = "p (%s) -> p %s" % (" ".join(names), " ".join(names))
            ap = ap.rearrange(pat, **{names[i]: shape[i] for i in range(len(shape))})
        return ap


D = 1024
NT = 34
EPS = 1e-6
NEG = -1.0e30


class KB:
    def __init__(self, dbg=None):
        self.dbg = dbg or {}
        nc = self.nc = bass.Bass("TRN2", target_bir_lowering=False)
        P = self.P = Prog(nc)
        self.A = Arena(P, 172000)
        self.psum = P.ps("psum", [128, 8, 512], F32)
        di = lambda n, s, dt=F32: nc.dram_tensor(n, list(s), dt, kind="ExternalInput")
        self.x = di("x", [4096, D]); self.ctx = di("ctx", [256, D])
        self.condT = di("condT", [128, 16])
        self.consts = di("consts", [128, 1024])
        self.ada_w = di("ada_w", [2, D, 6 * D]); self.ada_bT = di("ada_bT", [2, 128, 48])
        self.nmixT = di("nmixT", [2, 128, 8]); self.nffnT = di("nffnT", [2, 128, 8])
        self.nf_row = di("nf_row", [128, D])
        self.peer_wq = di("peer_wq", [2, D, 2048]); self.keysT = di("keysT", [2, 128, 2048])
        self.peer_u = [di("peer_u%d" % l, [16384, D]) for l in range(2)]; self.peer_v = [di("peer_v%d" % l, [16384, D]) for l in range(2)]
        self.out = nc.dram_tensor("out", [4096, D], F32, kind="ExternalOutput")
        self.x1 = nc.dram_tensor("x1", [NT * 128, D], F32, kind=self.dbg.get("x1", "Internal"))
        self.x2 = nc.dram_tensor("x2", [NT * 128, D], F32, kind=self.dbg.get("x2", "Internal"))
        self.x3 = nc.dram_tensor("x3", [NT * 128, D], F32, kind=self.dbg.get("x3", "Internal"))
        self.uid = 0

    def key(self, s):
        self.uid += 1
        return "%s#%d" % (s, self.uid)

    def bank(self, b, n=512, off=0):
        return self.psum[:, b, off:off + n]

    def src_tile(self, i):
        if i < 2:
            return self.ctx[i * 128:(i + 1) * 128, :]
        return self.x[(i - 2) * 128:(i - 1) * 128, :]

    def setup_consts(self):
        P, A = self.P, self.A
        self.cst = A.alloc([1024]);
        P.dma("sync", lambda e: e.dma_start(out=self.cst, in_=self.consts.ap()), writes=["cst"], sem="misc")
        self.ident = self.cst[:, 0:128]; self.ones = self.cst[:, 128:256]
        self.cond = A.alloc([16])
        P.dma("sync", lambda e: e.dma_start(out=self.cond, in_=self.condT.ap()), writes=["cond"], sem="misc")
        self.condS = A.alloc([8, 2])
        P.op("scalar", lambda e: e.activation(out=self.condS, in_=self.cond.rearrange("p (a b) -> p a b", b=2), func=AF.Silu),
             reads=["cond"], writes=["condS"])
        self.modsFM = [A.alloc([48, 2]) for _ in range(2)]
        self.identb = A.alloc([128], BF16)
        P.op("vector", lambda e: e.tensor_copy(out=self.identb, in_=self.ident), reads=["cst"], writes=["identb"])
        P.barrier()

    def mods_phase(self, l):
        P, A = self.P, self.A
        mark = A.off
        ws = [A.alloc([8, 768]) for _ in range(2)]
        abT = A.alloc([48])
        P.dma("sync", lambda e: e.dma_start(out=abT, in_=self.ada_bT[l]), writes=["abT"], sem="misc")
        pm = self.psum[:, 0, 0:96]
        awv = self.ada_w[l].rearrange("(k p) n -> p k n", p=128)
        for g in range(8):
            s = g % 2
            P.dma("sync" if s == 0 else "scalar", lambda e, g=g, s=s: e.dma_start(out=ws[s], in_=awv[:, :, g * 768:(g + 1) * 768]),
                  writes=[("ws", s)])
            for jj in range(6):
                j = g * 6 + jj
                for k in range(8):
                    P.op("tensor", lambda e, k=k, s=s, j=j, jj=jj: e.matmul(pm[:, 2 * j:2 * j + 2], lhsT=ws[s][:, k, jj * 128:(jj + 1) * 128],
                                                                           rhs=self.condS[:, k, :], start=(k == 0), stop=(k == 7)),
                         reads=[("ws", s), "condS"], writes=[("ps", 0)])
        m = self.modsFM[l]
        P.op("vector", lambda e: e.tensor_tensor(out=m, in0=pm.rearrange("p (a b) -> p a b", b=2),
                                                 in1=abT.unsqueeze(2).to_broadcast([128, 48, 2]), op=ALU.add),
             reads=[("ps", 0), "abT"], writes=[("mods", l)])
        P.barrier()
        A.off = mark

    def make_row(self, dst, col, dstkey, colkeys, banks=(6, 7)):
        P = self.P
        dg = self.diag
        for jj in range(8):
            P.op("vector", lambda e, jj=jj: e.tensor_scalar(out=dg, in0=self.ident, scalar1=col[:, jj:jj + 1], scalar2=None, op0=ALU.mult),
                 reads=["cst"] + list(colkeys), writes=["diag"])
            b = banks[jj // 4]
            P.op("tensor", lambda e, jj=jj, b=b: e.matmul(self.bank(b, 128, (jj % 4) * 128), lhsT=self.ones, rhs=dg, start=True, stop=True),
                 reads=["diag", "cst"], writes=[("ps", b)])
        for h in range(2):
            P.op("scalar", lambda e, h=h: e.copy(out=dst[:, h * 512:(h + 1) * 512], in_=self.bank(banks[h])),
                 reads=[("ps", banks[h])], writes=[dstkey])

    def peer_phase(self, l, src_fn, dst, tiles, final_norm=False):
        P, A = self.P, self.A
        mark = A.off
        V = lambda fn, r, w: P.op("vector", fn, r, w)
        S = lambda fn, r, w: P.op("scalar", fn, r, w)
        T = lambda fn, r, w: P.op("tensor", fn, r, w)
        m = self.modsFM[l]
        mk = [("mods", l)]
        wq = A.alloc([8, 2048])
        for k in range(8):
            P.dma("sync" if k % 2 == 0 else "scalar",
                  lambda e, k=k: e.dma_start(out=wq[:, k, :], in_=self.peer_wq[l, k * 128:(k + 1) * 128, :]), writes=["wq"], sem="misc")
        kT = A.alloc([16, 128])
        P.dma("sync", lambda e: e.dma_start(out=kT, in_=self.keysT[l].rearrange("p (a b) -> p a b", b=128)), writes=["kT"], sem="misc")
        nf = A.alloc([8])
        P.dma("sync", lambda e: e.dma_start(out=nf, in_=self.nffnT[l]), writes=["nf"], sem="misc")
        Arow = A.alloc([D]); Brow = A.alloc([D]); Grow = A.alloc([D])
        acol = A.alloc([8])
        xt = A.alloc([D]); h2 = A.alloc([D]); hT = A.alloc([8, 128]); xn = h2
        qT = A.alloc([16, 128]); sub = A.alloc([16, 128]); sub2 = A.alloc([16, 128])
        cand = sub.rearrange("p a b -> p (a b)").rearrange("p (a b) -> p a b", b=256)
        candi = qT.rearrange("p a b -> p (a b)").rearrange("p (a b) -> p a b", b=256)
        cand2 = sub2.rearrange("p a b -> p (a b)").rearrange("p (a b) -> p a b", b=256)
        st = A.alloc([16, 16]); it = A.alloc([16, 16], U32); itf = A.alloc([16, 16])
        bs = A.alloc([8, 16]); idxf = A.alloc([128]); idx = A.alloc([128], U32)
        gate = A.alloc([8, 16]); sm = A.alloc([16]); act = A.alloc([128]); araw = A.alloc([128])
        junk = A.alloc([D]); acc = junk
        NS = 4
        gs = [A.alloc([D]) for _ in range(NS)]
        gcount = [0]
        if final_norm:
            nfr = A.alloc([D])
            P.dma("sync", lambda e: e.dma_start(out=nfr, in_=self.nf_row.ap()), writes=["nfr"], sem="misc")
        utab = self.peer_u[l].ap(); vtab = self.peer_v[l].ap()
        cur_c = [None]

        def set_cond(c):
            if cur_c[0] == c:
                return
            cur_c[0] = c
            V(lambda e: e.scalar_tensor_tensor(out=acol, in0=m[:, 32:40, c], scalar=1.0, in1=nf, op0=ALU.add, op1=ALU.mult),
              mk + ["nf"], ["acol"])
            self.make_row(Arow, acol, "Arow", ["acol"])
            self.make_row(Brow, m[:, 24:32, c], "Brow", mk)
            self.make_row(Grow, m[:, 40:48, c], "Grow", mk)

        for i in tiles:
            set_cond(0 if i >= 2 else 1)
            P.dma("sync", lambda e, i=i: e.dma_start(out=xt, in_=src_fn(i)), writes=["xt"])
            S(lambda e: e.activation(out=junk, in_=xt, func=AF.Square, accum_out=sm[:, 0:1]), ["xt"], ["junk", "sm0"])
            S(lambda e: e.activation(out=sm[:, 1:2], in_=sm[:, 0:1], func=AF.Sqrt, scale=1.0 / D, bias=EPS), ["sm0"], ["sm1"])
            V(lambda e: e.reciprocal(out=sm[:, 2:3], in_=sm[:, 1:2]), ["sm1"], ["sm2"])
            V(lambda e: e.tensor_scalar(out=xn, in0=xt, scalar1=sm[:, 2:3], scalar2=None, op0=ALU.mult), ["xt", "sm2"], ["h2"])
            V(lambda e: e.tensor_tensor(out=xn, in0=xn, in1=Arow, op=ALU.mult), ["h2", "Arow"], ["h2"])
            V(lambda e: e.tensor_tensor(out=h2, in0=xn, in1=Brow, op=ALU.add), ["h2", "Brow"], ["h2"])
            for k in range(8):
                T(lambda e, k=k: e.transpose(self.bank(k // 4, 128, (k % 4) * 128), h2[:, k * 128:(k + 1) * 128], self.ident),
                  ["h2", "cst"], [("ps", k // 4)])
            S(lambda e: e.copy(out=hT.rearrange("p a b -> p (a b)").rearrange("p (a b) -> p a b", b=512), in_=self.psum[:, 0:2, :]),
              [("ps", 0), ("ps", 1)], ["hT"])
            for j in range(16):
                for k in range(8):
                    T(lambda e, j=j, k=k: e.matmul(self.bank(2 + j // 4, 128, (j % 4) * 128), lhsT=wq[:, k, j * 128:(j + 1) * 128],
                                                  rhs=hT[:, k, :], start=(k == 0), stop=(k == 7)),
                      ["wq", "hT"], [("ps", 2 + j // 4)])
            S(lambda e: e.copy(out=qT.rearrange("p a b -> p (a b)").rearrange("p (a b) -> p a b", b=512), in_=self.psum[:, 2:6, :]),
              [("ps", b) for b in (2, 3, 4, 5)], ["qT"])
            sb_banks = (0, 1, 6, 7)
            for j in range(16):
                b = sb_banks[j // 4]
                T(lambda e, j=j, b=b: e.matmul(self.bank(b, 128, (j % 4) * 128), lhsT=qT[:, j, :], rhs=kT[:, j, :], start=True, stop=True),
                  ["qT", "kT"], [("ps", b)])
            for q4 in range(4):
                b = sb_banks[q4]
                S(lambda e, q4=q4, b=b: e.copy(out=sub[:, q4 * 4:(q4 + 1) * 4, :].rearrange("p a b -> p (a b)"), in_=self.bank(b)),
                  [("ps", b)], ["sub"])
            for j in range(16):
                V(lambda e, j=j: e.max(out=st[:, j, 0:8], in_=sub[:, j, :]), ["sub"], ["st"])
                V(lambda e, j=j: e.max_index(out=it[:, j, 0:8], in_max=st[:, j, 0:8], in_values=sub[:, j, :]), ["sub", "st"], ["it"])
                V(lambda e, j=j: e.match_replace(out=sub2[:, j, :], in_to_replace=st[:, j, 0:8], in_values=sub[:, j, :], imm_value=NEG),
                  ["sub", "st"], ["sub2"])
                V(lambda e, j=j: e.max(out=st[:, j, 8:16], in_=sub2[:, j, :]), ["sub2"], ["st"])
                V(lambda e, j=j: e.max_index(out=it[:, j, 8:16], in_max=st[:, j, 8:16], in_values=sub2[:, j, :]), ["sub2", "st"], ["it"])
            V(lambda e: e.tensor_copy(out=itf, in_=it), ["it"], ["itf"])
            st4 = st.rearrange("p (h a) k -> p h a k", a=2)
            if4 = itf.rearrange("p (h a) k -> p h a k", a=2)
            V(lambda e: e.tensor_scalar(out=if4[:, :, 0, :], in0=if4[:, :, 0, :], scalar1=128.0, scalar2=None, op0=ALU.mult), ["itf"], ["itf"])
            c4 = cand.rearrange("p h (a b) -> p h a b", b=16)
            ci4 = candi.rearrange("p h (a b) -> p h a b", b=16)
            V(lambda e: e.tensor_tensor(out=c4, in0=st4[:, :, 0, :].unsqueeze(3).to_broadcast([128, 8, 16, 16]),
                                        in1=st4[:, :, 1, :].unsqueeze(2).to_broadcast([128, 8, 16, 16]), op=ALU.add), ["st"], ["sub"])
            V(lambda e: e.tensor_tensor(out=ci4, in0=if4[:, :, 0, :].unsqueeze(3).to_broadcast([128, 8, 16, 16]),
                                        in1=if4[:, :, 1, :].unsqueeze(2).to_broadcast([128, 8, 16, 16]), op=ALU.add), ["itf"], ["qT"])
            for h in range(8):
                V(lambda e, h=h: e.max(out=bs[:, h, 0:8], in_=cand[:, h, :]), ["sub"], ["bs"])
                V(lambda e, h=h: e.match_replace(out=cand2[:, h, :], in_to_replace=bs[:, h, 0:8], in_values=cand[:, h, :], imm_value=NEG),
                  ["sub", "bs"], ["sub2"])
                V(lambda e, h=h: e.max(out=bs[:, h, 8:16], in_=cand2[:, h, :]), ["sub2"], ["bs"])
            for h in range(8):
                for k in range(16):
                    V(lambda e, h=h, k=k: e.scalar_tensor_tensor(out=junk[:, 0:256], in0=cand[:, h, :], scalar=bs[:, h, k:k + 1],
                                                               in1=candi[:, h, :], op0=ALU.is_equal, op1=ALU.mult,
                                                               accum_out=idxf[:, h * 16 + k:h * 16 + k + 1]),
                      ["sub", "bs", "qT"], ["junk", "idxf"])
            V(lambda e: e.tensor_scalar(out=idxf, in0=idxf, scalar1=16383.0, scalar2=0.0, op0=ALU.min, op1=ALU.max), ["idxf"], ["idxf"])
            V(lambda e: e.tensor_copy(out=idx, in_=idxf), ["idxf"], ["idx"])
            V(lambda e: e.tensor_tensor(out=gate, in0=bs, in1=bs[:, :, 0:1].to_broadcast([128, 8, 16]), op=ALU.subtract), ["bs"], ["gate"])
            S(lambda e: e.activation(out=gate, in_=gate, func=AF.Exp), ["gate"], ["gate"])
            V(lambda e: e.tensor_reduce(out=sm[:, 4:12], in_=gate, axis=AX.X, op=ALU.add), ["gate"], ["sm4"])
            V(lambda e: e.reciprocal(out=sm[:, 4:12], in_=sm[:, 4:12]), ["sm4"], ["sm4"])
            V(lambda e: e.tensor_tensor(out=gate, in0=gate, in1=sm[:, 4:12].unsqueeze(2).to_broadcast([128, 8, 16]), op=ALU.mult),
              ["gate", "sm4"], ["gate"])
            for k in range(128):
                s = gcount[0] % NS; gcount[0] += 1
                P.dma("gpsimd", lambda e, k=k, s=s: e.indirect_dma_start(
                    out=gs[s], out_offset=None, in_=utab,
                    in_offset=bass.IndirectOffsetOnAxis(ap=idx[:, k:k + 1], axis=0)), reads=["idx"], writes=[("gs", s)],
                    sem=("gs", l, s, gcount[0] // (NS * 250)))
                V(lambda e, k=k, s=s: e.scalar_tensor_tensor(out=junk, in0=gs[s], scalar=1.0, in1=h2, op0=ALU.mult, op1=ALU.mult,
                                                           accum_out=araw[:, k:k + 1]), [("gs", s), "h2"], ["junk", "araw"])
            S(lambda e: e.activation(out=act, in_=araw, func=AF.Gelu), ["araw"], ["act"])
            V(lambda e: e.tensor_tensor(out=act, in0=act, in1=gate.rearrange("p a b -> p (a b)"), op=ALU.mult), ["act", "gate"], ["act"])
            for k in range(128):
                s = gcount[0] % NS; gcount[0] += 1
                P.dma("gpsimd", lambda e, k=k, s=s: e.indirect_dma_start(
                    out=gs[s], out_offset=None, in_=vtab,
                    in_offset=bass.IndirectOffsetOnAxis(ap=idx[:, k:k + 1], axis=0)), reads=["idx"], writes=[("gs", s)],
                    sem=("gs", l, s, gcount[0] // (NS * 250)))
                if k == 0:
                    V(lambda e, s=s: e.tensor_scalar(out=acc, in0=gs[s], scalar1=act[:, 0:1], scalar2=None, op0=ALU.mult),
                      [("gs", s), "act"], ["junk"])
                else:
                    V(lambda e, k=k, s=s: e.scalar_tensor_tensor(out=acc, in0=gs[s], scalar=act[:, k:k + 1], in1=acc,
                                                               op0=ALU.mult, op1=ALU.add), [("gs", s), "act", "junk"], ["junk"])
            V(lambda e: e.tensor_tensor(out=acc, in0=acc, in1=Grow, op=ALU.mult), ["junk", "Grow"], ["junk"])
            V(lambda e: e.tensor_tensor(out=acc, in0=acc, in1=xt, op=ALU.add), ["junk", "xt"], ["junk"])
            if final_norm:
                S(lambda e: e.activation(out=h2, in_=acc, func=AF.Square, accum_out=sm[:, 12:13]), ["junk"], ["h2", "sm12"])
                S(lambda e: e.activation(out=sm[:, 13:14], in_=sm[:, 12:13], func=AF.Sqrt, scale=1.0 / D, bias=EPS), ["sm12"], ["sm13"])
                V(lambda e: e.reciprocal(out=sm[:, 14:15], in_=sm[:, 13:14]), ["sm13"], ["sm14"])
                V(lambda e: e.scalar_tensor_tensor(out=acc, in0=acc, scalar=sm[:, 14:15], in1=nfr, op0=ALU.mult, op1=ALU.mult),
                  ["junk", "sm14", "nfr"], ["junk"])
            P.dma("scalar", lambda e, i=i: e.dma_start(out=dst(i), in_=acc), reads=["junk"], writes=[self.key("dst")], sem="acc_st")
        P.barrier()
        A.off = mark


def fm(v, nch):
    return np.ascontiguousarray(np.asarray(v, np.float32).reshape(nch, 128).T)


def make_consts():
    c = np.zeros((128, 1024), np.float32)
    k = np.arange(128)
    c[:, 0:128] = np.eye(128, dtype=np.float32)
    c[:, 128:256] = 1.0
    c[:, 256:384] = (k[:, None] <= k[None, :])
    c[:, 384:512] = (k[:, None] > k[None, :])
    c[:, 512:640] = (k[:, None] >= k[None, :])
    c[:, 640:768] = (k[:, None] < k[None, :])
    return c


def prep_shared(inp):
    g = {}
    f32 = lambda a: np.ascontiguousarray(np.asarray(a, np.float32))
    g["consts"] = make_consts()
    g["ada_w"] = f32(inp["ada_w"])
    g["ada_bT"] = np.stack([fm(inp["ada_b"][l], 48) for l in range(2)])
    g["nmixT"] = np.stack([fm(inp["norm_mix_w"][l], 8) for l in range(2)])
    g["nffnT"] = np.stack([fm(inp["norm_ffn_w"][l], 8) for l in range(2)])
    g["nf_row"] = np.ascontiguousarray(np.broadcast_to(f32(inp["norm_f_w"])[None, :], (128, D)))
    g["ev_w_in"] = f32(inp["ev_w_in"][0]); g["ev_w_out"] = f32(inp["ev_w_out"][0])
    cwf = lambda w, nj: np.ascontiguousarray(f32(w).reshape(3, nj, 128).transpose(2, 1, 0).reshape(128, nj * 3))
    g["ssd_cwT"] = cwf(inp["ssd_conv_w"][0], 16); g["sc_cwT"] = cwf(inp["sc_conv_w"][0], 8)
    g["ssd_cbT"] = fm(inp["ssd_conv_b"][0], 16)
    brow = lambda v: np.ascontiguousarray(np.broadcast_to(f32(v).reshape(1, -1), (128, f32(v).size)))
    g["dtb_row"] = brow(inp["ssd_dt_bias"][0]); g["alog_row"] = brow(inp["ssd_a_log"][0])
    g["dsk_row"] = brow(inp["ssd_d"][0]); g["ssdnw_row"] = brow(inp["ssd_norm_w"][0])
    g["od_w_in"] = f32(inp["od_w_in"][0]); g["od_w_out"] = f32(inp["od_w_out"][0])
    lg = f32(inp["hg_lb_logits"])
    g["lblT"] = np.ascontiguousarray(lg.reshape(2, 2, 8, 128).transpose(3, 0, 1, 2).reshape(128, 32))
    g["hgnw_row"] = brow(inp["hg_norm_w"][0])
    smk = np.ones((128, 8, 2, 64), np.float32); smk[:, :, :, 0] = 0.0
    g["scanmask"] = smk.reshape(128, 1024)
    g["peer_wq"] = f32(inp["peer_wq"])
    pk = f32(inp["peer_keys"])
    g["keysT"] = np.ascontiguousarray(pk.transpose(0, 4, 1, 2, 3).reshape(2, 128, 2048))
    for l in range(2):
        g["peer_u%d" % l] = f32(inp["peer_u"][l]); g["peer_v%d" % l] = f32(inp["peer_v"][l])
    return g


def prep_core(inp, b):
    m = {}
    m["x"] = np.ascontiguousarray(np.asarray(inp["x"][b], np.float32))
    m["ctx"] = np.ascontiguousarray(np.asarray(inp["ctx"][b], np.float32))
    ct = np.zeros((128, 8, 2), np.float32)
    ct[:, :, 0] = fm(inp["c"][b], 8); ct[:, :, 1] = fm(inp["c_ctx"], 8)
    m["condT"] = ct.reshape(128, 16)
    return m


def build_full():
    kb = KB()
    kb.ssd_inputs()
    kb.hg_inputs()
    kb.setup_consts()
    kb.diag = kb.A.alloc([128])
    tiles = list(range(NT))
    xtile = lambda t: (lambda i: t[i * 128:(i + 1) * 128, :])
    kb.mods_phase(0)
    src0 = kb.src_tile
    if hasattr(kb, "mixer0"):
        kb.mixer0(src0, xtile(kb.x1))
        src0 = xtile(kb.x1)
    kb.peer_phase(0, src0, xtile(kb.x2), tiles)
    kb.mods_phase(1)
    src1 = xtile(kb.x2)
    if hasattr(kb, "mixer1"):
        kb.mixer1(src1, xtile(kb.x3))
        src1 = xtile(kb.x3)
    kb.peer_phase(1, src1, lambda i: kb.out[(i - 2) * 128:(i - 1) * 128, :], tiles[2:], final_norm=True)
    kb.P.wait_all("sync", [])
    kb.P.build()
    return kb


def kernel(**inputs):
    inp = {k: np.asarray(v) for k, v in inputs.items()}
    kb = build_full()
    names = set()
    for a in kb.nc.allocations:
        if getattr(a, "kind", None) == "ExternalInput" and a.memorylocations:
            names.add(a.memorylocations[0].name)
    g = prep_shared(inp)
    in_maps = []
    for b in range(8):
        m = prep_core(inp, b)
        m.update(g)
        in_maps.append({k: v for k, v in m.items() if k in names})
    res = run_bass_kernel_spmd(kb.nc, in_maps, core_ids=list(range(8)))
    out = np.stack([np.asarray(r["out"], np.float32) for r in res.results], axis=0)
    return out


def _mix_helpers(self):
    P = self.P
    self.V = lambda fn, r, w: P.op("vector", fn, r, w)
    self.S = lambda fn, r, w: P.op("scalar", fn, r, w)
    self.T = lambda fn, r, w: P.op("tensor", fn, r, w)
    self.G = lambda fn, r, w: P.op("gpsimd", fn, r, w)


def load_w_bf16(self, dst, src, col0, ncols, key, stage):
    P = self.P
    piece = stage[0].shape[-1]
    n = 0
    for k in range(8):
        c = 0
        while c < ncols:
            w = min(piece, ncols - c)
            s = n % 2; n += 1
            P.dma("sync" if s == 0 else "scalar",
                  lambda e, k=k, c=c, w=w, s=s: e.dma_start(out=stage[s][:, 0:w], in_=src[k * 128:(k + 1) * 128, col0 + c:col0 + c + w]),
                  writes=[("stg", s)])
            P.op("gpsimd", lambda e, k=k, c=c, w=w, s=s: e.tensor_copy(out=dst[:, k, c:c + w], in_=stage[s][:, 0:w]),
                 reads=[("stg", s)], writes=[key])
            c += w


def norm_T(self, src_ap, Acol, Bcol, ckeys, dst, dkey, bufs):
    P, V, S, T = self.P, self.V, self.S, self.T
    xt, xnb, sm = bufs
    P.dma("sync", lambda e: e.dma_start(out=xt, in_=src_ap), writes=["n_xt"])
    S(lambda e: e.activation(out=xnb, in_=xt, func=AF.Square, accum_out=sm[:, 0:1]), ["n_xt"], ["n_xnb", "n_sm0"])
    S(lambda e: e.activation(out=sm[:, 1:2], in_=sm[:, 0:1], func=AF.Sqrt, scale=1.0 / D, bias=EPS), ["n_sm0"], ["n_sm1"])
    V(lambda e: e.reciprocal(out=sm[:, 2:3], in_=sm[:, 1:2]), ["n_sm1"], ["n_sm2"])
    V(lambda e: e.tensor_scalar(out=xnb, in0=xt, scalar1=sm[:, 2:3], scalar2=None, op0=ALU.mult), ["n_xt", "n_sm2"], ["n_xnb"])
    pb = self.psum[:, 7, :].bitcast(BF16)
    for k in range(8):
        T(lambda e, k=k: e.transpose(pb[:, k * 128:(k + 1) * 128], xnb[:, k * 128:(k + 1) * 128], self.identb),
          ["n_xnb", "identb"], [("ps", 7)])
    for k in range(8):
        S(lambda e, k=k: e.activation(out=dst[:, k, :], in_=pb[:, k * 128:(k + 1) * 128], func=AF.Identity,
                                      scale=Acol[:, k:k + 1], bias=Bcol[:, k:k + 1]), [("ps", 7)] + list(ckeys), [dkey])


def proj_fm(self, dst, dkey, w, wkey, col0, nj, hT, hkey, W):
    for jj in range(nj):
        b = 6
        for k in range(8):
            self.T(lambda e, jj=jj, k=k: e.matmul(self.bank(b, W), lhsT=w[:, k, col0 + jj * 128:col0 + (jj + 1) * 128], rhs=hT[:, k, 0:W],
                                                  start=(k == 0), stop=(k == 7)), [wkey, hkey], [("ps", b)])
        self.S(lambda e, jj=jj: e.copy(out=dst[:, jj, 0:W], in_=self.bank(b, W)), [("ps", b)], [dkey])


def conv3(self, dst, dkey, src, skey, cw, cwkey, nj, W, rows):
    L = W // rows
    for j in range(nj):
        d3 = dst[:, j, 0:W].rearrange("p (r l) -> p r l", l=L)
        s3 = src[:, j, 0:W].rearrange("p (r l) -> p r l", l=L)
        self.V(lambda e, j=j: e.tensor_scalar(out=dst[:, j, 0:W], in0=src[:, j, 0:W], scalar1=cw[:, j, 1:2], scalar2=None, op0=ALU.mult),
               [skey, cwkey], [dkey])
        self.V(lambda e, j=j, d3=d3, s3=s3: e.scalar_tensor_tensor(out=d3[:, :, 1:L], in0=s3[:, :, 0:L - 1], scalar=cw[:, j, 0:1],
                                                                   in1=d3[:, :, 1:L], op0=ALU.mult, op1=ALU.add), [skey, cwkey, dkey], [dkey])
        self.V(lambda e, j=j, d3=d3, s3=s3: e.scalar_tensor_tensor(out=d3[:, :, 0:L - 1], in0=s3[:, :, 1:L], scalar=cw[:, j, 2:3],
                                                                   in1=d3[:, :, 0:L - 1], op0=ALU.mult, op1=ALU.add), [skey, cwkey, dkey], [dkey])


KB._mix_helpers = _mix_helpers
KB.load_w_bf16 = load_w_bf16
KB.norm_T = norm_T
KB.proj_fm = proj_fm
KB.conv3 = conv3


def ssd_inputs(self):
    nc = self.nc
    di = lambda n, s, dt=F32: nc.dram_tensor(n, list(s), dt, kind="ExternalInput")
    self.ev_w_in = di("ev_w_in", [D, 6176]); self.ev_w_out = di("ev_w_out", [2048, D])
    self.ssd_cwT = di("ssd_cwT", [128, 48]); self.ssd_cbT = di("ssd_cbT", [128, 16]); self.sc_cwT = di("sc_cwT", [128, 24])
    self.dtb_row = di("dtb_row", [128, 32]); self.alog_row = di("alog_row", [128, 32])
    self.dsk_row = di("dsk_row", [128, 16]); self.ssdnw_row = di("ssdnw_row", [128, D])
    self.yscr = nc.dram_tensor("yscr", [NT * 128, D], F32, kind=self.dbg.get("yscr", "Internal"))
    self.xsscr = nc.dram_tensor("xsscr", [NT * 128, D], BF16, kind="Internal")


def mix_cols(self, l, c, nT, Acol, key):
    m = self.modsFM[l]
    self.V(lambda e: e.scalar_tensor_tensor(out=Acol, in0=m[:, 8:16, c], scalar=1.0, in1=nT, op0=ALU.add, op1=ALU.mult),
           [("mods", l), "nT"], [key])
    return m[:, 0:8, c]


def ssd_scan(self, d, src_fn):
    P, A, V, S, T = self.P, self.A, self.V, self.S, self.T
    mark = A.off
    l = 0
    wx = A.alloc([8, 2080], BF16)
    stage = [A.alloc([2080]) for _ in range(2)]
    self.load_w_bf16(wx, self.ev_w_in, 4096, 2080, "wx", stage)
    cw = A.alloc([16, 3]); cb = A.alloc([16]); nT = A.alloc([8])
    dtb = A.alloc([32]); arow = A.alloc([32])
    for dst_, src_, k_ in ((cw, self.ssd_cwT.ap().rearrange("p (a b) -> p a b", b=3), "cw"), (cb, self.ssd_cbT.ap(), "cb"),
                           (nT, self.nmixT[l], "nT"), (dtb, self.dtb_row.ap(), "dtb"), (arow, self.alog_row.ap(), "arow")):
        P.dma("sync", lambda e, dst_=dst_, src_=src_: e.dma_start(out=dst_, in_=src_), writes=[k_], sem="misc")
    S(lambda e: e.activation(out=arow, in_=arow, func=AF.Exp), ["arow"], ["arow"])
    V(lambda e: e.tensor_scalar(out=arow, in0=arow, scalar1=-1.0, scalar2=None, op0=ALU.mult), ["arow"], ["arow"])
    Acol = [A.alloc([8]) for _ in range(2)]
    Bcol = [self.mix_cols(l, c, nT, Acol[c], ("Acol", c)) for c in range(2)]
    xt = A.alloc([D]); xnb = A.alloc([D], BF16); sm = A.alloc([4])
    hTu = A.alloc([8, 256], BF16)
    praw = A.alloc([16, 256]); cv = A.alloc([16, 256]); xbcT = A.alloc([16, 256], BF16)
    dt = A.alloc([2, 16]); dta = A.alloc([2, 16]); e48 = A.alloc([48]); wend = A.alloc([16])
    R = A.alloc([16, 128]); cbm = A.alloc([4, 128]); Eq = A.alloc([4, 128]); attT = A.alloc([16, 128], BF16)
    xs_tm = A.alloc([D], BF16); bm_tm = A.alloc([512], BF16); xw = A.alloc([D], BF16)
    ych = A.alloc([D]); ypv = A.alloc([D]); St = A.alloc([D]); Sb = A.alloc([D], BF16)
    V(lambda e: e.memset(St, 0.0), [], ["St"])
    V(lambda e: e.memset(Sb, 0.0), [], ["Sb"])
    cst = self.cst
    if d == 0:
        Bm = cst[:, 256:384]; Am = cst[:, 384:512]; Mk = cst[:, 256:384]
    else:
        Bm = cst[:, 512:640]; Am = cst[:, 640:768]; Mk = cst[:, 512:640]
    units = [(0, 256, 1)] + [(i, 128, 0) for i in range(2, NT)]
    if d == 1:
        units = [units[0]] + units[1:][::-1]
    pbf = lambda b: self.psum[:, b, :].bitcast(BF16)
    def unit_body(t0, W, c):
        nsub = W // 128
        for sub in range(nsub):
            self.norm_T(src_fn(t0 + sub), Acol[c], Bcol[c], [("Acol", c), ("mods", l)], hTu[:, :, sub * 128:(sub + 1) * 128], "hTu", (xt, xnb, sm))
        self.proj_fm(praw, "praw", wx, "wx", 0, 16, hTu, "hTu", W)
        self.conv3(cv, "cv", praw, "praw", cw, "cw", 16, W, 1 if c == 1 else W // 64)
        for j in range(16):
            S(lambda e, j=j: e.activation(out=xbcT[:, j, 0:W], in_=cv[:, j, 0:W], func=AF.Silu, bias=cb[:, j:j + 1]), ["cv", "cb"], ["xbcT"])
        for sub in range(nsub):
            for k in range(8):
                T(lambda e, k=k, sub=sub: e.matmul(self.bank(6, 32), lhsT=hTu[:, k, sub * 128:(sub + 1) * 128], rhs=wx[:, k, 2048:2080],
                                                   start=(k == 0), stop=(k == 7)), ["hTu", "wx"], [("ps", 6)])
            V(lambda e, sub=sub: e.tensor_tensor(out=dt[:, sub, :], in0=self.bank(6, 16, d * 16), in1=dtb[:, d * 16:(d + 1) * 16], op=ALU.add),
              [("ps", 6), "dtb"], ["dt"])
        S(lambda e: e.activation(out=dt[:, 0:nsub, :], in_=dt[:, 0:nsub, :], func=AF.Exp), ["dt"], ["dt"])
        S(lambda e: e.activation(out=dt[:, 0:nsub, :], in_=dt[:, 0:nsub, :], func=AF.Ln, bias=1.0), ["dt"], ["dt"])
        V(lambda e: e.tensor_tensor(out=dta[:, 0:nsub, :], in0=dt[:, 0:nsub, :],
                                    in1=arow[:, d * 16:(d + 1) * 16].unsqueeze(1).to_broadcast([128, nsub, 16]), op=ALU.mult),
          ["dt", "arow"], ["dta"])
        chunks = list(range(nsub))
        if d == 1:
            chunks = chunks[::-1]
        def chunk_body(ch):
            tile = t0 + ch
            c0 = ch * 128
            cols = slice(c0, c0 + 128)
            for j in range(8):
                T(lambda e, j=j: e.transpose(pbf(7)[:, j * 128:(j + 1) * 128], xbcT[:, j, cols], self.identb), ["xbcT", "identb"], [("ps", 7)])
            S(lambda e: e.copy(out=xs_tm, in_=pbf(7)), [("ps", 7)], ["xs_tm"])
            for j in range(4):
                T(lambda e, j=j: e.transpose(pbf(6)[:, j * 128:(j + 1) * 128], xbcT[:, 8 + j, cols], self.identb), ["xbcT", "identb"], [("ps", 6)])
            S(lambda e: e.copy(out=bm_tm, in_=pbf(6)[:, 0:512]), [("ps", 6)], ["bm_tm"])
            for g in range(4):
                T(lambda e, g=g: e.matmul(self.bank(7, 128, g * 128), lhsT=xbcT[:, 8 + g, cols], rhs=xbcT[:, 12 + g, cols], start=True, stop=True),
                  ["xbcT"], [("ps", 7)])
            V(lambda e: e.tensor_tensor(out=cbm, in0=self.bank(7).rearrange("p (g t) -> p g t", t=128),
                                        in1=Mk.unsqueeze(1).to_broadcast([128, 4, 128]), op=ALU.mult), [("ps", 7), "cst"], ["cbm"])
            dta_c = dta[:, ch, :]; dt_c = dt[:, ch, :]
            for n_, lh in enumerate((Bm, Am, self.ones)):
                T(lambda e, n_=n_, lh=lh: e.matmul(self.bank(6, 16, n_ * 16), lhsT=lh, rhs=dta_c, start=True, stop=True), ["dta", "cst"], [("ps", 6)])
            S(lambda e: e.activation(out=e48, in_=self.bank(6, 48), func=AF.Exp), [("ps", 6)], ["e48"])
            V(lambda e: e.tensor_tensor(out=wend, in0=e48[:, 16:32], in1=dt_c, op=ALU.mult), ["e48", "dt"], ["wend"])
            V(lambda e: e.tensor_tensor(out=R, in0=dta_c.unsqueeze(2).to_broadcast([128, 16, 128]),
                                        in1=Bm.unsqueeze(1).to_broadcast([128, 16, 128]), op=ALU.mult), ["dta", "cst"], ["R"])
            for q in range(4):
                b = q % 2
                T(lambda e, q=q, b=b: e.matmul(self.bank(b), lhsT=Am, rhs=R[:, q * 4:(q + 1) * 4, :].rearrange("p a b -> p (a b)"), start=True, stop=True),
                  ["R", "cst"], [("ps", b)])
                S(lambda e, b=b: e.activation(out=Eq.rearrange("p a b -> p (a b)"), in_=self.bank(b), func=AF.Exp), [("ps", b)], ["Eq"])
                V(lambda e, q=q: e.tensor_tensor(out=Eq, in0=Eq, in1=cbm[:, q:q + 1, :].to_broadcast([128, 4, 128]), op=ALU.mult), ["Eq", "cbm"], ["Eq"])
                V(lambda e, q=q: e.tensor_tensor(out=attT[:, q * 4:(q + 1) * 4, :], in0=Eq,
                                                 in1=dt_c[:, q * 4:(q + 1) * 4].unsqueeze(2).to_broadcast([128, 4, 128]), op=ALU.mult),
                  ["Eq", "dt"], ["attT"])
            for h in range(16):
                T(lambda e, h=h: e.matmul(self.bank(2 + h // 8, 64, (h % 8) * 64), lhsT=attT[:, h, :], rhs=xs_tm[:, h * 64:(h + 1) * 64], start=True, stop=True),
                  ["attT", "xs_tm"], [("ps", 2 + h // 8)])
            for g in range(4):
                T(lambda e, g=g: e.matmul(self.bank(4 + g // 2, 256, (g % 2) * 256), lhsT=xbcT[:, 12 + g, cols], rhs=Sb[:, g * 256:(g + 1) * 256], start=True, stop=True),
                  ["xbcT", "Sb"], [("ps", 4 + g // 2)])
            V(lambda e: e.tensor_tensor(out=ych.rearrange("p (h q) -> p h q", q=64), in0=self.psum[:, 4:6, :].rearrange("p a (h q) -> p (a h) q", q=64),
                                        in1=e48[:, 0:16].unsqueeze(2).to_broadcast([128, 16, 64]), op=ALU.mult), [("ps", 4), ("ps", 5), "e48"], ["ych"])
            V(lambda e: e.tensor_tensor(out=ych.rearrange("p (a b) -> p a b", b=512), in0=ych.rearrange("p (a b) -> p a b", b=512), in1=self.psum[:, 2:4, :], op=ALU.add),
              ["ych", ("ps", 2), ("ps", 3)], ["ych"])
            ydst = self.yscr[tile * 128:(tile + 1) * 128, :]
            if d == 1:
                P.dma("sync", lambda e, ydst=ydst: e.dma_start(out=ypv, in_=ydst), reads=[("yscr", tile)], writes=["ypv"])
                V(lambda e: e.tensor_tensor(out=ych, in0=ych, in1=ypv, op=ALU.add), ["ych", "ypv"], ["ych"])
                P.dma("scalar", lambda e, tile=tile: e.dma_start(out=self.xsscr[tile * 128:(tile + 1) * 128, :], in_=xs_tm), reads=["xs_tm"],
                      writes=[("xsscr", tile)], sem="xs_st")
            P.dma("scalar", lambda e, ydst=ydst: e.dma_start(out=ydst, in_=ych), reads=["ych"], writes=[("yscr", tile)], sem="y_st")
            V(lambda e: e.tensor_tensor(out=xw.rearrange("p (h q) -> p h q", q=64), in0=xs_tm.rearrange("p (h q) -> p h q", q=64),
                                        in1=wend.unsqueeze(2).to_broadcast([128, 16, 64]), op=ALU.mult), ["xs_tm", "wend"], ["xw"])
            for g in range(4):
                T(lambda e, g=g: e.matmul(self.bank(4 + g // 2, 256, (g % 2) * 256), lhsT=bm_tm[:, g * 128:(g + 1) * 128], rhs=xw[:, g * 256:(g + 1) * 256], start=True, stop=True),
                  ["bm_tm", "xw"], [("ps", 4 + g // 2)])
            V(lambda e: e.tensor_tensor(out=St.rearrange("p (h q) -> p h q", q=64), in0=St.rearrange("p (h q) -> p h q", q=64),
                                        in1=e48[:, 32:48].unsqueeze(2).to_broadcast([128, 16, 64]), op=ALU.mult), ["St", "e48"], ["St"])
            V(lambda e: e.tensor_tensor(out=St.rearrange("p (a b) -> p a b", b=512), in0=St.rearrange("p (a b) -> p a b", b=512), in1=self.psum[:, 4:6, :], op=ALU.add),
              ["St", ("ps", 4), ("ps", 5)], ["St"])
            S(lambda e: e.copy(out=Sb, in_=St), ["St"], ["Sb"])
        for ch in chunks:
            chunk_body(ch)
    for u in units:
        unit_body(*u)
    P.barrier()
    A.off = mark


KB.ssd_inputs = ssd_inputs
KB.mix_cols = mix_cols
KB.ssd_scan = ssd_scan


def ssd_finish(self, src_fn, dst_fn):
    P, A, V, S, T = self.P, self.A, self.V, self.S, self.T
    mark = A.off
    l = 0
    wc = A.alloc([8, 4096], BF16)
    wo = A.alloc([16, D], BF16)
    stage = [A.alloc([2048]) for _ in range(2)]
    self.load_w_bf16(wc, self.ev_w_in, 0, 4096, "wc", stage)
    n = 0
    for k in range(16):
        s = n % 2; n += 1
        P.dma("sync" if s == 0 else "scalar", lambda e, k=k, s=s: e.dma_start(out=stage[s][:, 0:D], in_=self.ev_w_out[k * 128:(k + 1) * 128, :]),
              writes=[("stg", s)])
        P.op("gpsimd", lambda e, k=k, s=s: e.tensor_copy(out=wo[:, k, :], in_=stage[s][:, 0:D]), reads=[("stg", s)], writes=["wo"])
    A.off -= 2 * 2048
    P.barrier()
    cw = A.alloc([8, 3]); nT = A.alloc([8]); dsk = A.alloc([16]); nwr = A.alloc([D])
    for dst_, src_, k_ in ((cw, self.sc_cwT.ap().rearrange("p (a b) -> p a b", b=3), "cw"), (nT, self.nmixT[l], "nT"),
                           (dsk, self.dsk_row.ap(), "dsk"), (nwr, self.ssdnw_row.ap(), "nwr")):
        P.dma("sync", lambda e, dst_=dst_, src_=src_: e.dma_start(out=dst_, in_=src_), writes=[k_], sem="misc")
    Acol = [A.alloc([8]) for _ in range(2)]
    Bcol = [self.mix_cols(l, c, nT, Acol[c], ("Acol", c)) for c in range(2)]
    Grow = A.alloc([D])
    xt = A.alloc([D]); xnb = A.alloc([D], BF16); sm = A.alloc([16])
    hTu = A.alloc([8, 256], BF16)
    praw = A.alloc([16, 256]); cv = A.alloc([8, 256]); yaT = A.alloc([8, 256], BF16)
    zs = A.alloc([D]); ysum = A.alloc([D]); xsb = A.alloc([D], BF16); yb = A.alloc([D]); ybn = A.alloc([D], BF16)
    ybT = A.alloc([8, 128], BF16); xo = zs
    pbf = lambda b: self.psum[:, b, :].bitcast(BF16)
    units = [(0, 256, 1)] + [(i, 128, 0) for i in range(2, NT)]
    cur_c = [None]
    def unit_body(t0, W, c):
        nsub = W // 128
        if c != cur_c[0]:
            cur_c[0] = c
            self.make_row(Grow, self.modsFM[l][:, 16:24, c], "Grow", [("mods", l)], banks=(4, 5))
        for sub in range(nsub):
            self.norm_T(src_fn(t0 + sub), Acol[c], Bcol[c], [("Acol", c), ("mods", l)], hTu[:, :, sub * 128:(sub + 1) * 128], "hTu", (xt, xnb, sm))
        self.proj_fm(praw, "praw", wc, "wc", 0, 8, hTu, "hTu", W)
        self.proj_fm(praw[:, 8:16, :], "praw2", wc, "wc", 2048, 8, hTu, "hTu", W)
        V(lambda e: e.tensor_tensor(out=praw[:, 0:8, 0:W], in0=praw[:, 0:8, 0:W], in1=praw[:, 8:16, 0:W], op=ALU.mult), ["praw", "praw2"], ["praw"])
        self.conv3(cv, "cv", praw, "praw", cw, "cw", 8, W, 1 if c == 1 else W // 64)
        self.proj_fm(praw[:, 8:16, :], "praw2", wc, "wc", 1024, 8, hTu, "hTu", W)
        V(lambda e: e.tensor_tensor(out=yaT[:, :, 0:W], in0=cv[:, :, 0:W], in1=praw[:, 8:16, 0:W], op=ALU.mult), ["cv", "praw2"], ["yaT"])
        def sub_body(sub):
            tile = t0 + sub
            cols = slice(sub * 128, (sub + 1) * 128)
            for n2 in range(2):
                for k in range(8):
                    T(lambda e, n2=n2, k=k: e.matmul(self.bank(n2), lhsT=hTu[:, k, cols], rhs=wc[:, k, 3072 + n2 * 512:3072 + (n2 + 1) * 512],
                                                     start=(k == 0), stop=(k == 7)), ["hTu", "wc"], [("ps", n2)])
            S(lambda e: e.activation(out=zs.rearrange("p (a b) -> p a b", b=512), in_=self.psum[:, 0:2, :], func=AF.Silu), [("ps", 0), ("ps", 1)], ["zs"])
            P.dma("sync", lambda e, tile=tile: e.dma_start(out=ysum, in_=self.yscr[tile * 128:(tile + 1) * 128, :]), reads=[("yscr", tile)], writes=["ysum"])
            P.dma("sync", lambda e, tile=tile: e.dma_start(out=xsb, in_=self.xsscr[tile * 128:(tile + 1) * 128, :]), reads=[("xsscr", tile)], writes=["xsb"])
            V(lambda e: e.tensor_tensor(out=yb.rearrange("p (h q) -> p h q", q=64), in0=xsb.rearrange("p (h q) -> p h q", q=64),
                                        in1=dsk.unsqueeze(2).to_broadcast([128, 16, 64]), op=ALU.mult), ["xsb", "dsk"], ["yb"])
            V(lambda e: e.tensor_tensor(out=yb, in0=yb, in1=ysum, op=ALU.add), ["yb", "ysum"], ["yb"])
            V(lambda e: e.tensor_tensor(out=yb, in0=yb, in1=zs, op=ALU.mult), ["yb", "zs"], ["yb"])
            for g in range(4):
                S(lambda e, g=g: e.activation(out=ysum[:, g * 256:(g + 1) * 256], in_=yb[:, g * 256:(g + 1) * 256], func=AF.Square, accum_out=sm[:, 4 + g:5 + g]),
                  ["yb"], ["ysum", "sm4"])
            S(lambda e: e.activation(out=sm[:, 8:12], in_=sm[:, 4:8], func=AF.Sqrt, scale=1.0 / 256, bias=EPS), ["sm4"], ["sm8"])
            V(lambda e: e.reciprocal(out=sm[:, 8:12], in_=sm[:, 8:12]), ["sm8"], ["sm8"])
            V(lambda e: e.tensor_tensor(out=yb.rearrange("p (g q) -> p g q", q=256), in0=yb.rearrange("p (g q) -> p g q", q=256),
                                        in1=sm[:, 8:12].unsqueeze(2).to_broadcast([128, 4, 256]), op=ALU.mult), ["yb", "sm8"], ["yb"])
            V(lambda e: e.tensor_tensor(out=ybn, in0=yb, in1=nwr, op=ALU.mult), ["yb", "nwr"], ["ybn"])
            for k in range(8):
                T(lambda e, k=k: e.transpose(pbf(7)[:, k * 128:(k + 1) * 128], ybn[:, k * 128:(k + 1) * 128], self.identb), ["ybn", "identb"], [("ps", 7)])
            S(lambda e: e.copy(out=ybT.rearrange("p a b -> p (a b)"), in_=pbf(7)), [("ps", 7)], ["ybT"])
            for n2 in range(2):
                for kc in range(16):
                    lh = yaT[:, kc, cols] if kc < 8 else ybT[:, kc - 8, :]
                    T(lambda e, n2=n2, kc=kc, lh=lh: e.matmul(self.bank(2 + n2), lhsT=lh, rhs=wo[:, kc, n2 * 512:(n2 + 1) * 512],
                                                              start=(kc == 0), stop=(kc == 15)), ["yaT", "ybT", "wo"], [("ps", 2 + n2)])
            V(lambda e: e.tensor_tensor(out=xo.rearrange("p (a b) -> p a b", b=512), in0=self.psum[:, 2:4, :], in1=Grow.rearrange("p (a b) -> p a b", b=512), op=ALU.mult),
              [("ps", 2), ("ps", 3), "Grow"], ["zs"])
            P.dma("sync", lambda e, tile=tile: e.dma_start(out=ysum, in_=src_fn(tile)), writes=["ysum"])
            V(lambda e: e.tensor_tensor(out=xo, in0=xo, in1=ysum, op=ALU.add), ["zs", "ysum"], ["zs"])
            P.dma("scalar", lambda e, tile=tile: e.dma_start(out=dst_fn(tile), in_=xo), reads=["zs"], writes=[self.key("x1")], sem="xo_st")
        for sub in range(nsub):
            sub_body(sub)
    for u in units:
        unit_body(*u)
    P.barrier()
    A.off = mark


def mixer0(self, src_fn, dst_fn):
    self._mix_helpers()
    self.ssd_scan(0, src_fn)
    self.ssd_scan(1, src_fn)
    self.ssd_finish(src_fn, dst_fn)


KB.ssd_finish = ssd_finish
KB.mixer0 = mixer0


def hg_inputs(self):
    nc = self.nc
    di = lambda n, s, dt=F32: nc.dram_tensor(n, list(s), dt, kind="ExternalInput")
    self.od_w_in = di("od_w_in", [D, 5120]); self.od_w_out = di("od_w_out", [D, D])
    self.lblT = di("lblT", [128, 32])
    self.hgnw_row = di("hgnw_row", [128, D])
    self.scanmask = di("scanmask", [128, 1024])


def hg_pass(self, d, src_fn, dst_fn):
    P, A, V, S, T = self.P, self.A, self.V, self.S, self.T
    mark = A.off
    l = 1
    fin = (d == 1)
    ncol = 5 if fin else 3
    w = A.alloc([8, ncol * 1024], BF16)
    stage = [A.alloc([1024]) for _ in range(2)]
    colsrc = [0, 1024 + d * 1024, 3072, 4096]
    for n_, c0 in enumerate(colsrc[:4 if fin else 3]):
        self.load_w_bf16(w[:, :, n_ * 1024:(n_ + 1) * 1024], self.od_w_in, c0, 1024, "w", stage)
    if fin:
        n = 0
        for k in range(8):
            s = n % 2; n += 1
            P.dma("sync" if s == 0 else "scalar", lambda e, k=k, s=s: e.dma_start(out=stage[s], in_=self.od_w_out[k * 128:(k + 1) * 128, :]), writes=[("stg", s)])
            P.op("gpsimd", lambda e, k=k, s=s: e.tensor_copy(out=w[:, k, 4096:5120], in_=stage[s]), reads=[("stg", s)], writes=["w"])
    P.barrier()
    A.off -= 2 * 1024
    nT = A.alloc([8]); lbl = A.alloc([32]); lb = A.alloc([8]); oml = A.alloc([8]); smask = A.alloc([D])
    nwr = A.alloc([D])
    for dst_, src_, k_ in ((nT, self.nmixT[l], "nT"), (lbl, self.lblT.ap(), "lbl"), (smask, self.scanmask.ap(), "smask"), (nwr, self.hgnw_row.ap(), "nwr")):
        P.dma("sync", lambda e, dst_=dst_, src_=src_: e.dma_start(out=dst_, in_=src_), writes=[k_], sem="misc")
    V(lambda e: e.tensor_tensor(out=lb, in0=lbl[:, 16 + d * 8:24 + d * 8], in1=lbl[:, d * 8:d * 8 + 8], op=ALU.subtract), ["lbl"], ["lb"])
    S(lambda e: e.activation(out=lb, in_=lb, func=AF.Sigmoid), ["lb"], ["lb"])
    V(lambda e: e.tensor_scalar(out=oml, in0=lb, scalar1=-1.0, scalar2=1.0, op0=ALU.mult, op1=ALU.add), ["lb"], ["oml"])
    Acol = [A.alloc([8]) for _ in range(2)]
    Bcol = [self.mix_cols(l, c, nT, Acol[c], ("Acol", c)) for c in range(2)]
    xt = A.alloc([D]); xnb = A.alloc([D], BF16); sm = A.alloc([32])
    hT = A.alloc([8, 128], BF16)
    praw = A.alloc([8, 128]); qF = A.alloc([8, 128]); fF = A.alloc([8, 128]); gl = A.alloc([8, 128]); GC = A.alloc([8, 128])
    ex = A.alloc([8, 128])
    qg = A.alloc([8, 128], BF16); kg = A.alloc([8, 128], BF16); kend = A.alloc([8, 128], BF16); iF = A.alloc([8, 128], BF16)
    eGE = A.alloc([8, 2]); GE = A.alloc([8, 2])
    v_tm = A.alloc([D], BF16); kend_tm = A.alloc([D], BF16); attm = A.alloc([8, 64], BF16)
    och = A.alloc([D]); St = A.alloc([8, 128]); Sb = A.alloc([8, 128], BF16)
    V(lambda e: e.memset(St, 0.0), [], ["St"])
    V(lambda e: e.memset(Sb, 0.0), [], ["Sb"])
    if fin:
        opv = A.alloc([D]); gs = A.alloc([D]); onb = A.alloc([D], BF16); onT = A.alloc([8, 64], BF16)
        Grow = A.alloc([D])
        self.make_row(Grow, self.modsFM[l][:, 16:24, 0], "Grow", [("mods", l)], banks=(4, 5))
    cst = self.cst
    Mk = cst[0:64, 256:320] if d == 0 else cst[0:64, 512:576]
    pbf = lambda b: self.psum[:, b, :].bitcast(BF16)
    tiles = [0, 1] + list(range(2, NT))
    if d == 1:
        tiles = [1, 0] + list(range(NT - 1, 1, -1))
    flat = lambda a: a.rearrange("p a b -> p (a b)")
    bc8 = lambda col: col.unsqueeze(2).to_broadcast([128, 8, 128])

    def tile_body(i):
        c = 1 if i < 2 else 0
        need_out = (i >= 2)
        self.norm_T(src_fn(i), Acol[c], Bcol[c], [("Acol", c), ("mods", l)], hT, "hT", (xt, xnb, sm))
        if need_out:
            self.proj_fm(praw, "praw", w, "w", 0, 8, hT, "hT", 128)
            S(lambda e: e.activation(out=qF, in_=praw, func=AF.Silu), ["praw"], ["qF"])
        self.proj_fm(praw, "praw", w, "w", 1024, 8, hT, "hT", 128)
        S(lambda e: e.activation(out=fF, in_=praw, func=AF.Sigmoid), ["praw"], ["fF"])
        V(lambda e: e.tensor_tensor(out=fF, in0=fF, in1=bc8(oml), op=ALU.mult), ["fF", "oml"], ["fF"])
        V(lambda e: e.tensor_tensor(out=fF, in0=fF, in1=bc8(lb), op=ALU.add), ["fF", "lb"], ["fF"])
        S(lambda e: e.activation(out=gl, in_=fF, func=AF.Ln), ["fF"], ["gl"])
        V(lambda e: e.tensor_scalar(out=fF, in0=fF, scalar1=-1.0, scalar2=1.0, op0=ALU.mult, op1=ALU.add), ["fF"], ["fF"])
        V(lambda e: e.tensor_tensor_scan(out=flat(GC), data0=smask, data1=flat(gl), initial=0.0, op0=ALU.mult, op1=ALU.add), ["smask", "gl"], ["GC"])
        GC4 = GC.rearrange("p j (c t) -> p j c t", t=64)
        V(lambda e: e.tensor_copy(out=GE, in_=GC4[:, :, :, 63]), ["GC"], ["GE"])
        if d == 1:
            V(lambda e: e.tensor_tensor(out=GC, in0=gl, in1=GC, op=ALU.subtract), ["gl", "GC"], ["GC"])
            V(lambda e: e.tensor_tensor(out=GC4, in0=GC4, in1=GE.unsqueeze(3).to_broadcast([128, 8, 2, 64]), op=ALU.add), ["GC", "GE"], ["GC"])
        S(lambda e: e.activation(out=eGE, in_=GE, func=AF.Exp), ["GE"], ["eGE"])
        if need_out:
            S(lambda e: e.activation(out=ex, in_=GC, func=AF.Exp), ["GC"], ["ex"])
            V(lambda e: e.scalar_tensor_tensor(out=flat(qg), in0=flat(qF), scalar=float(128 ** -0.5), in1=flat(ex), op0=ALU.mult, op1=ALU.mult), ["qF", "ex"], ["qg"])
            S(lambda e: e.activation(out=ex, in_=GC, func=AF.Exp, scale=-1.0), ["GC"], ["ex"])
            V(lambda e: e.tensor_tensor(out=kg, in0=fF, in1=ex, op=ALU.mult), ["fF", "ex"], ["kg"])
        V(lambda e: e.tensor_tensor(out=GC4, in0=GE.unsqueeze(3).to_broadcast([128, 8, 2, 64]), in1=GC4, op=ALU.subtract), ["GC", "GE"], ["GC"])
        S(lambda e: e.activation(out=ex, in_=GC, func=AF.Exp), ["GC"], ["ex"])
        V(lambda e: e.tensor_tensor(out=kend, in0=fF, in1=ex, op=ALU.mult), ["fF", "ex"], ["kend"])
        self.proj_fm(praw, "praw", w, "w", 2048, 8, hT, "hT", 128)
        V(lambda e: e.tensor_copy(out=iF, in_=praw), ["praw"], ["iF"])
        subs = [0, 1] if d == 0 else [1, 0]

        def sub_body(sc):
            cols = slice(sc * 64, sc * 64 + 64)
            rows = slice(i * 128 + sc * 64, i * 128 + sc * 64 + 64)
            for j in range(8):
                T(lambda e, j=j: e.transpose(pbf(7)[0:64, j * 128:(j + 1) * 128], iF[:, j, cols], self.identb), ["iF", "identb"], [("ps", 7)])
            S(lambda e: e.copy(out=v_tm[0:64, :], in_=pbf(7)[0:64, :]), [("ps", 7)], ["v_tm"])
            for j in range(8):
                T(lambda e, j=j: e.transpose(pbf(6)[0:64, j * 128:(j + 1) * 128], kend[:, j, cols], self.identb), ["kend", "identb"], [("ps", 6)])
            S(lambda e: e.copy(out=kend_tm[0:64, :], in_=pbf(6)[0:64, :]), [("ps", 6)], ["kend_tm"])
            if need_out:
                for j in range(8):
                    T(lambda e, j=j: e.matmul(self.psum[0:64, 0, j * 64:(j + 1) * 64], lhsT=kg[:, j, cols], rhs=qg[:, j, cols], start=True, stop=True),
                      ["kg", "qg"], [("ps", 0)])
                V(lambda e: e.tensor_tensor(out=attm[0:64], in0=self.psum[0:64, 0, :].rearrange("p (j t) -> p j t", t=64),
                                            in1=Mk.unsqueeze(1).to_broadcast([64, 8, 64]), op=ALU.mult), [("ps", 0), "cst"], ["attm"])
                for j in range(8):
                    ob = self.psum[0:64, 2 + j // 4, (j % 4) * 128:(j % 4 + 1) * 128]
                    T(lambda e, j=j, ob=ob: e.matmul(ob, lhsT=attm[0:64, j, :], rhs=v_tm[0:64, j * 128:(j + 1) * 128], start=True, stop=False),
                      ["attm", "v_tm"], [("ps", 2 + j // 4)])
                    T(lambda e, j=j, ob=ob: e.matmul(ob, lhsT=qg[:, j, cols], rhs=Sb[:, j, :], start=False, stop=True),
                      ["qg", "Sb"], [("ps", 2 + j // 4)])
                ydst = self.yscr[rows, :]
                if not fin:
                    S(lambda e: e.copy(out=och[0:64].rearrange("p (a b) -> p a b", b=512), in_=self.psum[0:64, 2:4, :]), [("ps", 2), ("ps", 3)], ["och"])
                    P.dma("scalar", lambda e: e.dma_start(out=ydst, in_=och[0:64]), reads=["och"], writes=[("yscr", i, sc)], sem="y_st")
                else:
                    P.dma("sync", lambda e: e.dma_start(out=opv[0:64], in_=ydst), reads=[("yscr", i, sc)], writes=["opv"])
                    V(lambda e: e.tensor_tensor(out=och[0:64].rearrange("p (a b) -> p a b", b=512), in0=self.psum[0:64, 2:4, :],
                                                in1=opv[0:64].rearrange("p (a b) -> p a b", b=512), op=ALU.add), [("ps", 2), ("ps", 3), "opv"], ["och"])
            for j in range(8):
                T(lambda e, j=j: e.matmul(self.psum[:, 4 + j // 4, (j % 4) * 128:(j % 4 + 1) * 128], lhsT=kend_tm[0:64, j * 128:(j + 1) * 128],
                                          rhs=v_tm[0:64, j * 128:(j + 1) * 128], start=True, stop=True), ["kend_tm", "v_tm"], [("ps", 4 + j // 4)])
            V(lambda e: e.tensor_tensor(out=St, in0=St, in1=eGE[:, :, sc:sc + 1].to_broadcast([128, 8, 128]), op=ALU.mult), ["St", "eGE"], ["St"])
            V(lambda e: e.tensor_tensor(out=flat(St).rearrange("p (a b) -> p a b", b=512), in0=flat(St).rearrange("p (a b) -> p a b", b=512),
                                        in1=self.psum[:, 4:6, :], op=ALU.add), ["St", ("ps", 4), ("ps", 5)], ["St"])
            S(lambda e: e.copy(out=Sb, in_=St), ["St"], ["Sb"])
            if need_out and fin:
                for j in range(8):
                    S(lambda e, j=j: e.activation(out=opv[0:64, j * 128:(j + 1) * 128], in_=och[0:64, j * 128:(j + 1) * 128], func=AF.Square,
                                                  accum_out=sm[0:64, 8 + j:9 + j]), ["och"], ["opv", "sm8"])
                S(lambda e: e.activation(out=sm[0:64, 16:24], in_=sm[0:64, 8:16], func=AF.Sqrt, scale=1.0 / 128, bias=EPS), ["sm8"], ["sm16"])
                V(lambda e: e.reciprocal(out=sm[0:64, 16:24], in_=sm[0:64, 16:24]), ["sm16"], ["sm16"])
                V(lambda e: e.tensor_tensor(out=och[0:64].rearrange("p (j q) -> p j q", q=128), in0=och[0:64].rearrange("p (j q) -> p j q", q=128),
                                            in1=sm[0:64, 16:24].unsqueeze(2).to_broadcast([64, 8, 128]), op=ALU.mult), ["och", "sm16"], ["och"])
                V(lambda e: e.tensor_tensor(out=och[0:64], in0=och[0:64], in1=nwr[0:64], op=ALU.mult), ["och", "nwr"], ["och"])
                for n2 in range(2):
                    for k in range(8):
                        T(lambda e, n2=n2, k=k: e.matmul(self.psum[0:64, n2, :], lhsT=hT[:, k, cols], rhs=w[:, k, 3072 + n2 * 512:3072 + (n2 + 1) * 512],
                                                         start=(k == 0), stop=(k == 7)), ["hT", "w"], [("ps", n2)])
                S(lambda e: e.activation(out=gs[0:64].rearrange("p (a b) -> p a b", b=512), in_=self.psum[0:64, 0:2, :], func=AF.Silu), [("ps", 0), ("ps", 1)], ["gs"])
                V(lambda e: e.tensor_tensor(out=onb[0:64], in0=och[0:64], in1=gs[0:64], op=ALU.mult), ["och", "gs"], ["onb"])
                for k in range(8):
                    T(lambda e, k=k: e.transpose(pbf(7)[:, k * 64:(k + 1) * 64], onb[0:64, k * 128:(k + 1) * 128], self.identb[0:64, 0:64]),
                      ["onb", "identb"], [("ps", 7)])
                S(lambda e: e.copy(out=flat(onT), in_=pbf(7)[:, 0:512]), [("ps", 7)], ["onT"])
                for n2 in range(2):
                    for k in range(8):
                        T(lambda e, n2=n2, k=k: e.matmul(self.psum[0:64, 2 + n2, :], lhsT=onT[:, k, :], rhs=w[:, k, 4096 + n2 * 512:4096 + (n2 + 1) * 512],
                                                         start=(k == 0), stop=(k == 7)), ["onT", "w"], [("ps", 2 + n2)])
                V(lambda e: e.tensor_tensor(out=gs[0:64].rearrange("p (a b) -> p a b", b=512), in0=self.psum[0:64, 2:4, :],
                                            in1=Grow[0:64].rearrange("p (a b) -> p a b", b=512), op=ALU.mult), [("ps", 2), ("ps", 3), "Grow"], ["gs"])
                P.dma("sync", lambda e: e.dma_start(out=opv[0:64], in_=src_fn(i)[sc * 64:sc * 64 + 64, :]), writes=["opv"])
                V(lambda e: e.tensor_tensor(out=gs[0:64], in0=gs[0:64], in1=opv[0:64], op=ALU.add), ["gs", "opv"], ["gs"])
                P.dma("scalar", lambda e: e.dma_start(out=dst_fn(i)[sc * 64:sc * 64 + 64, :], in_=gs[0:64]), reads=["gs"], writes=[self.key("x3")], sem="xo_st")
        for sc in subs:
            sub_body(sc)
    for i in tiles:
        tile_body(i)
    P.barrier()
    A.off = mark


def mixer1(self, src_fn, dst_fn):
    self._mix_helpers()
    self.hg_pass(0, src_fn, dst_fn)
    self.hg_pass(1, src_fn, dst_fn)


KB.hg_inputs = hg_inputs
KB.hg_pass = hg_pass
KB.mixer1 = mixer1
```

```python
import contextlib
import numpy as np
import concourse.bass as bass
import concourse.mybir as mybir
from concourse.alu_op_type import AluOpType as ALU
from concourse.bass_utils import run_bass_kernel_spmd

AF = mybir.ActivationFunctionType
AX = mybir.AxisListType
F32 = mybir.dt.float32
BF16 = mybir.dt.bfloat16
U32 = mybir.dt.uint32
I32 = mybir.dt.int32
U16 = mybir.dt.uint16

ENGS = ("sync", "scalar", "vector", "gpsimd", "tensor")
SAME_ENGINE_WAITS = True


class Prog:
    def __init__(self, nc):
        self.nc = nc
        self.stack = contextlib.ExitStack()
        self.q = {e: [] for e in ENGS}
        self.cnt = {}
        self.waited = {e: {} for e in ENGS}
        self.last_w = {}
        self.reads = {}
        self.semkeys = []
        self.sems = {}
        self.nops = 0
        self.shared = set()

    def sb(self, name, shape, dt):
        return self.stack.enter_context(self.nc.sbuf_tensor(name, list(shape), dt))

    def ps(self, name, shape, dt):
        return self.stack.enter_context(self.nc.psum_tensor(name, list(shape), dt))

    def dram(self, name, shape, dt, kind="Internal"):
        return self.nc.dram_tensor(name, list(shape), dt, kind=kind)

    def _semkey(self, k):
        if k not in self.cnt:
            self.cnt[k] = 0
            self.semkeys.append(k)
        return k

    def _deps(self, eng, reads, writes):
        deps = {}
        def add(ev, war=False):
            if ev is None:
                return
            k, v = ev
            if k in self.shared:
                v = self.cnt[k]
            if k == ("E", eng):
                if eng == "tensor" or war or not SAME_ENGINE_WAITS:
                    return
            if deps.get(k, 0) < v:
                deps[k] = v
        for r in reads:
            add(self.last_w.get(r))
        for w in writes:
            add(self.last_w.get(w))
            for ev in self.reads.get(w, ()):
                add(ev, war=True)
        out = []
        wd = self.waited[eng]
        for k, v in deps.items():
            if wd.get(k, 0) >= v:
                continue
            wd[k] = v
            out.append((k, v))
        return out

    def _record(self, ev, reads, writes):
        for r in reads:
            self.reads.setdefault(r, []).append(ev)
        for w in writes:
            self.last_w[w] = ev
            self.reads[w] = []

    def op(self, eng, fn, reads=(), writes=()):
        reads = tuple(reads); writes = tuple(writes)
        waits = self._deps(eng, reads, writes)
        k = self._semkey(("E", eng))
        self.cnt[k] += 1
        v = self.cnt[k]
        self.q[eng].append((waits, fn, k, 1))
        self._record((k, v), reads, writes)
        self.nops += 1

    def dma(self, eng, fn, reads=(), writes=(), sem=None):
        reads = tuple(reads); writes = tuple(writes)
        waits = self._deps(eng, reads, writes)
        k = self._semkey(("D", sem if sem is not None else writes[0]))
        if sem == "misc":
            self.shared.add(k)
        self.cnt[k] += 16
        v = self.cnt[k]
        self.q[eng].append((waits, fn, k, 16))
        self._record((k, v), reads, writes)
        self.nops += 1

    def wait_all(self, eng, keys):
        waits = self._deps(eng, tuple(keys), ())
        self.q[eng].append((waits, None, None, 0))

    def build(self):
        nc = self.nc
        for i, k in enumerate(self.semkeys):
            self.sems[k] = self.stack.enter_context(nc.semaphore("s%d" % i))
        block = self.stack.enter_context(nc.Block())
        sems = self.sems

        def run(e, items):
            for waits, fn, k, inc in items:
                for wk, wv in waits:
                    e.wait_ge(sems[wk], wv)
                if fn is not None:
                    fn(e).then_inc(sems[k], inc)

        @block.sync
        def _(e):
            run(e, self.q["sync"])

        @block.scalar
        def _(e):
            run(e, self.q["scalar"])

        @block.vector
        def _(e):
            run(e, self.q["vector"])

        @block.gpsimd
        def _(e):
            run(e, self.q["gpsimd"])

        @block.tensor
        def _(e):
            run(e, self.q["tensor"])

    def close(self):
        self.stack.close()


def _barrier(P):
    allk = [(k, P.cnt[k]) for k in P.semkeys if P.cnt[k] > 0]
    for e in ENGS:
        waits = []
        for k, v in allk:
            if P.waited[e].get(k, 0) < v:
                P.waited[e][k] = v
                waits.append((k, v))
        if waits:
            P.q[e].append((waits, None, None, 0))


Prog.barrier = _barrier


class Arena:
    def __init__(self, P, nbytes):
        self.words = nbytes // 4
        self.t = P.sb("arena", [128, self.words], F32)
        self.off = 0
        self.n = 0

    def alloc(self, shape, dt=F32):
        n = 1
        for s in shape:
            n *= s
        esz = 2 if dt in (BF16, U16) else 4
        words = (n * esz + 3) // 4
        words = (words + 7) // 8 * 8
        assert self.off + words <= self.words, ("arena overflow", self.off, words)
        ap = self.t[:, self.off:self.off + words]
        self.off += words
        if dt != F32:
            ap = ap.bitcast(dt)
        ap = ap[:, 0:n]
        if len(shape) > 1:
            names = "abcd"[:len(shape)]
            pat = "p (%s) -> p %s" % (" ".join(names), " ".join(names))
            ap = ap.rearrange(pat, **{names[i]: shape[i] for i in range(len(shape))})
        return ap


D = 1024
NT = 34
EPS = 1e-6
NEG = -1.0e30


class KB:
    def __init__(self, dbg=None):
        self.dbg = dbg or {}
        nc = self.nc = bass.Bass("TRN2", target_bir_lowering=False)
        P = self.P = Prog(nc)
        self.A = Arena(P, 172000)
        self.psum = P.ps("psum", [128, 8, 512], F32)
        di = lambda n, s, dt=F32: nc.dram_tensor(n, list(s), dt, kind="ExternalInput")
        self.x = di("x", [4096, D]); self.ctx = di("ctx", [256, D])
        self.condT = di("condT", [128, 16])
        self.consts = di("consts", [128, 1024])
        self.ada_w = di("ada_w", [2, D, 6 * D]); self.ada_bT = di("ada_bT", [2, 128, 48])
        self.nmixT = di("nmixT", [2, 128, 8]); self.nffnT = di("nffnT", [2, 128, 8])
        self.nf_row = di("nf_row", [128, D])
        self.peer_wq = di("peer_wq", [2, D, 2048]); self.keysT = di("keysT", [2, 128, 2048])
        self.peer_u = [di("peer_u%d" % l, [16384, D]) for l in range(2)]; self.peer_v = [di("peer_v%d" % l, [16384, D]) for l in range(2)]
        self.out = nc.dram_tensor("out", [4096, D], F32, kind="ExternalOutput")
        self.x1 = nc.dram_tensor("x1", [NT * 128, D], F32, kind=self.dbg.get("x1", "Internal"))
        self.x2 = nc.dram_tensor("x2", [NT * 128, D], F32, kind=self.dbg.get("x2", "Internal"))
        self.x3 = nc.dram_tensor("x3", [NT * 128, D], F32, kind=self.dbg.get("x3", "Internal"))
        self.uid = 0
        self.tab_bf = {}
        for l in range(2):
            self.tab_bf[("u", l)] = nc.dram_tensor("ub%d" % l, [16384, D], BF16, kind="Internal")
            self.tab_bf[("v", l)] = nc.dram_tensor("vb%d" % l, [16384, D], BF16, kind="Internal")

    def key(self, s):
        self.uid += 1
        return "%s#%d" % (s, self.uid)

    def bank(self, b, n=512, off=0):
        return self.psum[:, b, off:off + n]

    def src_tile(self, i):
        if i < 2:
            return self.ctx[i * 128:(i + 1) * 128, :]
        return self.x[(i - 2) * 128:(i - 1) * 128, :]

    def setup_consts(self):
        P, A = self.P, self.A
        self.cst = A.alloc([1024]);
        P.dma("sync", lambda e: e.dma_start(out=self.cst, in_=self.consts.ap()), writes=["cst"], sem="misc")
        self.ident = self.cst[:, 0:128]; self.ones = self.cst[:, 128:256]
        self.cond = A.alloc([16])
        P.dma("sync", lambda e: e.dma_start(out=self.cond, in_=self.condT.ap()), writes=["cond"], sem="misc")
        self.condS = A.alloc([8, 2])
        P.op("scalar", lambda e: e.activation(out=self.condS, in_=self.cond.rearrange("p (a b) -> p a b", b=2), func=AF.Silu),
             reads=["cond"], writes=["condS"])
        self.modsFM = [A.alloc([48, 2]) for _ in range(2)]
        self.identb = A.alloc([128], BF16)
        P.op("vector", lambda e: e.tensor_copy(out=self.identb, in_=self.ident), reads=["cst"], writes=["identb"])
        P.barrier()

    def mods_phase(self, l):
        P, A = self.P, self.A
        mark = A.off
        ws = [A.alloc([8, 768]) for _ in range(2)]
        abT = A.alloc([48])
        P.dma("sync", lambda e: e.dma_start(out=abT, in_=self.ada_bT[l]), writes=["abT"], sem="misc")
        pm = self.psum[:, 0, 0:96]
        awv = self.ada_w[l].rearrange("(k p) n -> p k n", p=128)
        for g in range(8):
            s = g % 2
            P.dma("sync" if s == 0 else "scalar", lambda e, g=g, s=s: e.dma_start(out=ws[s], in_=awv[:, :, g * 768:(g + 1) * 768]),
                  writes=[("ws", s)])
            for jj in range(6):
                j = g * 6 + jj
                for k in range(8):
                    P.op("tensor", lambda e, k=k, s=s, j=j, jj=jj: e.matmul(pm[:, 2 * j:2 * j + 2], lhsT=ws[s][:, k, jj * 128:(jj + 1) * 128],
                                                                           rhs=self.condS[:, k, :], start=(k == 0), stop=(k == 7)),
                         reads=[("ws", s), "condS"], writes=[("ps", 0)])
        m = self.modsFM[l]
        P.op("vector", lambda e: e.tensor_tensor(out=m, in0=pm.rearrange("p (a b) -> p a b", b=2),
                                                 in1=abT.unsqueeze(2).to_broadcast([128, 48, 2]), op=ALU.add),
             reads=[("ps", 0), "abT"], writes=[("mods", l)])
        P.barrier()
        A.off = mark

    def make_row(self, dst, col, dstkey, colkeys, banks=(6, 7)):
        P = self.P
        dg = self.diag
        for jj in range(8):
            P.op("vector", lambda e, jj=jj: e.tensor_scalar(out=dg, in0=self.ident, scalar1=col[:, jj:jj + 1], scalar2=None, op0=ALU.mult),
                 reads=["cst"] + list(colkeys), writes=["diag"])
            b = banks[jj // 4]
            P.op("tensor", lambda e, jj=jj, b=b: e.matmul(self.bank(b, 128, (jj % 4) * 128), lhsT=self.ones, rhs=dg, start=True, stop=True),
                 reads=["diag", "cst"], writes=[("ps", b)])
        for h in range(2):
            P.op("scalar", lambda e, h=h: e.copy(out=dst[:, h * 512:(h + 1) * 512], in_=self.bank(banks[h])),
                 reads=[("ps", banks[h])], writes=[dstkey])

    def conv_tables(self):
        P, A = self.P, self.A
        mark = A.off
        RB = 4
        ib = [A.alloc([RB, D]) for _ in range(2)]
        ob = [A.alloc([RB, D], BF16) for _ in range(3)]
        n = 0
        for l in range(2):
            for nm, src in (("u", self.peer_u[l]), ("v", self.peer_v[l])):
                sv = src.ap().rearrange("(p r) d -> p r d", p=128)
                dv = self.tab_bf[(nm, l)].ap().rearrange("(p r) d -> p r d", p=128)
                for c in range(128 // RB):
                    si = n % 2; so = n % 3; n += 1
                    P.dma("sync", lambda e, sv=sv, c=c, si=si: e.dma_start(out=ib[si], in_=sv[:, c * RB:(c + 1) * RB, :]), writes=[("tib", si)])
                    eng = ("vector", "gpsimd", "scalar")[so]
                    if eng == "scalar":
                        P.op(eng, lambda e, si=si, so=so: e.copy(out=ob[so], in_=ib[si]), reads=[("tib", si)], writes=[("tob", so)])
                    else:
                        P.op(eng, lambda e, si=si, so=so: e.tensor_copy(out=ob[so], in_=ib[si]), reads=[("tib", si)], writes=[("tob", so)])
                    P.dma("scalar" if so != 2 else "gpsimd", lambda e, dv=dv, c=c, so=so: e.dma_start(out=dv[:, c * RB:(c + 1) * RB, :], in_=ob[so]),
                          reads=[("tob", so)], writes=[self.key("tbf")], sem=("tob_st", so))
        P.barrier()
        A.off = mark

    def peer_phase(self, l, src_fn, dst, tiles, final_norm=False):
        P, A = self.P, self.A
        mark = A.off
        V = lambda fn, r, w: P.op("vector", fn, r, w)
        S = lambda fn, r, w: P.op("scalar", fn, r, w)
        T = lambda fn, r, w: P.op("tensor", fn, r, w)
        m = self.modsFM[l]
        mk = [("mods", l)]
        wq = A.alloc([8, 2048])
        for k in range(8):
            P.dma("sync" if k % 2 == 0 else "scalar",
                  lambda e, k=k: e.dma_start(out=wq[:, k, :], in_=self.peer_wq[l, k * 128:(k + 1) * 128, :]), writes=["wq"], sem="misc")
        kT = A.alloc([16, 128])
        P.dma("sync", lambda e: e.dma_start(out=kT, in_=self.keysT[l].rearrange("p (a b) -> p a b", b=128)), writes=["kT"], sem="misc")
        nf = A.alloc([8])
        P.dma("sync", lambda e: e.dma_start(out=nf, in_=self.nffnT[l]), writes=["nf"], sem="misc")
        Arow = A.alloc([D]); Brow = A.alloc([D]); Grow = A.alloc([D])
        acol = A.alloc([8])
        xt = A.alloc([D]); h2 = A.alloc([D]); hT = A.alloc([8, 128]); xn = h2
        qT = A.alloc([16, 128]); sub = A.alloc([16, 128]); sub2 = sub
        cand = sub.rearrange("p a b -> p (a b)").rearrange("p (a b) -> p a b", b=256)
        candi = qT.rearrange("p a b -> p (a b)").rearrange("p (a b) -> p a b", b=256)
        st = A.alloc([16, 16]); it = A.alloc([16, 16], U32); itf = A.alloc([16, 16])
        bs = A.alloc([8, 16]); idxf = A.alloc([128]); idx = A.alloc([128], U32)
        gate = A.alloc([8, 16]); sm = A.alloc([16]); act = A.alloc([128]); araw = A.alloc([128])
        jbig = A.alloc([2 * D]); junk = jbig[:, 0:D]; acc1 = jbig[:, D:2 * D]; acc = A.alloc([D])
        cand2 = jbig.rearrange("p (a b) -> p a b", b=256)
        NS = 6
        gs = [A.alloc([D], BF16) for _ in range(NS)]
        gcount = [0]
        if final_norm:
            nfr = A.alloc([D])
            P.dma("sync", lambda e: e.dma_start(out=nfr, in_=self.nf_row.ap()), writes=["nfr"], sem="misc")
        utab = self.tab_bf[("u", l)].ap(); vtab = self.tab_bf[("v", l)].ap()
        cur_c = [None]

        def set_cond(c):
            if cur_c[0] == c:
                return
            cur_c[0] = c
            V(lambda e: e.scalar_tensor_tensor(out=acol, in0=m[:, 32:40, c], scalar=1.0, in1=nf, op0=ALU.add, op1=ALU.mult),
              mk + ["nf"], ["acol"])
            self.make_row(Arow, acol, "Arow", ["acol"])
            self.make_row(Brow, m[:, 24:32, c], "Brow", mk)
            self.make_row(Grow, m[:, 40:48, c], "Grow", mk)

        for i in tiles:
            set_cond(0 if i >= 2 else 1)
            P.dma("sync", lambda e, i=i: e.dma_start(out=xt, in_=src_fn(i)), writes=["xt"])
            S(lambda e: e.activation(out=junk, in_=xt, func=AF.Square, accum_out=sm[:, 0:1]), ["xt"], ["junk", "sm0"])
            S(lambda e: e.activation(out=sm[:, 1:2], in_=sm[:, 0:1], func=AF.Sqrt, scale=1.0 / D, bias=EPS), ["sm0"], ["sm1"])
            V(lambda e: e.reciprocal(out=sm[:, 2:3], in_=sm[:, 1:2]), ["sm1"], ["sm2"])
            V(lambda e: e.tensor_scalar(out=xn, in0=xt, scalar1=sm[:, 2:3], scalar2=None, op0=ALU.mult), ["xt", "sm2"], ["h2"])
            V(lambda e: e.tensor_tensor(out=xn, in0=xn, in1=Arow, op=ALU.mult), ["h2", "Arow"], ["h2"])
            V(lambda e: e.tensor_tensor(out=h2, in0=xn, in1=Brow, op=ALU.add), ["h2", "Brow"], ["h2"])
            for k in range(8):
                T(lambda e, k=k: e.transpose(self.bank(k // 4, 128, (k % 4) * 128), h2[:, k * 128:(k + 1) * 128], self.ident),
                  ["h2", "cst"], [("ps", k // 4)])
            S(lambda e: e.copy(out=hT.rearrange("p a b -> p (a b)").rearrange("p (a b) -> p a b", b=512), in_=self.psum[:, 0:2, :]),
              [("ps", 0), ("ps", 1)], ["hT"])
            for j in range(16):
                for k in range(8):
                    T(lambda e, j=j, k=k: e.matmul(self.bank(2 + j // 4, 128, (j % 4) * 128), lhsT=wq[:, k, j * 128:(j + 1) * 128],
                                                  rhs=hT[:, k, :], start=(k == 0), stop=(k == 7)),
                      ["wq", "hT"], [("ps", 2 + j // 4)])
            S(lambda e: e.copy(out=qT.rearrange("p a b -> p (a b)").rearrange("p (a b) -> p a b", b=512), in_=self.psum[:, 2:6, :]),
              [("ps", b) for b in (2, 3, 4, 5)], ["qT"])
            sb_banks = (0, 1, 6, 7)
            for j in range(16):
                b = sb_banks[j // 4]
                T(lambda e, j=j, b=b: e.matmul(self.bank(b, 128, (j % 4) * 128), lhsT=qT[:, j, :], rhs=kT[:, j, :], start=True, stop=True),
                  ["qT", "kT"], [("ps", b)])
            for q4 in range(4):
                b = sb_banks[q4]
                S(lambda e, q4=q4, b=b: e.copy(out=sub[:, q4 * 4:(q4 + 1) * 4, :].rearrange("p a b -> p (a b)"), in_=self.bank(b)),
                  [("ps", b)], ["sub"])
            for j in range(16):
                V(lambda e, j=j: e.max(out=st[:, j, 0:8], in_=sub[:, j, :]), ["sub"], [("st0", j)])
            for j in range(16):
                V(lambda e, j=j: e.max_index(out=it[:, j, 0:8], in_max=st[:, j, 0:8], in_values=sub[:, j, :]), ["sub", ("st0", j)], [("it0", j)])
            for j in range(16):
                V(lambda e, j=j: e.match_replace(out=sub[:, j, :], in_to_replace=st[:, j, 0:8], in_values=sub[:, j, :], imm_value=NEG),
                  ["sub", ("st0", j), ("it0", j)], [("sub2", j)])
            for j in range(16):
                V(lambda e, j=j: e.max(out=st[:, j, 8:16], in_=sub[:, j, :]), [("sub2", j)], [("st1", j)])
            for j in range(16):
                V(lambda e, j=j: e.max_index(out=it[:, j, 8:16], in_max=st[:, j, 8:16], in_values=sub[:, j, :]), [("sub2", j), ("st1", j)], [("it1", j)])
            V(lambda e: e.tensor_copy(out=itf, in_=it), [("it0", j) for j in range(16)] + [("it1", j) for j in range(16)], ["itf"])
            st4 = st.rearrange("p (h a) k -> p h a k", a=2)
            if4 = itf.rearrange("p (h a) k -> p h a k", a=2)
            V(lambda e: e.tensor_scalar(out=if4[:, :, 0, :], in0=if4[:, :, 0, :], scalar1=128.0, scalar2=None, op0=ALU.mult), ["itf"], ["itf"])
            c4 = cand.rearrange("p h (a b) -> p h a b", b=16)
            ci4 = candi.rearrange("p h (a b) -> p h a b", b=16)
            V(lambda e: e.tensor_tensor(out=c4, in0=st4[:, :, 0, :].unsqueeze(3).to_broadcast([128, 8, 16, 16]),
                                        in1=st4[:, :, 1, :].unsqueeze(2).to_broadcast([128, 8, 16, 16]), op=ALU.add), [("st0", j) for j in range(16)] + [("st1", j) for j in range(16)] + [("it0", j) for j in range(16)] + [("it1", j) for j in range(16)] + [("sub2", j) for j in range(16)], ["sub"])
            V(lambda e: e.tensor_tensor(out=ci4, in0=if4[:, :, 0, :].unsqueeze(3).to_broadcast([128, 8, 16, 16]),
                                        in1=if4[:, :, 1, :].unsqueeze(2).to_broadcast([128, 8, 16, 16]), op=ALU.add), ["itf"], ["qT"])
            for h in range(8):
                V(lambda e, h=h: e.max(out=bs[:, h, 0:8], in_=cand[:, h, :]), ["sub"], [("bs0", h)])
            for h in range(8):
                V(lambda e, h=h: e.match_replace(out=cand2[:, h, :], in_to_replace=bs[:, h, 0:8], in_values=cand[:, h, :], imm_value=NEG),
                  ["sub", ("bs0", h), "junk", "acc1"], [("cand2", h)])
            for h in range(8):
                V(lambda e, h=h: e.max(out=bs[:, h, 8:16], in_=cand2[:, h, :]), [("cand2", h)], [("bs1", h)])
            for h in range(8):
                for k in range(16):
                    V(lambda e, h=h, k=k: e.scalar_tensor_tensor(out=junk[:, 0:256], in0=cand[:, h, :], scalar=bs[:, h, k:k + 1],
                                                               in1=candi[:, h, :], op0=ALU.is_equal, op1=ALU.mult,
                                                               accum_out=idxf[:, h * 16 + k:h * 16 + k + 1]),
                      ["sub", ("bs0", h), ("bs1", h), "qT"], [("idxf1", h * 16 + k)])
            V(lambda e: e.tensor_scalar(out=idxf, in0=idxf, scalar1=16383.0, scalar2=0.0, op0=ALU.min, op1=ALU.max), [("idxf1", q) for q in range(128)], ["idxf"])
            V(lambda e: e.tensor_copy(out=idx, in_=idxf), ["idxf"], ["idx"])
            V(lambda e: e.tensor_tensor(out=gate, in0=bs, in1=bs[:, :, 0:1].to_broadcast([128, 8, 16]), op=ALU.subtract), [("bs0", h) for h in range(8)] + [("bs1", h) for h in range(8)], ["gate"])
            S(lambda e: e.activation(out=gate, in_=gate, func=AF.Exp), ["gate"], ["gate"])
            V(lambda e: e.tensor_reduce(out=sm[:, 4:12], in_=gate, axis=AX.X, op=ALU.add), ["gate"], ["sm4"])
            V(lambda e: e.reciprocal(out=sm[:, 4:12], in_=sm[:, 4:12]), ["sm4"], ["sm4"])
            V(lambda e: e.tensor_tensor(out=gate, in0=gate, in1=sm[:, 4:12].unsqueeze(2).to_broadcast([128, 8, 16]), op=ALU.mult),
              ["gate", "sm4"], ["gate"])
            for k in range(128):
                s = gcount[0] % NS; gcount[0] += 1
                P.dma("gpsimd", lambda e, k=k, s=s: e.indirect_dma_start(
                    out=gs[s], out_offset=None, in_=utab,
                    in_offset=bass.IndirectOffsetOnAxis(ap=idx[:, k:k + 1], axis=0)), reads=["idx"], writes=[("gs", s)],
                    sem=("gs", l, s, gcount[0] // (NS * 250)))
                V(lambda e, k=k, s=s: e.scalar_tensor_tensor(out=junk, in0=gs[s], scalar=1.0, in1=h2, op0=ALU.mult, op1=ALU.mult,
                                                           accum_out=araw[:, k:k + 1]), [("gs", s), "h2"], [("araw", k)])
            S(lambda e: e.activation(out=act, in_=araw, func=AF.Gelu), [("araw", k) for k in range(128)], ["act"])
            V(lambda e: e.tensor_tensor(out=act, in0=act, in1=gate.rearrange("p a b -> p (a b)"), op=ALU.mult), ["act", "gate"], ["act"])
            for k in range(128):
                s = gcount[0] % NS; gcount[0] += 1
                P.dma("gpsimd", lambda e, k=k, s=s: e.indirect_dma_start(
                    out=gs[s], out_offset=None, in_=vtab,
                    in_offset=bass.IndirectOffsetOnAxis(ap=idx[:, k:k + 1], axis=0)), reads=["idx"], writes=[("gs", s)],
                    sem=("gs", l, s, gcount[0] // (NS * 250)))
                ab = acc if k % 2 == 0 else acc1
                akey = "junk" if k % 2 == 0 else "acc1"
                if k < 2:
                    V(lambda e, k=k, s=s, ab=ab: e.tensor_scalar(out=ab, in0=gs[s], scalar1=act[:, k:k + 1], scalar2=None, op0=ALU.mult),
                      [("gs", s), "act"], [akey])
                else:
                    V(lambda e, k=k, s=s, ab=ab: e.scalar_tensor_tensor(out=ab, in0=gs[s], scalar=act[:, k:k + 1], in1=ab,
                                                                      op0=ALU.mult, op1=ALU.add), [("gs", s), "act", akey], [akey])
            V(lambda e: e.tensor_tensor(out=acc, in0=acc, in1=acc1, op=ALU.add), ["junk", "acc1"], ["junk"])
            V(lambda e: e.tensor_tensor(out=acc, in0=acc, in1=Grow, op=ALU.mult), ["junk", "Grow"], ["junk"])
            V(lambda e: e.tensor_tensor(out=acc, in0=acc, in1=xt, op=ALU.add), ["junk", "xt"], ["junk"])
            if final_norm:
                S(lambda e: e.activation(out=h2, in_=acc, func=AF.Square, accum_out=sm[:, 12:13]), ["junk"], ["h2", "sm12"])
                S(lambda e: e.activation(out=sm[:, 13:14], in_=sm[:, 12:13], func=AF.Sqrt, scale=1.0 / D, bias=EPS), ["sm12"], ["sm13"])
                V(lambda e: e.reciprocal(out=sm[:, 14:15], in_=sm[:, 13:14]), ["sm13"], ["sm14"])
                V(lambda e: e.scalar_tensor_tensor(out=acc, in0=acc, scalar=sm[:, 14:15], in1=nfr, op0=ALU.mult, op1=ALU.mult),
                  ["junk", "sm14", "nfr"], ["junk"])
            P.dma("scalar", lambda e, i=i: e.dma_start(out=dst(i), in_=acc), reads=["junk"], writes=[self.key("dst")], sem="acc_st")
        P.barrier()
        A.off = mark


def fm(v, nch):
    return np.ascontiguousarray(np.asarray(v, np.float32).reshape(nch, 128).T)


def make_consts():
    c = np.zeros((128, 1024), np.float32)
    k = np.arange(128)
    c[:, 0:128] = np.eye(128, dtype=np.float32)
    c[:, 128:256] = 1.0
    c[:, 256:384] = (k[:, None] <= k[None, :])
    c[:, 384:512] = (k[:, None] > k[None, :])
    c[:, 512:640] = (k[:, None] >= k[None, :])
    c[:, 640:768] = (k[:, None] < k[None, :])
    return c


def prep_shared(inp):
    g = {}
    f32 = lambda a: np.ascontiguousarray(np.asarray(a, np.float32))
    g["consts"] = make_consts()
    g["ada_w"] = f32(inp["ada_w"])
    g["ada_bT"] = np.stack([fm(inp["ada_b"][l], 48) for l in range(2)])
    g["nmixT"] = np.stack([fm(inp["norm_mix_w"][l], 8) for l in range(2)])
    g["nffnT"] = np.stack([fm(inp["norm_ffn_w"][l], 8) for l in range(2)])
    g["nf_row"] = np.ascontiguousarray(np.broadcast_to(f32(inp["norm_f_w"])[None, :], (128, D)))
    g["ev_w_in"] = f32(inp["ev_w_in"][0]); g["ev_w_out"] = f32(inp["ev_w_out"][0])
    cwf = lambda w, nj: np.ascontiguousarray(f32(w).reshape(3, nj, 128).transpose(2, 1, 0).reshape(128, nj * 3))
    g["ssd_cwT"] = cwf(inp["ssd_conv_w"][0], 16); g["sc_cwT"] = cwf(inp["sc_conv_w"][0], 8)
    g["ssd_cbT"] = fm(inp["ssd_conv_b"][0], 16)
    brow = lambda v: np.ascontiguousarray(np.broadcast_to(f32(v).reshape(1, -1), (128, f32(v).size)))
    g["dtb_row"] = brow(inp["ssd_dt_bias"][0]); g["alog_row"] = brow(inp["ssd_a_log"][0])
    g["dsk_row"] = brow(inp["ssd_d"][0]); g["ssdnw_row"] = brow(inp["ssd_norm_w"][0])
    g["od_w_in"] = f32(inp["od_w_in"][0]); g["od_w_out"] = f32(inp["od_w_out"][0])
    lg = f32(inp["hg_lb_logits"])
    g["lblT"] = np.ascontiguousarray(lg.reshape(2, 2, 8, 128).transpose(3, 0, 1, 2).reshape(128, 32))
    g["hgnw_row"] = brow(inp["hg_norm_w"][0])
    smk = np.ones((128, 8, 2, 64), np.float32); smk[:, :, :, 0] = 0.0
    g["scanmask"] = smk.reshape(128, 1024)
    g["peer_wq"] = f32(inp["peer_wq"])
    pk = f32(inp["peer_keys"])
    g["keysT"] = np.ascontiguousarray(pk.transpose(0, 4, 1, 2, 3).reshape(2, 128, 2048))
    for l in range(2):
        g["peer_u%d" % l] = f32(inp["peer_u"][l]); g["peer_v%d" % l] = f32(inp["peer_v"][l])
    return g


def prep_core(inp, b):
    m = {}
    m["x"] = np.ascontiguousarray(np.asarray(inp["x"][b], np.float32))
    m["ctx"] = np.ascontiguousarray(np.asarray(inp["ctx"][b], np.float32))
    ct = np.zeros((128, 8, 2), np.float32)
    ct[:, :, 0] = fm(inp["c"][b], 8); ct[:, :, 1] = fm(inp["c_ctx"], 8)
    m["condT"] = ct.reshape(128, 16)
    return m


def build_full():
    kb = KB()
    kb.ssd_inputs()
    kb.hg_inputs()
    kb.setup_consts()
    kb.diag = kb.A.alloc([128])
    tiles = list(range(NT))
    xtile = lambda t: (lambda i: t[i * 128:(i + 1) * 128, :])
    kb.conv_tables()
    kb.mods_phase(0)
    src0 = kb.src_tile
    if hasattr(kb, "mixer0"):
        kb.mixer0(src0, xtile(kb.x1))
        src0 = xtile(kb.x1)
    kb.peer_phase(0, src0, xtile(kb.x2), tiles)
    kb.mods_phase(1)
    src1 = xtile(kb.x2)
    if hasattr(kb, "mixer1"):
        kb.mixer1(src1, xtile(kb.x3))
        src1 = xtile(kb.x3)
    kb.peer_phase(1, src1, lambda i: kb.out[(i - 2) * 128:(i - 1) * 128, :], tiles[2:], final_norm=True)
    kb.P.wait_all("sync", [])
    kb.P.build()
    return kb


def kernel(**inputs):
    inp = {k: np.asarray(v) for k, v in inputs.items()}
    kb = build_full()
    names = set()
    for a in kb.nc.allocations:
        if getattr(a, "kind", None) == "ExternalInput" and a.memorylocations:
            names.add(a.memorylocations[0].name)
    g = prep_shared(inp)
    in_maps = []
    for b in range(8):
        m = prep_core(inp, b)
        m.update(g)
        in_maps.append({k: v for k, v in m.items() if k in names})
    res = run_bass_kernel_spmd(kb.nc, in_maps, core_ids=list(range(8)))
    out = np.stack([np.asarray(r["out"], np.float32) for r in res.results], axis=0)
    return out


def _mix_helpers(self):
    P = self.P
    self.V = lambda fn, r, w: P.op("vector", fn, r, w)
    self.S = lambda fn, r, w: P.op("scalar", fn, r, w)
    self.T = lambda fn, r, w: P.op("tensor", fn, r, w)
    self.G = lambda fn, r, w: P.op("gpsimd", fn, r, w)


def load_w_bf16(self, dst, src, col0, ncols, key, stage):
    P = self.P
    piece = stage[0].shape[-1]
    n = 0
    for k in range(8):
        c = 0
        while c < ncols:
            w = min(piece, ncols - c)
            s = n % 2; n += 1
            P.dma("sync" if s == 0 else "scalar",
                  lambda e, k=k, c=c, w=w, s=s: e.dma_start(out=stage[s][:, 0:w], in_=src[k * 128:(k + 1) * 128, col0 + c:col0 + c + w]),
                  writes=[("stg", s)])
            P.op("gpsimd", lambda e, k=k, c=c, w=w, s=s: e.tensor_copy(out=dst[:, k, c:c + w], in_=stage[s][:, 0:w]),
                 reads=[("stg", s)], writes=[key])
            c += w


def norm_T(self, src_ap, Acol, Bcol, ckeys, dst, dkey, bufs):
    P, V, S, T = self.P, self.V, self.S, self.T
    xt, xnb, sm = bufs
    P.dma("sync", lambda e: e.dma_start(out=xt, in_=src_ap), writes=["n_xt"])
    S(lambda e: e.activation(out=xnb, in_=xt, func=AF.Square, accum_out=sm[:, 0:1]), ["n_xt"], ["n_xnb", "n_sm0"])
    S(lambda e: e.activation(out=sm[:, 1:2], in_=sm[:, 0:1], func=AF.Sqrt, scale=1.0 / D, bias=EPS), ["n_sm0"], ["n_sm1"])
    V(lambda e: e.reciprocal(out=sm[:, 2:3], in_=sm[:, 1:2]), ["n_sm1"], ["n_sm2"])
    V(lambda e: e.tensor_scalar(out=xnb, in0=xt, scalar1=sm[:, 2:3], scalar2=None, op0=ALU.mult), ["n_xt", "n_sm2"], ["n_xnb"])
    pb = self.psum[:, 7, :].bitcast(BF16)
    for k in range(8):
        T(lambda e, k=k: e.transpose(pb[:, k * 128:(k + 1) * 128], xnb[:, k * 128:(k + 1) * 128], self.identb),
          ["n_xnb", "identb"], [("ps", 7)])
    for k in range(8):
        S(lambda e, k=k: e.activation(out=dst[:, k, :], in_=pb[:, k * 128:(k + 1) * 128], func=AF.Identity,
                                      scale=Acol[:, k:k + 1], bias=Bcol[:, k:k + 1]), [("ps", 7)] + list(ckeys), [dkey])


def proj_fm(self, dst, dkey, w, wkey, col0, nj, hT, hkey, W):
    for jj in range(nj):
        b = 6
        for k in range(8):
            self.T(lambda e, jj=jj, k=k: e.matmul(self.bank(b, W), lhsT=w[:, k, col0 + jj * 128:col0 + (jj + 1) * 128], rhs=hT[:, k, 0:W],
                                                  start=(k == 0), stop=(k == 7)), [wkey, hkey], [("ps", b)])
        self.S(lambda e, jj=jj: e.copy(out=dst[:, jj, 0:W], in_=self.bank(b, W)), [("ps", b)], [dkey])


def conv3(self, dst, dkey, src, skey, cw, cwkey, nj, W, rows):
    L = W // rows
    for j in range(nj):
        d3 = dst[:, j, 0:W].rearrange("p (r l) -> p r l", l=L)
        s3 = src[:, j, 0:W].rearrange("p (r l) -> p r l", l=L)
        self.V(lambda e, j=j: e.tensor_scalar(out=dst[:, j, 0:W], in0=src[:, j, 0:W], scalar1=cw[:, j, 1:2], scalar2=None, op0=ALU.mult),
               [skey, cwkey], [dkey])
        self.V(lambda e, j=j, d3=d3, s3=s3: e.scalar_tensor_tensor(out=d3[:, :, 1:L], in0=s3[:, :, 0:L - 1], scalar=cw[:, j, 0:1],
                                                                   in1=d3[:, :, 1:L], op0=ALU.mult, op1=ALU.add), [skey, cwkey, dkey], [dkey])
        self.V(lambda e, j=j, d3=d3, s3=s3: e.scalar_tensor_tensor(out=d3[:, :, 0:L - 1], in0=s3[:, :, 1:L], scalar=cw[:, j, 2:3],
                                                                   in1=d3[:, :, 0:L - 1], op0=ALU.mult, op1=ALU.add), [skey, cwkey, dkey], [dkey])


KB._mix_helpers = _mix_helpers
KB.load_w_bf16 = load_w_bf16
KB.norm_T = norm_T
KB.proj_fm = proj_fm
KB.conv3 = conv3


def ssd_inputs(self):
    nc = self.nc
    di = lambda n, s, dt=F32: nc.dram_tensor(n, list(s), dt, kind="ExternalInput")
    self.ev_w_in = di("ev_w_in", [D, 6176]); self.ev_w_out = di("ev_w_out", [2048, D])
    self.ssd_cwT = di("ssd_cwT", [128, 48]); self.ssd_cbT = di("ssd_cbT", [128, 16]); self.sc_cwT = di("sc_cwT", [128, 24])
    self.dtb_row = di("dtb_row", [128, 32]); self.alog_row = di("alog_row", [128, 32])
    self.dsk_row = di("dsk_row", [128, 16]); self.ssdnw_row = di("ssdnw_row", [128, D])
    self.yscr = nc.dram_tensor("yscr", [NT * 128, D], F32, kind=self.dbg.get("yscr", "Internal"))
    self.xsscr = nc.dram_tensor("xsscr", [NT * 128, D], BF16, kind="Internal")


def mix_cols(self, l, c, nT, Acol, key):
    m = self.modsFM[l]
    self.V(lambda e: e.scalar_tensor_tensor(out=Acol, in0=m[:, 8:16, c], scalar=1.0, in1=nT, op0=ALU.add, op1=ALU.mult),
           [("mods", l), "nT"], [key])
    return m[:, 0:8, c]


def ssd_scan(self, d, src_fn):
    P, A, V, S, T = self.P, self.A, self.V, self.S, self.T
    mark = A.off
    l = 0
    wx = A.alloc([8, 2080], BF16)
    stage = [A.alloc([2080]) for _ in range(2)]
    self.load_w_bf16(wx, self.ev_w_in, 4096, 2080, "wx", stage)
    cw = A.alloc([16, 3]); cb = A.alloc([16]); nT = A.alloc([8])
    dtb = A.alloc([32]); arow = A.alloc([32])
    for dst_, src_, k_ in ((cw, self.ssd_cwT.ap().rearrange("p (a b) -> p a b", b=3), "cw"), (cb, self.ssd_cbT.ap(), "cb"),
                           (nT, self.nmixT[l], "nT"), (dtb, self.dtb_row.ap(), "dtb"), (arow, self.alog_row.ap(), "arow")):
        P.dma("sync", lambda e, dst_=dst_, src_=src_: e.dma_start(out=dst_, in_=src_), writes=[k_], sem="misc")
    S(lambda e: e.activation(out=arow, in_=arow, func=AF.Exp), ["arow"], ["arow"])
    V(lambda e: e.tensor_scalar(out=arow, in0=arow, scalar1=-1.0, scalar2=None, op0=ALU.mult), ["arow"], ["arow"])
    Acol = [A.alloc([8]) for _ in range(2)]
    Bcol = [self.mix_cols(l, c, nT, Acol[c], ("Acol", c)) for c in range(2)]
    xt = A.alloc([D]); xnb = A.alloc([D], BF16); sm = A.alloc([4])
    hTu = A.alloc([8, 256], BF16)
    praw = A.alloc([16, 256]); cv = A.alloc([16, 256]); xbcT = A.alloc([16, 256], BF16)
    dt = A.alloc([2, 16]); dta = A.alloc([2, 16]); e48 = A.alloc([48]); wend = A.alloc([16])
    R = A.alloc([16, 128]); cbm = A.alloc([4, 128]); Eq = A.alloc([4, 128]); attT = A.alloc([16, 128], BF16)
    xs_tm = A.alloc([D], BF16); bm_tm = A.alloc([512], BF16); xw = A.alloc([D], BF16)
    ych = A.alloc([D]); ypv = A.alloc([D]); St = A.alloc([D]); Sb = A.alloc([D], BF16)
    V(lambda e: e.memset(St, 0.0), [], ["St"])
    V(lambda e: e.memset(Sb, 0.0), [], ["Sb"])
    cst = self.cst
    if d == 0:
        Bm = cst[:, 256:384]; Am = cst[:, 384:512]; Mk = cst[:, 256:384]
    else:
        Bm = cst[:, 512:640]; Am = cst[:, 640:768]; Mk = cst[:, 512:640]
    units = [(0, 256, 1)] + [(i, 128, 0) for i in range(2, NT)]
    if d == 1:
        units = [units[0]] + units[1:][::-1]
    pbf = lambda b: self.psum[:, b, :].bitcast(BF16)
    def unit_body(t0, W, c):
        nsub = W // 128
        for sub in range(nsub):
            self.norm_T(src_fn(t0 + sub), Acol[c], Bcol[c], [("Acol", c), ("mods", l)], hTu[:, :, sub * 128:(sub + 1) * 128], "hTu", (xt, xnb, sm))
        self.proj_fm(praw, "praw", wx, "wx", 0, 16, hTu, "hTu", W)
        self.conv3(cv, "cv", praw, "praw", cw, "cw", 16, W, 1 if c == 1 else W // 64)
        for j in range(16):
            S(lambda e, j=j: e.activation(out=xbcT[:, j, 0:W], in_=cv[:, j, 0:W], func=AF.Silu, bias=cb[:, j:j + 1]), ["cv", "cb"], ["xbcT"])
        for sub in range(nsub):
            for k in range(8):
                T(lambda e, k=k, sub=sub: e.matmul(self.bank(6, 32), lhsT=hTu[:, k, sub * 128:(sub + 1) * 128], rhs=wx[:, k, 2048:2080],
                                                   start=(k == 0), stop=(k == 7)), ["hTu", "wx"], [("ps", 6)])
            V(lambda e, sub=sub: e.tensor_tensor(out=dt[:, sub, :], in0=self.bank(6, 16, d * 16), in1=dtb[:, d * 16:(d + 1) * 16], op=ALU.add),
              [("ps", 6), "dtb"], ["dt"])
        S(lambda e: e.activation(out=dt[:, 0:nsub, :], in_=dt[:, 0:nsub, :], func=AF.Exp), ["dt"], ["dt"])
        S(lambda e: e.activation(out=dt[:, 0:nsub, :], in_=dt[:, 0:nsub, :], func=AF.Ln, bias=1.0), ["dt"], ["dt"])
        V(lambda e: e.tensor_tensor(out=dta[:, 0:nsub, :], in0=dt[:, 0:nsub, :],
                                    in1=arow[:, d * 16:(d + 1) * 16].unsqueeze(1).to_broadcast([128, nsub, 16]), op=ALU.mult),
          ["dt", "arow"], ["dta"])
        chunks = list(range(nsub))
        if d == 1:
            chunks = chunks[::-1]
        def chunk_body(ch):
            tile = t0 + ch
            c0 = ch * 128
            cols = slice(c0, c0 + 128)
            for j in range(8):
                T(lambda e, j=j: e.transpose(pbf(7)[:, j * 128:(j + 1) * 128], xbcT[:, j, cols], self.identb), ["xbcT", "identb"], [("ps", 7)])
            S(lambda e: e.copy(out=xs_tm, in_=pbf(7)), [("ps", 7)], ["xs_tm"])
            for j in range(4):
                T(lambda e, j=j: e.transpose(pbf(6)[:, j * 128:(j + 1) * 128], xbcT[:, 8 + j, cols], self.identb), ["xbcT", "identb"], [("ps", 6)])
            S(lambda e: e.copy(out=bm_tm, in_=pbf(6)[:, 0:512]), [("ps", 6)], ["bm_tm"])
            for g in range(4):
                T(lambda e, g=g: e.matmul(self.bank(7, 128, g * 128), lhsT=xbcT[:, 8 + g, cols], rhs=xbcT[:, 12 + g, cols], start=True, stop=True),
                  ["xbcT"], [("ps", 7)])
            V(lambda e: e.tensor_tensor(out=cbm, in0=self.bank(7).rearrange("p (g t) -> p g t", t=128),
                                        in1=Mk.unsqueeze(1).to_broadcast([128, 4, 128]), op=ALU.mult), [("ps", 7), "cst"], ["cbm"])
            dta_c = dta[:, ch, :]; dt_c = dt[:, ch, :]
            for n_, lh in enumerate((Bm, Am, self.ones)):
                T(lambda e, n_=n_, lh=lh: e.matmul(self.bank(6, 16, n_ * 16), lhsT=lh, rhs=dta_c, start=True, stop=True), ["dta", "cst"], [("ps", 6)])
            S(lambda e: e.activation(out=e48, in_=self.bank(6, 48), func=AF.Exp), [("ps", 6)], ["e48"])
            V(lambda e: e.tensor_tensor(out=wend, in0=e48[:, 16:32], in1=dt_c, op=ALU.mult), ["e48", "dt"], ["wend"])
            V(lambda e: e.tensor_tensor(out=R, in0=dta_c.unsqueeze(2).to_broadcast([128, 16, 128]),
                                        in1=Bm.unsqueeze(1).to_broadcast([128, 16, 128]), op=ALU.mult), ["dta", "cst"], ["R"])
            for q in range(4):
                b = q % 2
                T(lambda e, q=q, b=b: e.matmul(self.bank(b), lhsT=Am, rhs=R[:, q * 4:(q + 1) * 4, :].rearrange("p a b -> p (a b)"), start=True, stop=True),
                  ["R", "cst"], [("ps", b)])
                S(lambda e, b=b: e.activation(out=Eq.rearrange("p a b -> p (a b)"), in_=self.bank(b), func=AF.Exp), [("ps", b)], ["Eq"])
                V(lambda e, q=q: e.tensor_tensor(out=Eq, in0=Eq, in1=cbm[:, q:q + 1, :].to_broadcast([128, 4, 128]), op=ALU.mult), ["Eq", "cbm"], ["Eq"])
                V(lambda e, q=q: e.tensor_tensor(out=attT[:, q * 4:(q + 1) * 4, :], in0=Eq,
                                                 in1=dt_c[:, q * 4:(q + 1) * 4].unsqueeze(2).to_broadcast([128, 4, 128]), op=ALU.mult),
                  ["Eq", "dt"], ["attT"])
            for h in range(16):
                T(lambda e, h=h: e.matmul(self.bank(2 + h // 8, 64, (h % 8) * 64), lhsT=attT[:, h, :], rhs=xs_tm[:, h * 64:(h + 1) * 64], start=True, stop=True),
                  ["attT", "xs_tm"], [("ps", 2 + h // 8)])
            for g in range(4):
                T(lambda e, g=g: e.matmul(self.bank(4 + g // 2, 256, (g % 2) * 256), lhsT=xbcT[:, 12 + g, cols], rhs=Sb[:, g * 256:(g + 1) * 256], start=True, stop=True),
                  ["xbcT", "Sb"], [("ps", 4 + g // 2)])
            V(lambda e: e.tensor_tensor(out=ych.rearrange("p (h q) -> p h q", q=64), in0=self.psum[:, 4:6, :].rearrange("p a (h q) -> p (a h) q", q=64),
                                        in1=e48[:, 0:16].unsqueeze(2).to_broadcast([128, 16, 64]), op=ALU.mult), [("ps", 4), ("ps", 5), "e48"], ["ych"])
            V(lambda e: e.tensor_tensor(out=ych.rearrange("p (a b) -> p a b", b=512), in0=ych.rearrange("p (a b) -> p a b", b=512), in1=self.psum[:, 2:4, :], op=ALU.add),
              ["ych", ("ps", 2), ("ps", 3)], ["ych"])
            ydst = self.yscr[tile * 128:(tile + 1) * 128, :]
            if d == 1:
                P.dma("sync", lambda e, ydst=ydst: e.dma_start(out=ypv, in_=ydst), reads=[("yscr", tile)], writes=["ypv"])
                V(lambda e: e.tensor_tensor(out=ych, in0=ych, in1=ypv, op=ALU.add), ["ych", "ypv"], ["ych"])
                P.dma("scalar", lambda e, tile=tile: e.dma_start(out=self.xsscr[tile * 128:(tile + 1) * 128, :], in_=xs_tm), reads=["xs_tm"],
                      writes=[("xsscr", tile)], sem="xs_st")
            P.dma("scalar", lambda e, ydst=ydst: e.dma_start(out=ydst, in_=ych), reads=["ych"], writes=[("yscr", tile)], sem="y_st")
            V(lambda e: e.tensor_tensor(out=xw.rearrange("p (h q) -> p h q", q=64), in0=xs_tm.rearrange("p (h q) -> p h q", q=64),
                                        in1=wend.unsqueeze(2).to_broadcast([128, 16, 64]), op=ALU.mult), ["xs_tm", "wend"], ["xw"])
            for g in range(4):
                T(lambda e, g=g: e.matmul(self.bank(4 + g // 2, 256, (g % 2) * 256), lhsT=bm_tm[:, g * 128:(g + 1) * 128], rhs=xw[:, g * 256:(g + 1) * 256], start=True, stop=True),
                  ["bm_tm", "xw"], [("ps", 4 + g // 2)])
            V(lambda e: e.tensor_tensor(out=St.rearrange("p (h q) -> p h q", q=64), in0=St.rearrange("p (h q) -> p h q", q=64),
                                        in1=e48[:, 32:48].unsqueeze(2).to_broadcast([128, 16, 64]), op=ALU.mult), ["St", "e48"], ["St"])
            V(lambda e: e.tensor_tensor(out=St.rearrange("p (a b) -> p a b", b=512), in0=St.rearrange("p (a b) -> p a b", b=512), in1=self.psum[:, 4:6, :], op=ALU.add),
              ["St", ("ps", 4), ("ps", 5)], ["St"])
            S(lambda e: e.copy(out=Sb, in_=St), ["St"], ["Sb"])
        for ch in chunks:
            chunk_body(ch)
    for u in units:
        unit_body(*u)
    P.barrier()
    A.off = mark


KB.ssd_inputs = ssd_inputs
KB.mix_cols = mix_cols
KB.ssd_scan = ssd_scan


def ssd_finish(self, src_fn, dst_fn):
    P, A, V, S, T = self.P, self.A, self.V, self.S, self.T
    mark = A.off
    l = 0
    wc = A.alloc([8, 4096], BF16)
    wo = A.alloc([16, D], BF16)
    stage = [A.alloc([2048]) for _ in range(2)]
    self.load_w_bf16(wc, self.ev_w_in, 0, 4096, "wc", stage)
    n = 0
    for k in range(16):
        s = n % 2; n += 1
        P.dma("sync" if s == 0 else "scalar", lambda e, k=k, s=s: e.dma_start(out=stage[s][:, 0:D], in_=self.ev_w_out[k * 128:(k + 1) * 128, :]),
              writes=[("stg", s)])
        P.op("gpsimd", lambda e, k=k, s=s: e.tensor_copy(out=wo[:, k, :], in_=stage[s][:, 0:D]), reads=[("stg", s)], writes=["wo"])
    A.off -= 2 * 2048
    P.barrier()
    cw = A.alloc([8, 3]); nT = A.alloc([8]); dsk = A.alloc([16]); nwr = A.alloc([D])
    for dst_, src_, k_ in ((cw, self.sc_cwT.ap().rearrange("p (a b) -> p a b", b=3), "cw"), (nT, self.nmixT[l], "nT"),
                           (dsk, self.dsk_row.ap(), "dsk"), (nwr, self.ssdnw_row.ap(), "nwr")):
        P.dma("sync", lambda e, dst_=dst_, src_=src_: e.dma_start(out=dst_, in_=src_), writes=[k_], sem="misc")
    Acol = [A.alloc([8]) for _ in range(2)]
    Bcol = [self.mix_cols(l, c, nT, Acol[c], ("Acol", c)) for c in range(2)]
    Grow = A.alloc([D])
    xt = A.alloc([D]); xnb = A.alloc([D], BF16); sm = A.alloc([16])
    hTu = A.alloc([8, 256], BF16)
    praw = A.alloc([16, 256]); cv = A.alloc([8, 256]); yaT = A.alloc([8, 256], BF16)
    zs = A.alloc([D]); ysum = A.alloc([D]); xsb = A.alloc([D], BF16); yb = A.alloc([D]); ybn = A.alloc([D], BF16)
    ybT = A.alloc([8, 128], BF16); xo = zs
    pbf = lambda b: self.psum[:, b, :].bitcast(BF16)
    units = [(0, 256, 1)] + [(i, 128, 0) for i in range(2, NT)]
    cur_c = [None]
    def unit_body(t0, W, c):
        nsub = W // 128
        if c != cur_c[0]:
            cur_c[0] = c
            self.make_row(Grow, self.modsFM[l][:, 16:24, c], "Grow", [("mods", l)], banks=(4, 5))
        for sub in range(nsub):
            self.norm_T(src_fn(t0 + sub), Acol[c], Bcol[c], [("Acol", c), ("mods", l)], hTu[:, :, sub * 128:(sub + 1) * 128], "hTu", (xt, xnb, sm))
        self.proj_fm(praw, "praw", wc, "wc", 0, 8, hTu, "hTu", W)
        self.proj_fm(praw[:, 8:16, :], "praw2", wc, "wc", 2048, 8, hTu, "hTu", W)
        V(lambda e: e.tensor_tensor(out=praw[:, 0:8, 0:W], in0=praw[:, 0:8, 0:W], in1=praw[:, 8:16, 0:W], op=ALU.mult), ["praw", "praw2"], ["praw"])
        self.conv3(cv, "cv", praw, "praw", cw, "cw", 8, W, 1 if c == 1 else W // 64)
        self.proj_fm(praw[:, 8:16, :], "praw2", wc, "wc", 1024, 8, hTu, "hTu", W)
        V(lambda e: e.tensor_tensor(out=yaT[:, :, 0:W], in0=cv[:, :, 0:W], in1=praw[:, 8:16, 0:W], op=ALU.mult), ["cv", "praw2"], ["yaT"])
        def sub_body(sub):
            tile = t0 + sub
            cols = slice(sub * 128, (sub + 1) * 128)
            for n2 in range(2):
                for k in range(8):
                    T(lambda e, n2=n2, k=k: e.matmul(self.bank(n2), lhsT=hTu[:, k, cols], rhs=wc[:, k, 3072 + n2 * 512:3072 + (n2 + 1) * 512],
                                                     start=(k == 0), stop=(k == 7)), ["hTu", "wc"], [("ps", n2)])
            S(lambda e: e.activation(out=zs.rearrange("p (a b) -> p a b", b=512), in_=self.psum[:, 0:2, :], func=AF.Silu), [("ps", 0), ("ps", 1)], ["zs"])
            P.dma("sync", lambda e, tile=tile: e.dma_start(out=ysum, in_=self.yscr[tile * 128:(tile + 1) * 128, :]), reads=[("yscr", tile)], writes=["ysum"])
            P.dma("sync", lambda e, tile=tile: e.dma_start(out=xsb, in_=self.xsscr[tile * 128:(tile + 1) * 128, :]), reads=[("xsscr", tile)], writes=["xsb"])
            V(lambda e: e.tensor_tensor(out=yb.rearrange("p (h q) -> p h q", q=64), in0=xsb.rearrange("p (h q) -> p h q", q=64),
                                        in1=dsk.unsqueeze(2).to_broadcast([128, 16, 64]), op=ALU.mult), ["xsb", "dsk"], ["yb"])
            V(lambda e: e.tensor_tensor(out=yb, in0=yb, in1=ysum, op=ALU.add), ["yb", "ysum"], ["yb"])
            V(lambda e: e.tensor_tensor(out=yb, in0=yb, in1=zs, op=ALU.mult), ["yb", "zs"], ["yb"])
            for g in range(4):
                S(lambda e, g=g: e.activation(out=ysum[:, g * 256:(g + 1) * 256], in_=yb[:, g * 256:(g + 1) * 256], func=AF.Square, accum_out=sm[:, 4 + g:5 + g]),
                  ["yb"], ["ysum", "sm4"])
            S(lambda e: e.activation(out=sm[:, 8:12], in_=sm[:, 4:8], func=AF.Sqrt, scale=1.0 / 256, bias=EPS), ["sm4"], ["sm8"])
            V(lambda e: e.reciprocal(out=sm[:, 8:12], in_=sm[:, 8:12]), ["sm8"], ["sm8"])
            V(lambda e: e.tensor_tensor(out=yb.rearrange("p (g q) -> p g q", q=256), in0=yb.rearrange("p (g q) -> p g q", q=256),
                                        in1=sm[:, 8:12].unsqueeze(2).to_broadcast([128, 4, 256]), op=ALU.mult), ["yb", "sm8"], ["yb"])
            V(lambda e: e.tensor_tensor(out=ybn, in0=yb, in1=nwr, op=ALU.mult), ["yb", "nwr"], ["ybn"])
            for k in range(8):
                T(lambda e, k=k: e.transpose(pbf(7)[:, k * 128:(k + 1) * 128], ybn[:, k * 128:(k + 1) * 128], self.identb), ["ybn", "identb"], [("ps", 7)])
            S(lambda e: e.copy(out=ybT.rearrange("p a b -> p (a b)"), in_=pbf(7)), [("ps", 7)], ["ybT"])
            for n2 in range(2):
                for kc in range(16):
                    lh = yaT[:, kc, cols] if kc < 8 else ybT[:, kc - 8, :]
                    T(lambda e, n2=n2, kc=kc, lh=lh: e.matmul(self.bank(2 + n2), lhsT=lh, rhs=wo[:, kc, n2 * 512:(n2 + 1) * 512],
                                                              start=(kc == 0), stop=(kc == 15)), ["yaT", "ybT", "wo"], [("ps", 2 + n2)])
            V(lambda e: e.tensor_tensor(out=xo.rearrange("p (a b) -> p a b", b=512), in0=self.psum[:, 2:4, :], in1=Grow.rearrange("p (a b) -> p a b", b=512), op=ALU.mult),
              [("ps", 2), ("ps", 3), "Grow"], ["zs"])
            P.dma("sync", lambda e, tile=tile: e.dma_start(out=ysum, in_=src_fn(tile)), writes=["ysum"])
            V(lambda e: e.tensor_tensor(out=xo, in0=xo, in1=ysum, op=ALU.add), ["zs", "ysum"], ["zs"])
            P.dma("scalar", lambda e, tile=tile: e.dma_start(out=dst_fn(tile), in_=xo), reads=["zs"], writes=[self.key("x1")], sem="xo_st")
        for sub in range(nsub):
            sub_body(sub)
    for u in units:
        unit_body(*u)
    P.barrier()
    A.off = mark


def mixer0(self, src_fn, dst_fn):
    self._mix_helpers()
    self.ssd_scan(0, src_fn)
    self.ssd_scan(1, src_fn)
    self.ssd_finish(src_fn, dst_fn)


KB.ssd_finish = ssd_finish
KB.mixer0 = mixer0


def hg_inputs(self):
    nc = self.nc
    di = lambda n, s, dt=F32: nc.dram_tensor(n, list(s), dt, kind="ExternalInput")
    self.od_w_in = di("od_w_in", [D, 5120]); self.od_w_out = di("od_w_out", [D, D])
    self.lblT = di("lblT", [128, 32])
    self.hgnw_row = di("hgnw_row", [128, D])
    self.scanmask = di("scanmask", [128, 1024])


def hg_pass(self, d, src_fn, dst_fn):
    P, A, V, S, T = self.P, self.A, self.V, self.S, self.T
    mark = A.off
    l = 1
    fin = (d == 1)
    ncol = 5 if fin else 3
    w = A.alloc([8, ncol * 1024], BF16)
    stage = [A.alloc([1024]) for _ in range(2)]
    colsrc = [0, 1024 + d * 1024, 3072, 4096]
    for n_, c0 in enumerate(colsrc[:4 if fin else 3]):
        self.load_w_bf16(w[:, :, n_ * 1024:(n_ + 1) * 1024], self.od_w_in, c0, 1024, "w", stage)
    if fin:
        n = 0
        for k in range(8):
            s = n % 2; n += 1
            P.dma("sync" if s == 0 else "scalar", lambda e, k=k, s=s: e.dma_start(out=stage[s], in_=self.od_w_out[k * 128:(k + 1) * 128, :]), writes=[("stg", s)])
            P.op("gpsimd", lambda e, k=k, s=s: e.tensor_copy(out=w[:, k, 4096:5120], in_=stage[s]), reads=[("stg", s)], writes=["w"])
    P.barrier()
    A.off -= 2 * 1024
    nT = A.alloc([8]); lbl = A.alloc([32]); lb = A.alloc([8]); oml = A.alloc([8]); smask = A.alloc([D])
    nwr = A.alloc([D])
    for dst_, src_, k_ in ((nT, self.nmixT[l], "nT"), (lbl, self.lblT.ap(), "lbl"), (smask, self.scanmask.ap(), "smask"), (nwr, self.hgnw_row.ap(), "nwr")):
        P.dma("sync", lambda e, dst_=dst_, src_=src_: e.dma_start(out=dst_, in_=src_), writes=[k_], sem="misc")
    V(lambda e: e.tensor_tensor(out=lb, in0=lbl[:, 16 + d * 8:24 + d * 8], in1=lbl[:, d * 8:d * 8 + 8], op=ALU.subtract), ["lbl"], ["lb"])
    S(lambda e: e.activation(out=lb, in_=lb, func=AF.Sigmoid), ["lb"], ["lb"])
    V(lambda e: e.tensor_scalar(out=oml, in0=lb, scalar1=-1.0, scalar2=1.0, op0=ALU.mult, op1=ALU.add), ["lb"], ["oml"])
    Acol = [A.alloc([8]) for _ in range(2)]
    Bcol = [self.mix_cols(l, c, nT, Acol[c], ("Acol", c)) for c in range(2)]
    xt = A.alloc([D]); xnb = A.alloc([D], BF16); sm = A.alloc([32])
    hT = A.alloc([8, 128], BF16)
    praw = A.alloc([8, 128]); qF = A.alloc([8, 128]); fF = A.alloc([8, 128]); gl = A.alloc([8, 128]); GC = A.alloc([8, 128])
    ex = A.alloc([8, 128])
    qg = A.alloc([8, 128], BF16); kg = A.alloc([8, 128], BF16); kend = A.alloc([8, 128], BF16); iF = A.alloc([8, 128], BF16)
    eGE = A.alloc([8, 2]); GE = A.alloc([8, 2])
    v_tm = A.alloc([D], BF16); kend_tm = A.alloc([D], BF16); attm = A.alloc([8, 64], BF16)
    och = A.alloc([D]); St = A.alloc([8, 128]); Sb = A.alloc([8, 128], BF16)
    V(lambda e: e.memset(St, 0.0), [], ["St"])
    V(lambda e: e.memset(Sb, 0.0), [], ["Sb"])
    if fin:
        opv = A.alloc([D]); gs = A.alloc([D]); onb = A.alloc([D], BF16); onT = A.alloc([8, 64], BF16)
        Grow = A.alloc([D])
        self.make_row(Grow, self.modsFM[l][:, 16:24, 0], "Grow", [("mods", l)], banks=(4, 5))
    cst = self.cst
    Mk = cst[0:64, 256:320] if d == 0 else cst[0:64, 512:576]
    pbf = lambda b: self.psum[:, b, :].bitcast(BF16)
    tiles = [0, 1] + list(range(2, NT))
    if d == 1:
        tiles = [1, 0] + list(range(NT - 1, 1, -1))
    flat = lambda a: a.rearrange("p a b -> p (a b)")
    bc8 = lambda col: col.unsqueeze(2).to_broadcast([128, 8, 128])

    def tile_body(i):
        c = 1 if i < 2 else 0
        need_out = (i >= 2)
        self.norm_T(src_fn(i), Acol[c], Bcol[c], [("Acol", c), ("mods", l)], hT, "hT", (xt, xnb, sm))
        if need_out:
            self.proj_fm(praw, "praw", w, "w", 0, 8, hT, "hT", 128)
            S(lambda e: e.activation(out=qF, in_=praw, func=AF.Silu), ["praw"], ["qF"])
        self.proj_fm(praw, "praw", w, "w", 1024, 8, hT, "hT", 128)
        S(lambda e: e.activation(out=fF, in_=praw, func=AF.Sigmoid), ["praw"], ["fF"])
        V(lambda e: e.tensor_tensor(out=fF, in0=fF, in1=bc8(oml), op=ALU.mult), ["fF", "oml"], ["fF"])
        V(lambda e: e.tensor_tensor(out=fF, in0=fF, in1=bc8(lb), op=ALU.add), ["fF", "lb"], ["fF"])
        S(lambda e: e.activation(out=gl, in_=fF, func=AF.Ln), ["fF"], ["gl"])
        V(lambda e: e.tensor_scalar(out=fF, in0=fF, scalar1=-1.0, scalar2=1.0, op0=ALU.mult, op1=ALU.add), ["fF"], ["fF"])
        V(lambda e: e.tensor_tensor_scan(out=flat(GC), data0=smask, data1=flat(gl), initial=0.0, op0=ALU.mult, op1=ALU.add), ["smask", "gl"], ["GC"])
        GC4 = GC.rearrange("p j (c t) -> p j c t", t=64)
        V(lambda e: e.tensor_copy(out=GE, in_=GC4[:, :, :, 63]), ["GC"], ["GE"])
        if d == 1:
            V(lambda e: e.tensor_tensor(out=GC, in0=gl, in1=GC, op=ALU.subtract), ["gl", "GC"], ["GC"])
            V(lambda e: e.tensor_tensor(out=GC4, in0=GC4, in1=GE.unsqueeze(3).to_broadcast([128, 8, 2, 64]), op=ALU.add), ["GC", "GE"], ["GC"])
        S(lambda e: e.activation(out=eGE, in_=GE, func=AF.Exp), ["GE"], ["eGE"])
        if need_out:
            S(lambda e: e.activation(out=ex, in_=GC, func=AF.Exp), ["GC"], ["ex"])
            V(lambda e: e.scalar_tensor_tensor(out=flat(qg), in0=flat(qF), scalar=float(128 ** -0.5), in1=flat(ex), op0=ALU.mult, op1=ALU.mult), ["qF", "ex"], ["qg"])
            S(lambda e: e.activation(out=ex, in_=GC, func=AF.Exp, scale=-1.0), ["GC"], ["ex"])
            V(lambda e: e.tensor_tensor(out=kg, in0=fF, in1=ex, op=ALU.mult), ["fF", "ex"], ["kg"])
        V(lambda e: e.tensor_tensor(out=GC4, in0=GE.unsqueeze(3).to_broadcast([128, 8, 2, 64]), in1=GC4, op=ALU.subtract), ["GC", "GE"], ["GC"])
        S(lambda e: e.activation(out=ex, in_=GC, func=AF.Exp), ["GC"], ["ex"])
        V(lambda e: e.tensor_tensor(out=kend, in0=fF, in1=ex, op=ALU.mult), ["fF", "ex"], ["kend"])
        self.proj_fm(praw, "praw", w, "w", 2048, 8, hT, "hT", 128)
        V(lambda e: e.tensor_copy(out=iF, in_=praw), ["praw"], ["iF"])
        subs = [0, 1] if d == 0 else [1, 0]

        def sub_body(sc):
            cols = slice(sc * 64, sc * 64 + 64)
            rows = slice(i * 128 + sc * 64, i * 128 + sc * 64 + 64)
            for j in range(8):
                T(lambda e, j=j: e.transpose(pbf(7)[0:64, j * 128:(j + 1) * 128], iF[:, j, cols], self.identb), ["iF", "identb"], [("ps", 7)])
            S(lambda e: e.copy(out=v_tm[0:64, :], in_=pbf(7)[0:64, :]), [("ps", 7)], ["v_tm"])
            for j in range(8):
                T(lambda e, j=j: e.transpose(pbf(6)[0:64, j * 128:(j + 1) * 128], kend[:, j, cols], self.identb), ["kend", "identb"], [("ps", 6)])
            S(lambda e: e.copy(out=kend_tm[0:64, :], in_=pbf(6)[0:64, :]), [("ps", 6)], ["kend_tm"])
            if need_out:
                for j in range(8):
                    T(lambda e, j=j: e.matmul(self.psum[0:64, 0, j * 64:(j + 1) * 64], lhsT=kg[:, j, cols], rhs=qg[:, j, cols], start=True, stop=True),
                      ["kg", "qg"], [("ps", 0)])
                V(lambda e: e.tensor_tensor(out=attm[0:64], in0=self.psum[0:64, 0, :].rearrange("p (j t) -> p j t", t=64),
                                            in1=Mk.unsqueeze(1).to_broadcast([64, 8, 64]), op=ALU.mult), [("ps", 0), "cst"], ["attm"])
                for j in range(8):
                    ob = self.psum[0:64, 2 + j // 4, (j % 4) * 128:(j % 4 + 1) * 128]
                    T(lambda e, j=j, ob=ob: e.matmul(ob, lhsT=attm[0:64, j, :], rhs=v_tm[0:64, j * 128:(j + 1) * 128], start=True, stop=False),
                      ["attm", "v_tm"], [("ps", 2 + j // 4)])
                    T(lambda e, j=j, ob=ob: e.matmul(ob, lhsT=qg[:, j, cols], rhs=Sb[:, j, :], start=False, stop=True),
                      ["qg", "Sb"], [("ps", 2 + j // 4)])
                ydst = self.yscr[rows, :]
                if not fin:
                    S(lambda e: e.copy(out=och[0:64].rearrange("p (a b) -> p a b", b=512), in_=self.psum[0:64, 2:4, :]), [("ps", 2), ("ps", 3)], ["och"])
                    P.dma("scalar", lambda e: e.dma_start(out=ydst, in_=och[0:64]), reads=["och"], writes=[("yscr", i, sc)], sem="y_st")
                else:
                    P.dma("sync", lambda e: e.dma_start(out=opv[0:64], in_=ydst), reads=[("yscr", i, sc)], writes=["opv"])
                    V(lambda e: e.tensor_tensor(out=och[0:64].rearrange("p (a b) -> p a b", b=512), in0=self.psum[0:64, 2:4, :],
                                                in1=opv[0:64].rearrange("p (a b) -> p a b", b=512), op=ALU.add), [("ps", 2), ("ps", 3), "opv"], ["och"])
            for j in range(8):
                T(lambda e, j=j: e.matmul(self.psum[:, 4 + j // 4, (j % 4) * 128:(j % 4 + 1) * 128], lhsT=kend_tm[0:64, j * 128:(j + 1) * 128],
                                          rhs=v_tm[0:64, j * 128:(j + 1) * 128], start=True, stop=True), ["kend_tm", "v_tm"], [("ps", 4 + j // 4)])
            V(lambda e: e.tensor_tensor(out=St, in0=St, in1=eGE[:, :, sc:sc + 1].to_broadcast([128, 8, 128]), op=ALU.mult), ["St", "eGE"], ["St"])
            V(lambda e: e.tensor_tensor(out=flat(St).rearrange("p (a b) -> p a b", b=512), in0=flat(St).rearrange("p (a b) -> p a b", b=512),
                                        in1=self.psum[:, 4:6, :], op=ALU.add), ["St", ("ps", 4), ("ps", 5)], ["St"])
            S(lambda e: e.copy(out=Sb, in_=St), ["St"], ["Sb"])
            if need_out and fin:
                for j in range(8):
                    S(lambda e, j=j: e.activation(out=opv[0:64, j * 128:(j + 1) * 128], in_=och[0:64, j * 128:(j + 1) * 128], func=AF.Square,
                                                  accum_out=sm[0:64, 8 + j:9 + j]), ["och"], ["opv", "sm8"])
                S(lambda e: e.activation(out=sm[0:64, 16:24], in_=sm[0:64, 8:16], func=AF.Sqrt, scale=1.0 / 128, bias=EPS), ["sm8"], ["sm16"])
                V(lambda e: e.reciprocal(out=sm[0:64, 16:24], in_=sm[0:64, 16:24]), ["sm16"], ["sm16"])
                V(lambda e: e.tensor_tensor(out=och[0:64].rearrange("p (j q) -> p j q", q=128), in0=och[0:64].rearrange("p (j q) -> p j q", q=128),
                                            in1=sm[0:64, 16:24].unsqueeze(2).to_broadcast([64, 8, 128]), op=ALU.mult), ["och", "sm16"], ["och"])
                V(lambda e: e.tensor_tensor(out=och[0:64], in0=och[0:64], in1=nwr[0:64], op=ALU.mult), ["och", "nwr"], ["och"])
                for n2 in range(2):
                    for k in range(8):
                        T(lambda e, n2=n2, k=k: e.matmul(self.psum[0:64, n2, :], lhsT=hT[:, k, cols], rhs=w[:, k, 3072 + n2 * 512:3072 + (n2 + 1) * 512],
                                                         start=(k == 0), stop=(k == 7)), ["hT", "w"], [("ps", n2)])
                S(lambda e: e.activation(out=gs[0:64].rearrange("p (a b) -> p a b", b=512), in_=self.psum[0:64, 0:2, :], func=AF.Silu), [("ps", 0), ("ps", 1)], ["gs"])
                V(lambda e: e.tensor_tensor(out=onb[0:64], in0=och[0:64], in1=gs[0:64], op=ALU.mult), ["och", "gs"], ["onb"])
                for k in range(8):
                    T(lambda e, k=k: e.transpose(pbf(7)[:, k * 64:(k + 1) * 64], onb[0:64, k * 128:(k + 1) * 128], self.identb[0:64, 0:64]),
                      ["onb", "identb"], [("ps", 7)])
                S(lambda e: e.copy(out=flat(onT), in_=pbf(7)[:, 0:512]), [("ps", 7)], ["onT"])
                for n2 in range(2):
                    for k in range(8):
                        T(lambda e, n2=n2, k=k: e.matmul(self.psum[0:64, 2 + n2, :], lhsT=onT[:, k, :], rhs=w[:, k, 4096 + n2 * 512:4096 + (n2 + 1) * 512],
                                                         start=(k == 0), stop=(k == 7)), ["onT", "w"], [("ps", 2 + n2)])
                V(lambda e: e.tensor_tensor(out=gs[0:64].rearrange("p (a b) -> p a b", b=512), in0=self.psum[0:64, 2:4, :],
                                            in1=Grow[0:64].rearrange("p (a b) -> p a b", b=512), op=ALU.mult), [("ps", 2), ("ps", 3), "Grow"], ["gs"])
                P.dma("sync", lambda e: e.dma_start(out=opv[0:64], in_=src_fn(i)[sc * 64:sc * 64 + 64, :]), writes=["opv"])
                V(lambda e: e.tensor_tensor(out=gs[0:64], in0=gs[0:64], in1=opv[0:64], op=ALU.add), ["gs", "opv"], ["gs"])
                P.dma("scalar", lambda e: e.dma_start(out=dst_fn(i)[sc * 64:sc * 64 + 64, :], in_=gs[0:64]), reads=["gs"], writes=[self.key("x3")], sem="xo_st")
        for sc in subs:
            sub_body(sc)
    for i in tiles:
        tile_body(i)
    P.barrier()
    A.off = mark


def mixer1(self, src_fn, dst_fn):
    self._mix_helpers()
    self.hg_pass(0, src_fn, dst_fn)
    self.hg_pass(1, src_fn, dst_fn)


KB.hg_inputs = hg_inputs
KB.hg_pass = hg_pass
KB.mixer1 = mixer1
```

```python
import contextlib
import numpy as np
import concourse.bass as bass
import concourse.mybir as mybir
from concourse.alu_op_type import AluOpType as ALU
from concourse.bass_utils import run_bass_kernel_spmd

AF = mybir.ActivationFunctionType
AX = mybir.AxisListType
F32 = mybir.dt.float32
BF16 = mybir.dt.bfloat16
U32 = mybir.dt.uint32
I32 = mybir.dt.int32
U16 = mybir.dt.uint16

ENGS = ("sync", "scalar", "vector", "gpsimd", "tensor")
SAME_ENGINE_WAITS = True


class Prog:
    def __init__(self, nc):
        self.nc = nc
        self.stack = contextlib.ExitStack()
        self.q = {e: [] for e in ENGS}
        self.cnt = {}
        self.waited = {e: {} for e in ENGS}
        self.last_w = {}
        self.reads = {}
        self.semkeys = []
        self.sems = {}
        self.nops = 0
        self.shared = set()

    def sb(self, name, shape, dt):
        return self.stack.enter_context(self.nc.sbuf_tensor(name, list(shape), dt))

    def ps(self, name, shape, dt):
        return self.stack.enter_context(self.nc.psum_tensor(name, list(shape), dt))

    def dram(self, name, shape, dt, kind="Internal"):
        return self.nc.dram_tensor(name, list(shape), dt, kind=kind)

    def _semkey(self, k):
        if k not in self.cnt:
            self.cnt[k] = 0
            self.semkeys.append(k)
        return k

    def _deps(self, eng, reads, writes):
        deps = {}
        def add(ev, war=False):
            if ev is None:
                return
            k, v = ev
            if k in self.shared:
                v = self.cnt[k]
            if k == ("E", eng):
                if eng == "tensor" or war or not SAME_ENGINE_WAITS:
                    return
            if deps.get(k, 0) < v:
                deps[k] = v
        for r in reads:
            add(self.last_w.get(r))
        for w in writes:
            add(self.last_w.get(w))
            for ev in self.reads.get(w, ()):
                add(ev, war=True)
        out = []
        wd = self.waited[eng]
        for k, v in deps.items():
            if wd.get(k, 0) >= v:
                continue
            wd[k] = v
            out.append((k, v))
        return out

    def _record(self, ev, reads, writes):
        for r in reads:
            self.reads.setdefault(r, []).append(ev)
        for w in writes:
            self.last_w[w] = ev
            self.reads[w] = []

    def op(self, eng, fn, reads=(), writes=()):
        reads = tuple(reads); writes = tuple(writes)
        waits = self._deps(eng, reads, writes)
        k = self._semkey(("E", eng))
        self.cnt[k] += 1
        v = self.cnt[k]
        self.q[eng].append((waits, fn, k, 1))
        self._record((k, v), reads, writes)
        self.nops += 1

    def dma(self, eng, fn, reads=(), writes=(), sem=None):
        reads = tuple(reads); writes = tuple(writes)
        waits = self._deps(eng, reads, writes)
        k = self._semkey(("D", sem if sem is not None else writes[0]))
        if sem == "misc":
            self.shared.add(k)
        self.cnt[k] += 16
        v = self.cnt[k]
        self.q[eng].append((waits, fn, k, 16))
        self._record((k, v), reads, writes)
        self.nops += 1

    def wait_all(self, eng, keys):
        waits = self._deps(eng, tuple(keys), ())
        self.q[eng].append((waits, None, None, 0))

    def build(self):
        nc = self.nc
        for i, k in enumerate(self.semkeys):
            self.sems[k] = self.stack.enter_context(nc.semaphore("s%d" % i))
        block = self.stack.enter_context(nc.Block())
        sems = self.sems

        def run(e, items):
            for waits, fn, k, inc in items:
                for wk, wv in waits:
                    e.wait_ge(sems[wk], wv)
                if fn is not None:
                    fn(e).then_inc(sems[k], inc)

        @block.sync
        def _(e):
            run(e, self.q["sync"])

        @block.scalar
        def _(e):
            run(e, self.q["scalar"])

        @block.vector
        def _(e):
            run(e, self.q["vector"])

        @block.gpsimd
        def _(e):
            run(e, self.q["gpsimd"])

        @block.tensor
        def _(e):
            run(e, self.q["tensor"])

    def close(self):
        self.stack.close()


def _barrier(P):
    allk = [(k, P.cnt[k]) for k in P.semkeys if P.cnt[k] > 0]
    for e in ENGS:
        waits = []
        for k, v in allk:
            if P.waited[e].get(k, 0) < v:
                P.waited[e][k] = v
                waits.append((k, v))
        if waits:
            P.q[e].append((waits, None, None, 0))


Prog.barrier = _barrier


class Arena:
    def __init__(self, P, nbytes):
        self.words = nbytes // 4
        self.t = P.sb("arena", [128, self.words], F32)
        self.off = 0
        self.n = 0

    def alloc(self, shape, dt=F32):
        n = 1
        for s in shape:
            n *= s
        esz = 2 if dt in (BF16, U16) else 4
        words = (n * esz + 3) // 4
        words = (words + 7) // 8 * 8
        assert self.off + words <= self.words, ("arena overflow", self.off, words)
        ap = self.t[:, self.off:self.off + words]
        self.off += words
        if dt != F32:
            ap = ap.bitcast(dt)
        ap = ap[:, 0:n]
        if len(shape) > 1:
            names = "abcd"[:len(shape)]
            pat = "p (%s) -> p %s" % (" ".join(names), " ".join(names))
            ap = ap.rearrange(pat, **{names[i]: shape[i] for i in range(len(shape))})
        return ap


D = 1024
NT = 34
EPS = 1e-6
NEG = -1.0e30


class KB:
    def __init__(self, dbg=None):
        self.dbg = dbg or {}
        nc = self.nc = bass.Bass("TRN2", target_bir_lowering=False)
        P = self.P = Prog(nc)
        self.A = Arena(P, 172000)
        self.psum = P.ps("psum", [128, 8, 512], F32)
        di = lambda n, s, dt=F32: nc.dram_tensor(n, list(s), dt, kind="ExternalInput")
        self.x = di("x", [4096, D]); self.ctx = di("ctx", [256, D])
        self.condT = di("condT", [128, 16])
        self.consts = di("consts", [128, 1024])
        self.ada_w = di("ada_w", [2, D, 6 * D]); self.ada_bT = di("ada_bT", [2, 128, 48])
        self.nmixT = di("nmixT", [2, 128, 8]); self.nffnT = di("nffnT", [2, 128, 8])
        self.nf_row = di("nf_row", [128, D])
        self.peer_wq = di("peer_wq", [2, D, 2048]); self.keysT = di("keysT", [2, 128, 2048])
        self.peer_u = [di("peer_u%d" % l, [16384, D]) for l in range(2)]; self.peer_v = [di("peer_v%d" % l, [16384, D]) for l in range(2)]
        self.out = nc.dram_tensor("out", [4096, D], F32, kind="ExternalOutput")
        self.x1 = nc.dram_tensor("x1", [NT * 128, D], F32, kind=self.dbg.get("x1", "Internal"))
        self.x2 = nc.dram_tensor("x2", [NT * 128, D], F32, kind=self.dbg.get("x2", "Internal"))
        self.x3 = nc.dram_tensor("x3", [NT * 128, D], F32, kind=self.dbg.get("x3", "Internal"))
        self.uid = 0
        self.tab_bf = {}
        for l in range(2):
            self.tab_bf[("u", l)] = nc.dram_tensor("ub%d" % l, [16384, D], BF16, kind="Internal")
            self.tab_bf[("v", l)] = nc.dram_tensor("vb%d" % l, [16384, D], BF16, kind="Internal")

    def key(self, s):
        self.uid += 1
        return "%s#%d" % (s, self.uid)

    def bank(self, b, n=512, off=0):
        return self.psum[:, b, off:off + n]

    def src_tile(self, i):
        if i < 2:
            return self.ctx[i * 128:(i + 1) * 128, :]
        return self.x[(i - 2) * 128:(i - 1) * 128, :]

    def setup_consts(self):
        P, A = self.P, self.A
        self.cst = A.alloc([1024]);
        P.dma("sync", lambda e: e.dma_start(out=self.cst, in_=self.consts.ap()), writes=["cst"], sem="misc")
        self.ident = self.cst[:, 0:128]; self.ones = self.cst[:, 128:256]
        self.cond = A.alloc([16])
        P.dma("sync", lambda e: e.dma_start(out=self.cond, in_=self.condT.ap()), writes=["cond"], sem="misc")
        self.condS = A.alloc([8, 2])
        P.op("scalar", lambda e: e.activation(out=self.condS, in_=self.cond.rearrange("p (a b) -> p a b", b=2), func=AF.Silu),
             reads=["cond"], writes=["condS"])
        self.modsFM = [A.alloc([48, 2]) for _ in range(2)]
        self.identb = A.alloc([128], BF16)
        P.op("vector", lambda e: e.tensor_copy(out=self.identb, in_=self.ident), reads=["cst"], writes=["identb"])
        P.barrier()

    def mods_phase(self, l):
        P, A = self.P, self.A
        mark = A.off
        ws = [A.alloc([8, 768]) for _ in range(2)]
        abT = A.alloc([48])
        P.dma("sync", lambda e: e.dma_start(out=abT, in_=self.ada_bT[l]), writes=["abT"], sem="misc")
        pm = self.psum[:, 0, 0:96]
        awv = self.ada_w[l].rearrange("(k p) n -> p k n", p=128)
        for g in range(8):
            s = g % 2
            P.dma("sync" if s == 0 else "scalar", lambda e, g=g, s=s: e.dma_start(out=ws[s], in_=awv[:, :, g * 768:(g + 1) * 768]),
                  writes=[("ws", s)])
            for jj in range(6):
                j = g * 6 + jj
                for k in range(8):
                    P.op("tensor", lambda e, k=k, s=s, j=j, jj=jj: e.matmul(pm[:, 2 * j:2 * j + 2], lhsT=ws[s][:, k, jj * 128:(jj + 1) * 128],
                                                                           rhs=self.condS[:, k, :], start=(k == 0), stop=(k == 7)),
                         reads=[("ws", s), "condS"], writes=[("ps", 0)])
        m = self.modsFM[l]
        P.op("vector", lambda e: e.tensor_tensor(out=m, in0=pm.rearrange("p (a b) -> p a b", b=2),
                                                 in1=abT.unsqueeze(2).to_broadcast([128, 48, 2]), op=ALU.add),
             reads=[("ps", 0), "abT"], writes=[("mods", l)])
        P.barrier()
        A.off = mark

    def make_row(self, dst, col, dstkey, colkeys, banks=(6, 7)):
        P = self.P
        dg = self.diag
        for jj in range(8):
            P.op("vector", lambda e, jj=jj: e.tensor_scalar(out=dg, in0=self.ident, scalar1=col[:, jj:jj + 1], scalar2=None, op0=ALU.mult),
                 reads=["cst"] + list(colkeys), writes=["diag"])
            b = banks[jj // 4]
            P.op("tensor", lambda e, jj=jj, b=b: e.matmul(self.bank(b, 128, (jj % 4) * 128), lhsT=self.ones, rhs=dg, start=True, stop=True),
                 reads=["diag", "cst"], writes=[("ps", b)])
        for h in range(2):
            P.op("scalar", lambda e, h=h: e.copy(out=dst[:, h * 512:(h + 1) * 512], in_=self.bank(banks[h])),
                 reads=[("ps", banks[h])], writes=[dstkey])

    def conv_tables(self):
        P, A = self.P, self.A
        mark = A.off
        RB = 4
        ib = [A.alloc([RB, D]) for _ in range(2)]
        ob = [A.alloc([RB, D], BF16) for _ in range(3)]
        n = 0
        for l in range(2):
            for nm, src in (("u", self.peer_u[l]), ("v", self.peer_v[l])):
                sv = src.ap().rearrange("(p r) d -> p r d", p=128)
                dv = self.tab_bf[(nm, l)].ap().rearrange("(p r) d -> p r d", p=128)
                for c in range(128 // RB):
                    si = n % 2; so = n % 3; n += 1
                    P.dma("sync", lambda e, sv=sv, c=c, si=si: e.dma_start(out=ib[si], in_=sv[:, c * RB:(c + 1) * RB, :]), writes=[("tib", si)])
                    eng = ("vector", "gpsimd", "scalar")[so]
                    if eng == "scalar":
                        P.op(eng, lambda e, si=si, so=so: e.copy(out=ob[so], in_=ib[si]), reads=[("tib", si)], writes=[("tob", so)])
                    else:
                        P.op(eng, lambda e, si=si, so=so: e.tensor_copy(out=ob[so], in_=ib[si]), reads=[("tib", si)], writes=[("tob", so)])
                    P.dma("scalar" if so != 2 else "gpsimd", lambda e, dv=dv, c=c, so=so: e.dma_start(out=dv[:, c * RB:(c + 1) * RB, :], in_=ob[so]),
                          reads=[("tob", so)], writes=[self.key("tbf")], sem=("tob_st", so))
        P.barrier()
        A.off = mark

    def peer_phase(self, l, src_fn, dst, tiles, final_norm=False):
        P, A = self.P, self.A
        mark = A.off
        V = lambda fn, r, w: P.op("vector", fn, r, w)
        S = lambda fn, r, w: P.op("scalar", fn, r, w)
        T = lambda fn, r, w: P.op("tensor", fn, r, w)
        m = self.modsFM[l]
        mk = [("mods", l)]
        wq = A.alloc([8, 2048])
        for k in range(8):
            P.dma("sync" if k % 2 == 0 else "scalar",
                  lambda e, k=k: e.dma_start(out=wq[:, k, :], in_=self.peer_wq[l, k * 128:(k + 1) * 128, :]), writes=["wq"], sem="misc")
        kT = A.alloc([16, 128])
        P.dma("sync", lambda e: e.dma_start(out=kT, in_=self.keysT[l].rearrange("p (a b) -> p a b", b=128)), writes=["kT"], sem="misc")
        nf = A.alloc([8])
        P.dma("sync", lambda e: e.dma_start(out=nf, in_=self.nffnT[l]), writes=["nf"], sem="misc")
        Arow = A.alloc([D]); Brow = A.alloc([D]); Grow = A.alloc([D])
        acol = A.alloc([8])
        xt = A.alloc([D]); h2 = A.alloc([D]); hT = A.alloc([8, 128]); xn = h2
        qT = A.alloc([16, 128]); sub = A.alloc([16, 128]); sub2 = sub
        cand = sub.rearrange("p a b -> p (a b)").rearrange("p (a b) -> p a b", b=256)
        candi = qT.rearrange("p a b -> p (a b)").rearrange("p (a b) -> p a b", b=256)
        st = A.alloc([16, 16]); it = A.alloc([16, 16], U32); itf = A.alloc([16, 16])
        bs = A.alloc([8, 16]); idxf = A.alloc([128]); idx = A.alloc([128], U32)
        gate = A.alloc([8, 16]); sm = A.alloc([16]); act = A.alloc([128]); araw = A.alloc([128])
        jbig = A.alloc([2 * D]); junk = jbig[:, 0:D]; acc1 = jbig[:, D:2 * D]; acc = A.alloc([D])
        cand2 = jbig.rearrange("p (a b) -> p a b", b=256)
        NS = 6
        gs = [A.alloc([D], BF16) for _ in range(NS)]
        gcount = [0]
        if final_norm:
            nfr = A.alloc([D])
            P.dma("sync", lambda e: e.dma_start(out=nfr, in_=self.nf_row.ap()), writes=["nfr"], sem="misc")
        utab = self.tab_bf[("u", l)].ap(); vtab = self.tab_bf[("v", l)].ap()
        cur_c = [None]

        def set_cond(c):
            if cur_c[0] == c:
                return
            cur_c[0] = c
            V(lambda e: e.scalar_tensor_tensor(out=acol, in0=m[:, 32:40, c], scalar=1.0, in1=nf, op0=ALU.add, op1=ALU.mult),
              mk + ["nf"], ["acol"])
            self.make_row(Arow, acol, "Arow", ["acol"])
            self.make_row(Brow, m[:, 24:32, c], "Brow", mk)
            self.make_row(Grow, m[:, 40:48, c], "Grow", mk)

        for i in tiles:
            set_cond(0 if i >= 2 else 1)
            P.dma("sync", lambda e, i=i: e.dma_start(out=xt, in_=src_fn(i)), writes=["xt"])
            S(lambda e: e.activation(out=junk, in_=xt, func=AF.Square, accum_out=sm[:, 0:1]), ["xt"], ["junk", "sm0"])
            S(lambda e: e.activation(out=sm[:, 1:2], in_=sm[:, 0:1], func=AF.Sqrt, scale=1.0 / D, bias=EPS), ["sm0"], ["sm1"])
            V(lambda e: e.reciprocal(out=sm[:, 2:3], in_=sm[:, 1:2]), ["sm1"], ["sm2"])
            V(lambda e: e.tensor_scalar(out=xn, in0=xt, scalar1=sm[:, 2:3], scalar2=None, op0=ALU.mult), ["xt", "sm2"], ["h2"])
            V(lambda e: e.tensor_tensor(out=xn, in0=xn, in1=Arow, op=ALU.mult), ["h2", "Arow"], ["h2"])
            V(lambda e: e.tensor_tensor(out=h2, in0=xn, in1=Brow, op=ALU.add), ["h2", "Brow"], ["h2"])
            for k in range(8):
                T(lambda e, k=k: e.transpose(self.bank(k // 4, 128, (k % 4) * 128), h2[:, k * 128:(k + 1) * 128], self.ident),
                  ["h2", "cst"], [("ps", k // 4)])
            S(lambda e: e.copy(out=hT.rearrange("p a b -> p (a b)").rearrange("p (a b) -> p a b", b=512), in_=self.psum[:, 0:2, :]),
              [("ps", 0), ("ps", 1)], ["hT"])
            for j in range(16):
                for k in range(8):
                    T(lambda e, j=j, k=k: e.matmul(self.bank(2 + j // 4, 128, (j % 4) * 128), lhsT=wq[:, k, j * 128:(j + 1) * 128],
                                                  rhs=hT[:, k, :], start=(k == 0), stop=(k == 7)),
                      ["wq", "hT"], [("ps", 2 + j // 4)])
            S(lambda e: e.copy(out=qT.rearrange("p a b -> p (a b)").rearrange("p (a b) -> p a b", b=512), in_=self.psum[:, 2:6, :]),
              [("ps", b) for b in (2, 3, 4, 5)], ["qT"])
            sb_banks = (0, 1, 6, 7)
            for j in range(16):
                b = sb_banks[j // 4]
                T(lambda e, j=j, b=b: e.matmul(self.bank(b, 128, (j % 4) * 128), lhsT=qT[:, j, :], rhs=kT[:, j, :], start=True, stop=True),
                  ["qT", "kT"], [("ps", b)])
            for q4 in range(4):
                b = sb_banks[q4]
                S(lambda e, q4=q4, b=b: e.copy(out=sub[:, q4 * 4:(q4 + 1) * 4, :].rearrange("p a b -> p (a b)"), in_=self.bank(b)),
                  [("ps", b)], ["sub"])
            for j in range(16):
                V(lambda e, j=j: e.max(out=st[:, j, 0:8], in_=sub[:, j, :]), ["sub"], [("st0", j)])
            for j in range(16):
                V(lambda e, j=j: e.max_index(out=it[:, j, 0:8], in_max=st[:, j, 0:8], in_values=sub[:, j, :]), ["sub", ("st0", j)], [("it0", j)])
            for j in range(16):
                V(lambda e, j=j: e.match_replace(out=sub[:, j, :], in_to_replace=st[:, j, 0:8], in_values=sub[:, j, :], imm_value=NEG),
                  ["sub", ("st0", j), ("it0", j)], [("sub2", j)])
            for j in range(16):
                V(lambda e, j=j: e.max(out=st[:, j, 8:16], in_=sub[:, j, :]), [("sub2", j)], [("st1", j)])
            for j in range(16):
                V(lambda e, j=j: e.max_index(out=it[:, j, 8:16], in_max=st[:, j, 8:16], in_values=sub[:, j, :]), [("sub2", j), ("st1", j)], [("it1", j)])
            V(lambda e: e.tensor_copy(out=itf, in_=it), [("it0", j) for j in range(16)] + [("it1", j) for j in range(16)], ["itf"])
            st4 = st.rearrange("p (h a) k -> p h a k", a=2)
            if4 = itf.rearrange("p (h a) k -> p h a k", a=2)
            V(lambda e: e.tensor_scalar(out=if4[:, :, 0, :], in0=if4[:, :, 0, :], scalar1=128.0, scalar2=None, op0=ALU.mult), ["itf"], ["itf"])
            c4 = cand.rearrange("p h (a b) -> p h a b", b=16)
            ci4 = candi.rearrange("p h (a b) -> p h a b", b=16)
            V(lambda e: e.tensor_tensor(out=c4, in0=st4[:, :, 0, :].unsqueeze(3).to_broadcast([128, 8, 16, 16]),
                                        in1=st4[:, :, 1, :].unsqueeze(2).to_broadcast([128, 8, 16, 16]), op=ALU.add), [("st0", j) for j in range(16)] + [("st1", j) for j in range(16)] + [("it0", j) for j in range(16)] + [("it1", j) for j in range(16)] + [("sub2", j) for j in range(16)], ["sub"])
            V(lambda e: e.tensor_tensor(out=ci4, in0=if4[:, :, 0, :].unsqueeze(3).to_broadcast([128, 8, 16, 16]),
                                        in1=if4[:, :, 1, :].unsqueeze(2).to_broadcast([128, 8, 16, 16]), op=ALU.add), ["itf"], ["qT"])
            for h in range(8):
                V(lambda e, h=h: e.max(out=bs[:, h, 0:8], in_=cand[:, h, :]), ["sub"], [("bs0", h)])
            for h in range(8):
                V(lambda e, h=h: e.match_replace(out=cand2[:, h, :], in_to_replace=bs[:, h, 0:8], in_values=cand[:, h, :], imm_value=NEG),
                  ["sub", ("bs0", h), "junk", "acc1"], [("cand2", h)])
            for h in range(8):
                V(lambda e, h=h: e.max(out=bs[:, h, 8:16], in_=cand2[:, h, :]), [("cand2", h)], [("bs1", h)])
            for h in range(8):
                for k in range(16):
                    V(lambda e, h=h, k=k: e.scalar_tensor_tensor(out=junk[:, 0:256], in0=cand[:, h, :], scalar=bs[:, h, k:k + 1],
                                                               in1=candi[:, h, :], op0=ALU.is_equal, op1=ALU.mult,
                                                               accum_out=idxf[:, h * 16 + k:h * 16 + k + 1]),
                      ["sub", ("bs0", h), ("bs1", h), "qT"], [("idxf1", h * 16 + k)])
            V(lambda e: e.tensor_scalar(out=idxf, in0=idxf, scalar1=16383.0, scalar2=0.0, op0=ALU.min, op1=ALU.max), [("idxf1", q) for q in range(128)], ["idxf"])
            V(lambda e: e.tensor_copy(out=idx, in_=idxf), ["idxf"], ["idx"])
            V(lambda e: e.tensor_tensor(out=gate, in0=bs, in1=bs[:, :, 0:1].to_broadcast([128, 8, 16]), op=ALU.subtract), [("bs0", h) for h in range(8)] + [("bs1", h) for h in range(8)], ["gate"])
            S(lambda e: e.activation(out=gate, in_=gate, func=AF.Exp), ["gate"], ["gate"])
            V(lambda e: e.tensor_reduce(out=sm[:, 4:12], in_=gate, axis=AX.X, op=ALU.add), ["gate"], ["sm4"])
            V(lambda e: e.reciprocal(out=sm[:, 4:12], in_=sm[:, 4:12]), ["sm4"], ["sm4"])
            V(lambda e: e.tensor_tensor(out=gate, in0=gate, in1=sm[:, 4:12].unsqueeze(2).to_broadcast([128, 8, 16]), op=ALU.mult),
              ["gate", "sm4"], ["gate"])
            for k in range(128):
                s = gcount[0] % NS; gcount[0] += 1
                P.dma("gpsimd", lambda e, k=k, s=s: e.indirect_dma_start(
                    out=gs[s], out_offset=None, in_=utab,
                    in_offset=bass.IndirectOffsetOnAxis(ap=idx[:, k:k + 1], axis=0)), reads=["idx"], writes=[("gs", s)],
                    sem=("gs", l, s, gcount[0] // (NS * 250)))
                V(lambda e, k=k, s=s: e.scalar_tensor_tensor(out=junk, in0=gs[s], scalar=1.0, in1=h2, op0=ALU.mult, op1=ALU.mult,
                                                           accum_out=araw[:, k:k + 1]), [("gs", s), "h2"], [("araw", k)])
            S(lambda e: e.activation(out=act, in_=araw, func=AF.Gelu), [("araw", k) for k in range(128)], ["act"])
            V(lambda e: e.tensor_tensor(out=act, in0=act, in1=gate.rearrange("p a b -> p (a b)"), op=ALU.mult), ["act", "gate"], ["act"])
            for k in range(128):
                s = gcount[0] % NS; gcount[0] += 1
                P.dma("gpsimd", lambda e, k=k, s=s: e.indirect_dma_start(
                    out=gs[s], out_offset=None, in_=vtab,
                    in_offset=bass.IndirectOffsetOnAxis(ap=idx[:, k:k + 1], axis=0)), reads=["idx"], writes=[("gs", s)],
                    sem=("gs", l, s, gcount[0] // (NS * 250)))
                ab = acc if k % 2 == 0 else acc1
                akey = "junk" if k % 2 == 0 else "acc1"
                if k < 2:
                    V(lambda e, k=k, s=s, ab=ab: e.tensor_scalar(out=ab, in0=gs[s], scalar1=act[:, k:k + 1], scalar2=None, op0=ALU.mult),
                      [("gs", s), "act"], [akey])
                else:
                    V(lambda e, k=k, s=s, ab=ab: e.scalar_tensor_tensor(out=ab, in0=gs[s], scalar=act[:, k:k + 1], in1=ab,
                                                                      op0=ALU.mult, op1=ALU.add), [("gs", s), "act", akey], [akey])
            V(lambda e: e.tensor_tensor(out=acc, in0=acc, in1=acc1, op=ALU.add), ["junk", "acc1"], ["junk"])
            V(lambda e: e.tensor_tensor(out=acc, in0=acc, in1=Grow, op=ALU.mult), ["junk", "Grow"], ["junk"])
            V(lambda e: e.tensor_tensor(out=acc, in0=acc, in1=xt, op=ALU.add), ["junk", "xt"], ["junk"])
            if final_norm:
                S(lambda e: e.activation(out=h2, in_=acc, func=AF.Square, accum_out=sm[:, 12:13]), ["junk"], ["h2", "sm12"])
                S(lambda e: e.activation(out=sm[:, 13:14], in_=sm[:, 12:13], func=AF.Sqrt, scale=1.0 / D, bias=EPS), ["sm12"], ["sm13"])
                V(lambda e: e.reciprocal(out=sm[:, 14:15], in_=sm[:, 13:14]), ["sm13"], ["sm14"])
                V(lambda e: e.scalar_tensor_tensor(out=acc, in0=acc, scalar=sm[:, 14:15], in1=nfr, op0=ALU.mult, op1=ALU.mult),
                  ["junk", "sm14", "nfr"], ["junk"])
            P.dma("scalar", lambda e, i=i: e.dma_start(out=dst(i), in_=acc), reads=["junk"], writes=[self.key("dst")], sem="acc_st")
        P.barrier()
        A.off = mark


def fm(v, nch):
    return np.ascontiguousarray(np.asarray(v, np.float32).reshape(nch, 128).T)


def make_consts():
    c = np.zeros((128, 1024), np.float32)
    k = np.arange(128)
    c[:, 0:128] = np.eye(128, dtype=np.float32)
    c[:, 128:256] = 1.0
    c[:, 256:384] = (k[:, None] <= k[None, :])
    c[:, 384:512] = (k[:, None] > k[None, :])
    c[:, 512:640] = (k[:, None] >= k[None, :])
    c[:, 640:768] = (k[:, None] < k[None, :])
    return c


def prep_shared(inp):
    g = {}
    f32 = lambda a: np.ascontiguousarray(np.asarray(a, np.float32))
    g["consts"] = make_consts()
    g["ada_w"] = f32(inp["ada_w"])
    g["ada_bT"] = np.stack([fm(inp["ada_b"][l], 48) for l in range(2)])
    g["nmixT"] = np.stack([fm(inp["norm_mix_w"][l], 8) for l in range(2)])
    g["nffnT"] = np.stack([fm(inp["norm_ffn_w"][l], 8) for l in range(2)])
    g["nf_row"] = np.ascontiguousarray(np.broadcast_to(f32(inp["norm_f_w"])[None, :], (128, D)))
    g["ev_w_in"] = f32(inp["ev_w_in"][0]); g["ev_w_out"] = f32(inp["ev_w_out"][0])
    cwf = lambda w, nj: np.ascontiguousarray(f32(w).reshape(3, nj, 128).transpose(2, 1, 0).reshape(128, nj * 3))
    g["ssd_cwT"] = cwf(inp["ssd_conv_w"][0], 16); g["sc_cwT"] = cwf(inp["sc_conv_w"][0], 8)
    g["ssd_cbT"] = fm(inp["ssd_conv_b"][0], 16)
    brow = lambda v: np.ascontiguousarray(np.broadcast_to(f32(v).reshape(1, -1), (128, f32(v).size)))
    g["dtb_row"] = brow(inp["ssd_dt_bias"][0]); g["alog_row"] = brow(inp["ssd_a_log"][0])
    g["dsk_row"] = brow(inp["ssd_d"][0]); g["ssdnw_row"] = brow(inp["ssd_norm_w"][0])
    g["od_w_in"] = f32(inp["od_w_in"][0]); g["od_w_out"] = f32(inp["od_w_out"][0])
    lg = f32(inp["hg_lb_logits"])
    g["lblT"] = np.ascontiguousarray(lg.reshape(2, 2, 8, 128).transpose(3, 0, 1, 2).reshape(128, 32))
    g["hgnw_row"] = brow(inp["hg_norm_w"][0])
    smk = np.ones((128, 8, 2, 64), np.float32); smk[:, :, :, 0] = 0.0
    g["scanmask"] = smk.reshape(128, 1024)
    g["peer_wq"] = f32(inp["peer_wq"])
    pk = f32(inp["peer_keys"])
    g["keysT"] = np.ascontiguousarray(pk.transpose(0, 4, 1, 2, 3).reshape(2, 128, 2048))
    for l in range(2):
        g["peer_u%d" % l] = f32(inp["peer_u"][l]); g["peer_v%d" % l] = f32(inp["peer_v"][l])
    return g


def prep_core(inp, b):
    m = {}
    m["x"] = np.ascontiguousarray(np.asarray(inp["x"][b], np.float32))
    m["ctx"] = np.ascontiguousarray(np.asarray(inp["ctx"][b], np.float32))
    ct = np.zeros((128, 8, 2), np.float32)
    ct[:, :, 0] = fm(inp["c"][b], 8); ct[:, :, 1] = fm(inp["c_ctx"], 8)
    m["condT"] = ct.reshape(128, 16)
    return m


def build_full():
    kb = KB()
    kb.ssd_inputs()
    kb.hg_inputs()
    kb.setup_consts()
    kb.diag = kb.A.alloc([128])
    tiles = list(range(NT))
    xtile = lambda t: (lambda i: t[i * 128:(i + 1) * 128, :])
    kb.conv_tables()
    kb.mods_phase(0)
    src0 = kb.src_tile
    if hasattr(kb, "mixer0"):
        kb.mixer0(src0, xtile(kb.x1))
        src0 = xtile(kb.x1)
    kb.peer_phase(0, src0, xtile(kb.x2), tiles)
    kb.mods_phase(1)
    src1 = xtile(kb.x2)
    if hasattr(kb, "mixer1"):
        kb.mixer1(src1, xtile(kb.x3))
        src1 = xtile(kb.x3)
    kb.peer_phase(1, src1, lambda i: kb.out[(i - 2) * 128:(i - 1) * 128, :], tiles[2:], final_norm=True)
    kb.P.wait_all("sync", [])
    kb.P.build()
    return kb


def kernel(**inputs):
    inp = {k: np.asarray(v) for k, v in inputs.items()}
    kb = build_full()
    names = set()
    for a in kb.nc.allocations:
        if getattr(a, "kind", None) == "ExternalInput" and a.memorylocations:
            names.add(a.memorylocations[0].name)
    g = prep_shared(inp)
    in_maps = []
    for b in range(8):
        m = prep_core(inp, b)
        m.update(g)
        in_maps.append({k: v for k, v in m.items() if k in names})
    res = run_bass_kernel_spmd(kb.nc, in_maps, core_ids=list(range(8)))
    out = np.stack([np.asarray(r["out"], np.float32) for r in res.results], axis=0)
    return out


def _mix_helpers(self):
    P = self.P
    self.V = lambda fn, r, w: P.op("vector", fn, r, w)
    self.S = lambda fn, r, w: P.op("scalar", fn, r, w)
    self.T = lambda fn, r, w: P.op("tensor", fn, r, w)
    self.G = lambda fn, r, w: P.op("gpsimd", fn, r, w)


def load_w_bf16(self, dst, src, col0, ncols, key, stage):
    P = self.P
    piece = stage[0].shape[-1]
    n = 0
    for k in range(8):
        c = 0
        while c < ncols:
            w = min(piece, ncols - c)
            s = n % 2; n += 1
            P.dma("sync" if s == 0 else "scalar",
                  lambda e, k=k, c=c, w=w, s=s: e.dma_start(out=stage[s][:, 0:w], in_=src[k * 128:(k + 1) * 128, col0 + c:col0 + c + w]),
                  writes=[("stg", s)])
            P.op("gpsimd", lambda e, k=k, c=c, w=w, s=s: e.tensor_copy(out=dst[:, k, c:c + w], in_=stage[s][:, 0:w]),
                 reads=[("stg", s)], writes=[key])
            c += w


def norm_T(self, src_ap, Acol, Bcol, ckeys, dst, dkey, bufs):
    P, V, S, T = self.P, self.V, self.S, self.T
    xt, xnb, sm = bufs
    P.dma("sync", lambda e: e.dma_start(out=xt, in_=src_ap), writes=["n_xt"])
    S(lambda e: e.activation(out=xnb, in_=xt, func=AF.Square, accum_out=sm[:, 0:1]), ["n_xt"], ["n_xnb", "n_sm0"])
    S(lambda e: e.activation(out=sm[:, 1:2], in_=sm[:, 0:1], func=AF.Sqrt, scale=1.0 / D, bias=EPS), ["n_sm0"], ["n_sm1"])
    V(lambda e: e.reciprocal(out=sm[:, 2:3], in_=sm[:, 1:2]), ["n_sm1"], ["n_sm2"])
    V(lambda e: e.tensor_scalar(out=xnb, in0=xt, scalar1=sm[:, 2:3], scalar2=None, op0=ALU.mult), ["n_xt", "n_sm2"], ["n_xnb"])
    pb = self.psum[:, 7, :].bitcast(BF16)
    for k in range(8):
        T(lambda e, k=k: e.transpose(pb[:, k * 128:(k + 1) * 128], xnb[:, k * 128:(k + 1) * 128], self.identb),
          ["n_xnb", "identb"], [("ps", 7)])
    for k in range(8):
        S(lambda e, k=k: e.activation(out=dst[:, k, :], in_=pb[:, k * 128:(k + 1) * 128], func=AF.Identity,
                                      scale=Acol[:, k:k + 1], bias=Bcol[:, k:k + 1]), [("ps", 7)] + list(ckeys), [dkey])


def proj_fm(self, dst, dkey, w, wkey, col0, nj, hT, hkey, W):
    for jj in range(nj):
        b = 6 + (jj % 2)
        for k in range(8):
            self.T(lambda e, jj=jj, k=k, b=b: e.matmul(self.bank(b, W), lhsT=w[:, k, col0 + jj * 128:col0 + (jj + 1) * 128], rhs=hT[:, k, 0:W],
                                                       start=(k == 0), stop=(k == 7)), [wkey, hkey], [("ps", b)])
        self.S(lambda e, jj=jj, b=b: e.copy(out=dst[:, jj, 0:W], in_=self.bank(b, W)), [("ps", b)], [(dkey, jj)])


def conv3(self, dst, dkey, src, skeys, cw, cwkey, nj, W, rows):
    L = W // rows
    d3 = lambda j: dst[:, j, 0:W].rearrange("p (r l) -> p r l", l=L)
    s3 = lambda j: src[:, j, 0:W].rearrange("p (r l) -> p r l", l=L)
    for j in range(nj):
        self.V(lambda e, j=j: e.tensor_scalar(out=dst[:, j, 0:W], in0=src[:, j, 0:W], scalar1=cw[:, j, 1:2], scalar2=None, op0=ALU.mult),
               skeys(j) + [cwkey], [(dkey, j)])
    for j in range(nj):
        self.V(lambda e, j=j: e.scalar_tensor_tensor(out=d3(j)[:, :, 1:L], in0=s3(j)[:, :, 0:L - 1], scalar=cw[:, j, 0:1],
                                                     in1=d3(j)[:, :, 1:L], op0=ALU.mult, op1=ALU.add), skeys(j) + [cwkey, (dkey, j)], [(dkey, j)])
    for j in range(nj):
        self.V(lambda e, j=j: e.scalar_tensor_tensor(out=d3(j)[:, :, 0:L - 1], in0=s3(j)[:, :, 1:L], scalar=cw[:, j, 2:3],
                                                     in1=d3(j)[:, :, 0:L - 1], op0=ALU.mult, op1=ALU.add), skeys(j) + [cwkey, (dkey, j)], [(dkey, j)])


KB._mix_helpers = _mix_helpers
KB.load_w_bf16 = load_w_bf16
KB.norm_T = norm_T
KB.proj_fm = proj_fm
KB.conv3 = conv3


def ssd_inputs(self):
    nc = self.nc
    di = lambda n, s, dt=F32: nc.dram_tensor(n, list(s), dt, kind="ExternalInput")
    self.ev_w_in = di("ev_w_in", [D, 6176]); self.ev_w_out = di("ev_w_out", [2048, D])
    self.ssd_cwT = di("ssd_cwT", [128, 48]); self.ssd_cbT = di("ssd_cbT", [128, 16]); self.sc_cwT = di("sc_cwT", [128, 24])
    self.dtb_row = di("dtb_row", [128, 32]); self.alog_row = di("alog_row", [128, 32])
    self.dsk_row = di("dsk_row", [128, 16]); self.ssdnw_row = di("ssdnw_row", [128, D])
    self.yscr = nc.dram_tensor("yscr", [NT * 128, D], F32, kind=self.dbg.get("yscr", "Internal"))
    self.xsscr = nc.dram_tensor("xsscr", [NT * 128, D], BF16, kind="Internal")


def mix_cols(self, l, c, nT, Acol, key):
    m = self.modsFM[l]
    self.V(lambda e: e.scalar_tensor_tensor(out=Acol, in0=m[:, 8:16, c], scalar=1.0, in1=nT, op0=ALU.add, op1=ALU.mult),
           [("mods", l), "nT"], [key])
    return m[:, 0:8, c]


def ssd_scan(self, d, src_fn):
    P, A, V, S, T = self.P, self.A, self.V, self.S, self.T
    mark = A.off
    l = 0
    wx = A.alloc([8, 2080], BF16)
    stage = [A.alloc([2080]) for _ in range(2)]
    self.load_w_bf16(wx, self.ev_w_in, 4096, 2080, "wx", stage)
    cw = A.alloc([16, 3]); cb = A.alloc([16]); nT = A.alloc([8])
    dtb = A.alloc([32]); arow = A.alloc([32])
    for dst_, src_, k_ in ((cw, self.ssd_cwT.ap().rearrange("p (a b) -> p a b", b=3), "cw"), (cb, self.ssd_cbT.ap(), "cb"),
                           (nT, self.nmixT[l], "nT"), (dtb, self.dtb_row.ap(), "dtb"), (arow, self.alog_row.ap(), "arow")):
        P.dma("sync", lambda e, dst_=dst_, src_=src_: e.dma_start(out=dst_, in_=src_), writes=[k_], sem="misc")
    S(lambda e: e.activation(out=arow, in_=arow, func=AF.Exp), ["arow"], ["arow"])
    V(lambda e: e.tensor_scalar(out=arow, in0=arow, scalar1=-1.0, scalar2=None, op0=ALU.mult), ["arow"], ["arow"])
    Acol = [A.alloc([8]) for _ in range(2)]
    Bcol = [self.mix_cols(l, c, nT, Acol[c], ("Acol", c)) for c in range(2)]
    xt = A.alloc([D]); xnb = A.alloc([D], BF16); sm = A.alloc([4])
    hTu = A.alloc([8, 256], BF16)
    praw = A.alloc([16, 256]); cv = A.alloc([16, 256]); xbcT = A.alloc([16, 256], BF16)
    dt = A.alloc([2, 16]); dta = A.alloc([2, 16]); e48 = A.alloc([48]); wend = A.alloc([16])
    R = A.alloc([16, 128]); cbm = A.alloc([4, 128]); Eq = A.alloc([4, 128]); attT = A.alloc([16, 128], BF16)
    xs_tm = A.alloc([D], BF16); bm_tm = A.alloc([512], BF16); xw = A.alloc([D], BF16)
    ych = A.alloc([D]); ypv = A.alloc([D]); St = A.alloc([D]); Sb = A.alloc([D], BF16)
    V(lambda e: e.memset(St, 0.0), [], ["St"])
    V(lambda e: e.memset(Sb, 0.0), [], ["Sb"])
    cst = self.cst
    if d == 0:
        Bm = cst[:, 256:384]; Am = cst[:, 384:512]; Mk = cst[:, 256:384]
    else:
        Bm = cst[:, 512:640]; Am = cst[:, 640:768]; Mk = cst[:, 512:640]
    units = [(0, 256, 1)] + [(i, 128, 0) for i in range(2, NT)]
    if d == 1:
        units = [units[0]] + units[1:][::-1]
    pbf = lambda b: self.psum[:, b, :].bitcast(BF16)
    def unit_body(t0, W, c):
        nsub = W // 128
        for sub in range(nsub):
            self.norm_T(src_fn(t0 + sub), Acol[c], Bcol[c], [("Acol", c), ("mods", l)], hTu[:, :, sub * 128:(sub + 1) * 128], "hTu", (xt, xnb, sm))
        self.proj_fm(praw, "praw", wx, "wx", 0, 16, hTu, "hTu", W)
        self.conv3(cv, "cv", praw, lambda j: [("praw", j)], cw, "cw", 16, W, 1 if c == 1 else W // 64)
        for j in range(16):
            S(lambda e, j=j: e.activation(out=xbcT[:, j, 0:W], in_=cv[:, j, 0:W], func=AF.Silu, bias=cb[:, j:j + 1]), [("cv", j), "cb"], ["xbcT"])
        for sub in range(nsub):
            for k in range(8):
                T(lambda e, k=k, sub=sub: e.matmul(self.bank(6, 32), lhsT=hTu[:, k, sub * 128:(sub + 1) * 128], rhs=wx[:, k, 2048:2080],
                                                   start=(k == 0), stop=(k == 7)), ["hTu", "wx"], [("ps", 6)])
            V(lambda e, sub=sub: e.tensor_tensor(out=dt[:, sub, :], in0=self.bank(6, 16, d * 16), in1=dtb[:, d * 16:(d + 1) * 16], op=ALU.add),
              [("ps", 6), "dtb"], ["dt"])
        S(lambda e: e.activation(out=dt[:, 0:nsub, :], in_=dt[:, 0:nsub, :], func=AF.Exp), ["dt"], ["dt"])
        S(lambda e: e.activation(out=dt[:, 0:nsub, :], in_=dt[:, 0:nsub, :], func=AF.Ln, bias=1.0), ["dt"], ["dt"])
        V(lambda e: e.tensor_tensor(out=dta[:, 0:nsub, :], in0=dt[:, 0:nsub, :],
                                    in1=arow[:, d * 16:(d + 1) * 16].unsqueeze(1).to_broadcast([128, nsub, 16]), op=ALU.mult),
          ["dt", "arow"], ["dta"])
        chunks = list(range(nsub))
        if d == 1:
            chunks = chunks[::-1]
        def chunk_body(ch):
            tile = t0 + ch
            c0 = ch * 128
            cols = slice(c0, c0 + 128)
            for j in range(8):
                T(lambda e, j=j: e.transpose(pbf(7)[:, j * 128:(j + 1) * 128], xbcT[:, j, cols], self.identb), ["xbcT", "identb"], [("ps", 7)])
            S(lambda e: e.copy(out=xs_tm, in_=pbf(7)), [("ps", 7)], ["xs_tm"])
            for j in range(4):
                T(lambda e, j=j: e.transpose(pbf(6)[:, j * 128:(j + 1) * 128], xbcT[:, 8 + j, cols], self.identb), ["xbcT", "identb"], [("ps", 6)])
            S(lambda e: e.copy(out=bm_tm, in_=pbf(6)[:, 0:512]), [("ps", 6)], ["bm_tm"])
            for g in range(4):
                T(lambda e, g=g: e.matmul(self.bank(7, 128, g * 128), lhsT=xbcT[:, 8 + g, cols], rhs=xbcT[:, 12 + g, cols], start=True, stop=True),
                  ["xbcT"], [("ps", 7)])
            V(lambda e: e.tensor_tensor(out=cbm, in0=self.bank(7).rearrange("p (g t) -> p g t", t=128),
                                        in1=Mk.unsqueeze(1).to_broadcast([128, 4, 128]), op=ALU.mult), [("ps", 7), "cst"], ["cbm"])
            dta_c = dta[:, ch, :]; dt_c = dt[:, ch, :]
            for n_, lh in enumerate((Bm, Am, self.ones)):
                T(lambda e, n_=n_, lh=lh: e.matmul(self.bank(6, 16, n_ * 16), lhsT=lh, rhs=dta_c, start=True, stop=True), ["dta", "cst"], [("ps", 6)])
            S(lambda e: e.activation(out=e48, in_=self.bank(6, 48), func=AF.Exp), [("ps", 6)], ["e48"])
            V(lambda e: e.tensor_tensor(out=wend, in0=e48[:, 16:32], in1=dt_c, op=ALU.mult), ["e48", "dt"], ["wend"])
            V(lambda e: e.tensor_tensor(out=R, in0=dta_c.unsqueeze(2).to_broadcast([128, 16, 128]),
                                        in1=Bm.unsqueeze(1).to_broadcast([128, 16, 128]), op=ALU.mult), ["dta", "cst"], ["R"])
            for q in range(4):
                b = q % 2
                T(lambda e, q=q, b=b: e.matmul(self.bank(b), lhsT=Am, rhs=R[:, q * 4:(q + 1) * 4, :].rearrange("p a b -> p (a b)"), start=True, stop=True),
                  ["R", "cst"], [("ps", b)])
                S(lambda e, b=b: e.activation(out=Eq.rearrange("p a b -> p (a b)"), in_=self.bank(b), func=AF.Exp), [("ps", b)], ["Eq"])
                V(lambda e, q=q: e.tensor_tensor(out=Eq, in0=Eq, in1=cbm[:, q:q + 1, :].to_broadcast([128, 4, 128]), op=ALU.mult), ["Eq", "cbm"], ["Eq"])
                V(lambda e, q=q: e.tensor_tensor(out=attT[:, q * 4:(q + 1) * 4, :], in0=Eq,
                                                 in1=dt_c[:, q * 4:(q + 1) * 4].unsqueeze(2).to_broadcast([128, 4, 128]), op=ALU.mult),
                  ["Eq", "dt"], ["attT"])
            for h in range(16):
                T(lambda e, h=h: e.matmul(self.bank(2 + h // 8, 64, (h % 8) * 64), lhsT=attT[:, h, :], rhs=xs_tm[:, h * 64:(h + 1) * 64], start=True, stop=True),
                  ["attT", "xs_tm"], [("ps", 2 + h // 8)])
            for g in range(4):
                T(lambda e, g=g: e.matmul(self.bank(4 + g // 2, 256, (g % 2) * 256), lhsT=xbcT[:, 12 + g, cols], rhs=Sb[:, g * 256:(g + 1) * 256], start=True, stop=True),
                  ["xbcT", "Sb"], [("ps", 4 + g // 2)])
            V(lambda e: e.tensor_tensor(out=ych.rearrange("p (h q) -> p h q", q=64), in0=self.psum[:, 4:6, :].rearrange("p a (h q) -> p (a h) q", q=64),
                                        in1=e48[:, 0:16].unsqueeze(2).to_broadcast([128, 16, 64]), op=ALU.mult), [("ps", 4), ("ps", 5), "e48"], ["ych"])
            V(lambda e: e.tensor_tensor(out=ych.rearrange("p (a b) -> p a b", b=512), in0=ych.rearrange("p (a b) -> p a b", b=512), in1=self.psum[:, 2:4, :], op=ALU.add),
              ["ych", ("ps", 2), ("ps", 3)], ["ych"])
            ydst = self.yscr[tile * 128:(tile + 1) * 128, :]
            if d == 1:
                P.dma("sync", lambda e, ydst=ydst: e.dma_start(out=ypv, in_=ydst), reads=[("yscr", tile)], writes=["ypv"])
                V(lambda e: e.tensor_tensor(out=ych, in0=ych, in1=ypv, op=ALU.add), ["ych", "ypv"], ["ych"])
                P.dma("scalar", lambda e, tile=tile: e.dma_start(out=self.xsscr[tile * 128:(tile + 1) * 128, :], in_=xs_tm), reads=["xs_tm"],
                      writes=[("xsscr", tile)], sem="xs_st")
            P.dma("scalar", lambda e, ydst=ydst: e.dma_start(out=ydst, in_=ych), reads=["ych"], writes=[("yscr", tile)], sem="y_st")
            V(lambda e: e.tensor_tensor(out=xw.rearrange("p (h q) -> p h q", q=64), in0=xs_tm.rearrange("p (h q) -> p h q", q=64),
                                        in1=wend.unsqueeze(2).to_broadcast([128, 16, 64]), op=ALU.mult), ["xs_tm", "wend"], ["xw"])
            for g in range(4):
                T(lambda e, g=g: e.matmul(self.bank(4 + g // 2, 256, (g % 2) * 256), lhsT=bm_tm[:, g * 128:(g + 1) * 128], rhs=xw[:, g * 256:(g + 1) * 256], start=True, stop=True),
                  ["bm_tm", "xw"], [("ps", 4 + g // 2)])
            V(lambda e: e.tensor_tensor(out=St.rearrange("p (h q) -> p h q", q=64), in0=St.rearrange("p (h q) -> p h q", q=64),
                                        in1=e48[:, 32:48].unsqueeze(2).to_broadcast([128, 16, 64]), op=ALU.mult), ["St", "e48"], ["St"])
            V(lambda e: e.tensor_tensor(out=St.rearrange("p (a b) -> p a b", b=512), in0=St.rearrange("p (a b) -> p a b", b=512), in1=self.psum[:, 4:6, :], op=ALU.add),
              ["St", ("ps", 4), ("ps", 5)], ["St"])
            S(lambda e: e.copy(out=Sb, in_=St), ["St"], ["Sb"])
        for ch in chunks:
            chunk_body(ch)
    for u in units:
        unit_body(*u)
    P.barrier()
    A.off = mark


KB.ssd_inputs = ssd_inputs
KB.mix_cols = mix_cols
KB.ssd_scan = ssd_scan


def ssd_finish(self, src_fn, dst_fn):
    P, A, V, S, T = self.P, self.A, self.V, self.S, self.T
    mark = A.off
    l = 0
    wc = A.alloc([8, 4096], BF16)
    wo = A.alloc([16, D], BF16)
    stage = [A.alloc([2048]) for _ in range(2)]
    self.load_w_bf16(wc, self.ev_w_in, 0, 4096, "wc", stage)
    n = 0
    for k in range(16):
        s = n % 2; n += 1
        P.dma("sync" if s == 0 else "scalar", lambda e, k=k, s=s: e.dma_start(out=stage[s][:, 0:D], in_=self.ev_w_out[k * 128:(k + 1) * 128, :]),
              writes=[("stg", s)])
        P.op("gpsimd", lambda e, k=k, s=s: e.tensor_copy(out=wo[:, k, :], in_=stage[s][:, 0:D]), reads=[("stg", s)], writes=["wo"])
    A.off -= 2 * 2048
    P.barrier()
    cw = A.alloc([8, 3]); nT = A.alloc([8]); dsk = A.alloc([16]); nwr = A.alloc([D])
    for dst_, src_, k_ in ((cw, self.sc_cwT.ap().rearrange("p (a b) -> p a b", b=3), "cw"), (nT, self.nmixT[l], "nT"),
                           (dsk, self.dsk_row.ap(), "dsk"), (nwr, self.ssdnw_row.ap(), "nwr")):
        P.dma("sync", lambda e, dst_=dst_, src_=src_: e.dma_start(out=dst_, in_=src_), writes=[k_], sem="misc")
    Acol = [A.alloc([8]) for _ in range(2)]
    Bcol = [self.mix_cols(l, c, nT, Acol[c], ("Acol", c)) for c in range(2)]
    Grow = A.alloc([D])
    xt = A.alloc([D]); xnb = A.alloc([D], BF16); sm = A.alloc([16])
    hTu = A.alloc([8, 256], BF16)
    praw = A.alloc([16, 256]); cv = A.alloc([8, 256]); yaT = A.alloc([8, 256], BF16)
    zs = A.alloc([D]); ysum = A.alloc([D]); xsb = A.alloc([D], BF16); yb = A.alloc([D]); ybn = A.alloc([D], BF16)
    ybT = A.alloc([8, 128], BF16); xo = zs
    pbf = lambda b: self.psum[:, b, :].bitcast(BF16)
    units = [(0, 256, 1)] + [(i, 128, 0) for i in range(2, NT)]
    cur_c = [None]
    def unit_body(t0, W, c):
        nsub = W // 128
        if c != cur_c[0]:
            cur_c[0] = c
            self.make_row(Grow, self.modsFM[l][:, 16:24, c], "Grow", [("mods", l)], banks=(4, 5))
        for sub in range(nsub):
            self.norm_T(src_fn(t0 + sub), Acol[c], Bcol[c], [("Acol", c), ("mods", l)], hTu[:, :, sub * 128:(sub + 1) * 128], "hTu", (xt, xnb, sm))
        self.proj_fm(praw, "praw", wc, "wc", 0, 8, hTu, "hTu", W)
        self.proj_fm(praw[:, 8:16, :], "praw2", wc, "wc", 2048, 8, hTu, "hTu", W)
        pk1 = [("praw", j) for j in range(8)]; pk2 = [("praw2", j) for j in range(8)]
        V(lambda e: e.tensor_tensor(out=praw[:, 0:8, 0:W], in0=praw[:, 0:8, 0:W], in1=praw[:, 8:16, 0:W], op=ALU.mult), pk1 + pk2, ["prawm"])
        self.conv3(cv, "cv", praw, lambda j: ["prawm", ("praw", j)], cw, "cw", 8, W, 1 if c == 1 else W // 64)
        self.proj_fm(praw[:, 8:16, :], "praw2", wc, "wc", 1024, 8, hTu, "hTu", W)
        V(lambda e: e.tensor_tensor(out=yaT[:, :, 0:W], in0=cv[:, :, 0:W], in1=praw[:, 8:16, 0:W], op=ALU.mult), [("cv", j) for j in range(8)] + pk2, ["yaT"])
        def sub_body(sub):
            tile = t0 + sub
            cols = slice(sub * 128, (sub + 1) * 128)
            for n2 in range(2):
                for k in range(8):
                    T(lambda e, n2=n2, k=k: e.matmul(self.bank(n2), lhsT=hTu[:, k, cols], rhs=wc[:, k, 3072 + n2 * 512:3072 + (n2 + 1) * 512],
                                                     start=(k == 0), stop=(k == 7)), ["hTu", "wc"], [("ps", n2)])
            S(lambda e: e.activation(out=zs.rearrange("p (a b) -> p a b", b=512), in_=self.psum[:, 0:2, :], func=AF.Silu), [("ps", 0), ("ps", 1)], ["zs"])
            P.dma("sync", lambda e, tile=tile: e.dma_start(out=ysum, in_=self.yscr[tile * 128:(tile + 1) * 128, :]), reads=[("yscr", tile)], writes=["ysum"])
            P.dma("sync", lambda e, tile=tile: e.dma_start(out=xsb, in_=self.xsscr[tile * 128:(tile + 1) * 128, :]), reads=[("xsscr", tile)], writes=["xsb"])
            V(lambda e: e.tensor_tensor(out=yb.rearrange("p (h q) -> p h q", q=64), in0=xsb.rearrange("p (h q) -> p h q", q=64),
                                        in1=dsk.unsqueeze(2).to_broadcast([128, 16, 64]), op=ALU.mult), ["xsb", "dsk"], ["yb"])
            V(lambda e: e.tensor_tensor(out=yb, in0=yb, in1=ysum, op=ALU.add), ["yb", "ysum"], ["yb"])
            V(lambda e: e.tensor_tensor(out=yb, in0=yb, in1=zs, op=ALU.mult), ["yb", "zs"], ["yb"])
            for g in range(4):
                S(lambda e, g=g: e.activation(out=ysum[:, g * 256:(g + 1) * 256], in_=yb[:, g * 256:(g + 1) * 256], func=AF.Square, accum_out=sm[:, 4 + g:5 + g]),
                  ["yb"], ["ysum", "sm4"])
            S(lambda e: e.activation(out=sm[:, 8:12], in_=sm[:, 4:8], func=AF.Sqrt, scale=1.0 / 256, bias=EPS), ["sm4"], ["sm8"])
            V(lambda e: e.reciprocal(out=sm[:, 8:12], in_=sm[:, 8:12]), ["sm8"], ["sm8"])
            V(lambda e: e.tensor_tensor(out=yb.rearrange("p (g q) -> p g q", q=256), in0=yb.rearrange("p (g q) -> p g q", q=256),
                                        in1=sm[:, 8:12].unsqueeze(2).to_broadcast([128, 4, 256]), op=ALU.mult), ["yb", "sm8"], ["yb"])
            V(lambda e: e.tensor_tensor(out=ybn, in0=yb, in1=nwr, op=ALU.mult), ["yb", "nwr"], ["ybn"])
            for k in range(8):
                T(lambda e, k=k: e.transpose(pbf(7)[:, k * 128:(k + 1) * 128], ybn[:, k * 128:(k + 1) * 128], self.identb), ["ybn", "identb"], [("ps", 7)])
            S(lambda e: e.copy(out=ybT.rearrange("p a b -> p (a b)"), in_=pbf(7)), [("ps", 7)], ["ybT"])
            for n2 in range(2):
                for kc in range(16):
                    lh = yaT[:, kc, cols] if kc < 8 else ybT[:, kc - 8, :]
                    T(lambda e, n2=n2, kc=kc, lh=lh: e.matmul(self.bank(2 + n2), lhsT=lh, rhs=wo[:, kc, n2 * 512:(n2 + 1) * 512],
                                                              start=(kc == 0), stop=(kc == 15)), ["yaT", "ybT", "wo"], [("ps", 2 + n2)])
            V(lambda e: e.tensor_tensor(out=xo.rearrange("p (a b) -> p a b", b=512), in0=self.psum[:, 2:4, :], in1=Grow.rearrange("p (a b) -> p a b", b=512), op=ALU.mult),
              [("ps", 2), ("ps", 3), "Grow"], ["zs"])
            P.dma("sync", lambda e, tile=tile: e.dma_start(out=ysum, in_=src_fn(tile)), writes=["ysum"])
            V(lambda e: e.tensor_tensor(out=xo, in0=xo, in1=ysum, op=ALU.add), ["zs", "ysum"], ["zs"])
            P.dma("scalar", lambda e, tile=tile: e.dma_start(out=dst_fn(tile), in_=xo), reads=["zs"], writes=[self.key("x1")], sem="xo_st")
        for sub in range(nsub):
            sub_body(sub)
    for u in units:
        unit_body(*u)
    P.barrier()
    A.off = mark


def mixer0(self, src_fn, dst_fn):
    self._mix_helpers()
    self.ssd_scan(0, src_fn)
    self.ssd_scan(1, src_fn)
    self.ssd_finish(src_fn, dst_fn)


KB.ssd_finish = ssd_finish
KB.mixer0 = mixer0


def hg_inputs(self):
    nc = self.nc
    di = lambda n, s, dt=F32: nc.dram_tensor(n, list(s), dt, kind="ExternalInput")
    self.od_w_in = di("od_w_in", [D, 5120]); self.od_w_out = di("od_w_out", [D, D])
    self.lblT = di("lblT", [128, 32])
    self.hgnw_row = di("hgnw_row", [128, D])
    self.scanmask = di("scanmask", [128, 1024])


def hg_pass(self, d, src_fn, dst_fn):
    P, A, V, S, T = self.P, self.A, self.V, self.S, self.T
    mark = A.off
    l = 1
    fin = (d == 1)
    ncol = 5 if fin else 3
    w = A.alloc([8, ncol * 1024], BF16)
    stage = [A.alloc([1024]) for _ in range(2)]
    colsrc = [0, 1024 + d * 1024, 3072, 4096]
    for n_, c0 in enumerate(colsrc[:4 if fin else 3]):
        self.load_w_bf16(w[:, :, n_ * 1024:(n_ + 1) * 1024], self.od_w_in, c0, 1024, "w", stage)
    if fin:
        n = 0
        for k in range(8):
            s = n % 2; n += 1
            P.dma("sync" if s == 0 else "scalar", lambda e, k=k, s=s: e.dma_start(out=stage[s], in_=self.od_w_out[k * 128:(k + 1) * 128, :]), writes=[("stg", s)])
            P.op("gpsimd", lambda e, k=k, s=s: e.tensor_copy(out=w[:, k, 4096:5120], in_=stage[s]), reads=[("stg", s)], writes=["w"])
    P.barrier()
    A.off -= 2 * 1024
    nT = A.alloc([8]); lbl = A.alloc([32]); lb = A.alloc([8]); oml = A.alloc([8]); smask = A.alloc([D])
    nwr = A.alloc([D])
    for dst_, src_, k_ in ((nT, self.nmixT[l], "nT"), (lbl, self.lblT.ap(), "lbl"), (smask, self.scanmask.ap(), "smask"), (nwr, self.hgnw_row.ap(), "nwr")):
        P.dma("sync", lambda e, dst_=dst_, src_=src_: e.dma_start(out=dst_, in_=src_), writes=[k_], sem="misc")
    V(lambda e: e.tensor_tensor(out=lb, in0=lbl[:, 16 + d * 8:24 + d * 8], in1=lbl[:, d * 8:d * 8 + 8], op=ALU.subtract), ["lbl"], ["lb"])
    S(lambda e: e.activation(out=lb, in_=lb, func=AF.Sigmoid), ["lb"], ["lb"])
    V(lambda e: e.tensor_scalar(out=oml, in0=lb, scalar1=-1.0, scalar2=1.0, op0=ALU.mult, op1=ALU.add), ["lb"], ["oml"])
    Acol = [A.alloc([8]) for _ in range(2)]
    Bcol = [self.mix_cols(l, c, nT, Acol[c], ("Acol", c)) for c in range(2)]
    xt = A.alloc([D]); xnb = A.alloc([D], BF16); sm = A.alloc([32])
    hT = A.alloc([8, 128], BF16)
    praw = A.alloc([8, 128]); qF = A.alloc([8, 128]); fF = A.alloc([8, 128]); gl = A.alloc([8, 128]); GC = A.alloc([8, 128])
    ex = A.alloc([8, 128])
    qg = A.alloc([8, 128], BF16); kg = A.alloc([8, 128], BF16); kend = A.alloc([8, 128], BF16); iF = A.alloc([8, 128], BF16)
    eGE = A.alloc([8, 2]); GE = A.alloc([8, 2])
    v_tm = A.alloc([D], BF16); kend_tm = A.alloc([D], BF16); attm = A.alloc([8, 64], BF16)
    och = A.alloc([D]); St = A.alloc([8, 128]); Sb = A.alloc([8, 128], BF16)
    V(lambda e: e.memset(St, 0.0), [], ["St"])
    V(lambda e: e.memset(Sb, 0.0), [], ["Sb"])
    if fin:
        opv = A.alloc([D]); gs = A.alloc([D]); onb = A.alloc([D], BF16); onT = A.alloc([8, 64], BF16)
        Grow = A.alloc([D])
        self.make_row(Grow, self.modsFM[l][:, 16:24, 0], "Grow", [("mods", l)], banks=(4, 5))
    cst = self.cst
    Mk = cst[0:64, 256:320] if d == 0 else cst[0:64, 512:576]
    pbf = lambda b: self.psum[:, b, :].bitcast(BF16)
    tiles = [0, 1] + list(range(2, NT))
    if d == 1:
        tiles = [1, 0] + list(range(NT - 1, 1, -1))
    flat = lambda a: a.rearrange("p a b -> p (a b)")
    bc8 = lambda col: col.unsqueeze(2).to_broadcast([128, 8, 128])

    def tile_body(i):
        c = 1 if i < 2 else 0
        need_out = (i >= 2)
        self.norm_T(src_fn(i), Acol[c], Bcol[c], [("Acol", c), ("mods", l)], hT, "hT", (xt, xnb, sm))
        if need_out:
            self.proj_fm(praw, "praw", w, "w", 0, 8, hT, "hT", 128)
            S(lambda e: e.activation(out=qF, in_=praw, func=AF.Silu), [("praw", j) for j in range(8)], ["qF"])
        self.proj_fm(praw, "praw", w, "w", 1024, 8, hT, "hT", 128)
        S(lambda e: e.activation(out=fF, in_=praw, func=AF.Sigmoid), [("praw", j) for j in range(8)], ["fF"])
        V(lambda e: e.tensor_tensor(out=fF, in0=fF, in1=bc8(oml), op=ALU.mult), ["fF", "oml"], ["fF"])
        V(lambda e: e.tensor_tensor(out=fF, in0=fF, in1=bc8(lb), op=ALU.add), ["fF", "lb"], ["fF"])
        S(lambda e: e.activation(out=gl, in_=fF, func=AF.Ln), ["fF"], ["gl"])
        V(lambda e: e.tensor_scalar(out=fF, in0=fF, scalar1=-1.0, scalar2=1.0, op0=ALU.mult, op1=ALU.add), ["fF"], ["fF"])
        V(lambda e: e.tensor_tensor_scan(out=flat(GC), data0=smask, data1=flat(gl), initial=0.0, op0=ALU.mult, op1=ALU.add), ["smask", "gl"], ["GC"])
        GC4 = GC.rearrange("p j (c t) -> p j c t", t=64)
        V(lambda e: e.tensor_copy(out=GE, in_=GC4[:, :, :, 63]), ["GC"], ["GE"])
        if d == 1:
            V(lambda e: e.tensor_tensor(out=GC, in0=gl, in1=GC, op=ALU.subtract), ["gl", "GC"], ["GC"])
            V(lambda e: e.tensor_tensor(out=GC4, in0=GC4, in1=GE.unsqueeze(3).to_broadcast([128, 8, 2, 64]), op=ALU.add), ["GC", "GE"], ["GC"])
        S(lambda e: e.activation(out=eGE, in_=GE, func=AF.Exp), ["GE"], ["eGE"])
        if need_out:
            S(lambda e: e.activation(out=ex, in_=GC, func=AF.Exp), ["GC"], ["ex"])
            V(lambda e: e.scalar_tensor_tensor(out=flat(qg), in0=flat(qF), scalar=float(128 ** -0.5), in1=flat(ex), op0=ALU.mult, op1=ALU.mult), ["qF", "ex"], ["qg"])
            S(lambda e: e.activation(out=ex, in_=GC, func=AF.Exp, scale=-1.0), ["GC"], ["ex"])
            V(lambda e: e.tensor_tensor(out=kg, in0=fF, in1=ex, op=ALU.mult), ["fF", "ex"], ["kg"])
        V(lambda e: e.tensor_tensor(out=GC4, in0=GE.unsqueeze(3).to_broadcast([128, 8, 2, 64]), in1=GC4, op=ALU.subtract), ["GC", "GE"], ["GC"])
        S(lambda e: e.activation(out=ex, in_=GC, func=AF.Exp), ["GC"], ["ex"])
        V(lambda e: e.tensor_tensor(out=kend, in0=fF, in1=ex, op=ALU.mult), ["fF", "ex"], ["kend"])
        self.proj_fm(praw, "praw", w, "w", 2048, 8, hT, "hT", 128)
        V(lambda e: e.tensor_copy(out=iF, in_=praw), [("praw", j) for j in range(8)], ["iF"])
        subs = [0, 1] if d == 0 else [1, 0]

        def sub_body(sc):
            cols = slice(sc * 64, sc * 64 + 64)
            rows = slice(i * 128 + sc * 64, i * 128 + sc * 64 + 64)
            for j in range(8):
                T(lambda e, j=j: e.transpose(pbf(7)[0:64, j * 128:(j + 1) * 128], iF[:, j, cols], self.identb), ["iF", "identb"], [("ps", 7)])
            S(lambda e: e.copy(out=v_tm[0:64, :], in_=pbf(7)[0:64, :]), [("ps", 7)], ["v_tm"])
            for j in range(8):
                T(lambda e, j=j: e.transpose(pbf(6)[0:64, j * 128:(j + 1) * 128], kend[:, j, cols], self.identb), ["kend", "identb"], [("ps", 6)])
            S(lambda e: e.copy(out=kend_tm[0:64, :], in_=pbf(6)[0:64, :]), [("ps", 6)], ["kend_tm"])
            if need_out:
                for j in range(8):
                    T(lambda e, j=j: e.matmul(self.psum[0:64, 0, j * 64:(j + 1) * 64], lhsT=kg[:, j, cols], rhs=qg[:, j, cols], start=True, stop=True),
                      ["kg", "qg"], [("ps", 0)])
                V(lambda e: e.tensor_tensor(out=attm[0:64], in0=self.psum[0:64, 0, :].rearrange("p (j t) -> p j t", t=64),
                                            in1=Mk.unsqueeze(1).to_broadcast([64, 8, 64]), op=ALU.mult), [("ps", 0), "cst"], ["attm"])
                for j in range(8):
                    ob = self.psum[0:64, 2 + j // 4, (j % 4) * 128:(j % 4 + 1) * 128]
                    T(lambda e, j=j, ob=ob: e.matmul(ob, lhsT=attm[0:64, j, :], rhs=v_tm[0:64, j * 128:(j + 1) * 128], start=True, stop=False),
                      ["attm", "v_tm"], [("ps", 2 + j // 4)])
                    T(lambda e, j=j, ob=ob: e.matmul(ob, lhsT=qg[:, j, cols], rhs=Sb[:, j, :], start=False, stop=True),
                      ["qg", "Sb"], [("ps", 2 + j // 4)])
                ydst = self.yscr[rows, :]
                if not fin:
                    S(lambda e: e.copy(out=och[0:64].rearrange("p (a b) -> p a b", b=512), in_=self.psum[0:64, 2:4, :]), [("ps", 2), ("ps", 3)], ["och"])
                    P.dma("scalar", lambda e: e.dma_start(out=ydst, in_=och[0:64]), reads=["och"], writes=[("yscr", i, sc)], sem="y_st")
                else:
                    P.dma("sync", lambda e: e.dma_start(out=opv[0:64], in_=ydst), reads=[("yscr", i, sc)], writes=["opv"])
                    V(lambda e: e.tensor_tensor(out=och[0:64].rearrange("p (a b) -> p a b", b=512), in0=self.psum[0:64, 2:4, :],
                                                in1=opv[0:64].rearrange("p (a b) -> p a b", b=512), op=ALU.add), [("ps", 2), ("ps", 3), "opv"], ["och"])
            for j in range(8):
                T(lambda e, j=j: e.matmul(self.psum[:, 4 + j // 4, (j % 4) * 128:(j % 4 + 1) * 128], lhsT=kend_tm[0:64, j * 128:(j + 1) * 128],
                                          rhs=v_tm[0:64, j * 128:(j + 1) * 128], start=True, stop=True), ["kend_tm", "v_tm"], [("ps", 4 + j // 4)])
            V(lambda e: e.tensor_tensor(out=St, in0=St, in1=eGE[:, :, sc:sc + 1].to_broadcast([128, 8, 128]), op=ALU.mult), ["St", "eGE"], ["St"])
            V(lambda e: e.tensor_tensor(out=flat(St).rearrange("p (a b) -> p a b", b=512), in0=flat(St).rearrange("p (a b) -> p a b", b=512),
                                        in1=self.psum[:, 4:6, :], op=ALU.add), ["St", ("ps", 4), ("ps", 5)], ["St"])
            S(lambda e: e.copy(out=Sb, in_=St), ["St"], ["Sb"])
            if need_out and fin:
                for j in range(8):
                    S(lambda e, j=j: e.activation(out=opv[0:64, j * 128:(j + 1) * 128], in_=och[0:64, j * 128:(j + 1) * 128], func=AF.Square,
                                                  accum_out=sm[0:64, 8 + j:9 + j]), ["och"], ["opv", "sm8"])
                S(lambda e: e.activation(out=sm[0:64, 16:24], in_=sm[0:64, 8:16], func=AF.Sqrt, scale=1.0 / 128, bias=EPS), ["sm8"], ["sm16"])
                V(lambda e: e.reciprocal(out=sm[0:64, 16:24], in_=sm[0:64, 16:24]), ["sm16"], ["sm16"])
                V(lambda e: e.tensor_tensor(out=och[0:64].rearrange("p (j q) -> p j q", q=128), in0=och[0:64].rearrange("p (j q) -> p j q", q=128),
                                            in1=sm[0:64, 16:24].unsqueeze(2).to_broadcast([64, 8, 128]), op=ALU.mult), ["och", "sm16"], ["och"])
                V(lambda e: e.tensor_tensor(out=och[0:64], in0=och[0:64], in1=nwr[0:64], op=ALU.mult), ["och", "nwr"], ["och"])
                for n2 in range(2):
                    for k in range(8):
                        T(lambda e, n2=n2, k=k: e.matmul(self.psum[0:64, n2, :], lhsT=hT[:, k, cols], rhs=w[:, k, 3072 + n2 * 512:3072 + (n2 + 1) * 512],
                                                         start=(k == 0), stop=(k == 7)), ["hT", "w"], [("ps", n2)])
                S(lambda e: e.activation(out=gs[0:64].rearrange("p (a b) -> p a b", b=512), in_=self.psum[0:64, 0:2, :], func=AF.Silu), [("ps", 0), ("ps", 1)], ["gs"])
                V(lambda e: e.tensor_tensor(out=onb[0:64], in0=och[0:64], in1=gs[0:64], op=ALU.mult), ["och", "gs"], ["onb"])
                for k in range(8):
                    T(lambda e, k=k: e.transpose(pbf(7)[:, k * 64:(k + 1) * 64], onb[0:64, k * 128:(k + 1) * 128], self.identb[0:64, 0:64]),
                      ["onb", "identb"], [("ps", 7)])
                S(lambda e: e.copy(out=flat(onT), in_=pbf(7)[:, 0:512]), [("ps", 7)], ["onT"])
                for n2 in range(2):
                    for k in range(8):
                        T(lambda e, n2=n2, k=k: e.matmul(self.psum[0:64, 2 + n2, :], lhsT=onT[:, k, :], rhs=w[:, k, 4096 + n2 * 512:4096 + (n2 + 1) * 512],
                                                         start=(k == 0), stop=(k == 7)), ["onT", "w"], [("ps", 2 + n2)])
                V(lambda e: e.tensor_tensor(out=gs[0:64].rearrange("p (a b) -> p a b", b=512), in0=self.psum[0:64, 2:4, :],
                                            in1=Grow[0:64].rearrange("p (a b) -> p a b", b=512), op=ALU.mult), [("ps", 2), ("ps", 3), "Grow"], ["gs"])
                P.dma("sync", lambda e: e.dma_start(out=opv[0:64], in_=src_fn(i)[sc * 64:sc * 64 + 64, :]), writes=["opv"])
                V(lambda e: e.tensor_tensor(out=gs[0:64], in0=gs[0:64], in1=opv[0:64], op=ALU.add), ["gs", "opv"], ["gs"])
                P.dma("scalar", lambda e: e.dma_start(out=dst_fn(i)[sc * 64:sc * 64 + 64, :], in_=gs[0:64]), reads=["gs"], writes=[self.key("x3")], sem="xo_st")
        for sc in subs:
            sub_body(sc)
    for i in tiles:
        tile_body(i)
    P.barrier()
    A.off = mark


def mixer1(self, src_fn, dst_fn):
    self._mix_helpers()
    self.hg_pass(0, src_fn, dst_fn)
    self.hg_pass(1, src_fn, dst_fn)


KB.hg_inputs = hg_inputs
KB.hg_pass = hg_pass
KB.mixer1 = mixer1
```

```python
import contextlib
import numpy as np
import concourse.bass as bass
import concourse.mybir as mybir
from concourse.alu_op_type import AluOpType as ALU
from concourse.bass_utils import run_bass_kernel_spmd

AF = mybir.ActivationFunctionType
AX = mybir.AxisListType
F32 = mybir.dt.float32
BF16 = mybir.dt.bfloat16
U32 = mybir.dt.uint32
I32 = mybir.dt.int32
U16 = mybir.dt.uint16

ENGS = ("sync", "scalar", "vector", "gpsimd", "tensor")
SAME_ENGINE_WAITS = True


class Prog:
    def __init__(self, nc):
        self.nc = nc
        self.stack = contextlib.ExitStack()
        self.q = {e: [] for e in ENGS}
        self.cnt = {}
        self.waited = {e: {} for e in ENGS}
        self.last_w = {}
        self.reads = {}
        self.semkeys = []
        self.sems = {}
        self.nops = 0
        self.shared = set()

    def sb(self, name, shape, dt):
        return self.stack.enter_context(self.nc.sbuf_tensor(name, list(shape), dt))

    def ps(self, name, shape, dt):
        return self.stack.enter_context(self.nc.psum_tensor(name, list(shape), dt))

    def dram(self, name, shape, dt, kind="Internal"):
        return self.nc.dram_tensor(name, list(shape), dt, kind=kind)

    def _semkey(self, k):
        if k not in self.cnt:
            self.cnt[k] = 0
            self.semkeys.append(k)
        return k

    def _deps(self, eng, reads, writes):
        deps = {}
        def add(ev, war=False):
            if ev is None:
                return
            k, v = ev
            if k in self.shared:
                v = self.cnt[k]
            if k == ("E", eng):
                if eng == "tensor" or war or not SAME_ENGINE_WAITS:
                    return
            if deps.get(k, 0) < v:
                deps[k] = v
        for r in reads:
            add(self.last_w.get(r))
        for w in writes:
            add(self.last_w.get(w))
            for ev in self.reads.get(w, ()):
                add(ev, war=True)
        out = []
        wd = self.waited[eng]
        for k, v in deps.items():
            if wd.get(k, 0) >= v:
                continue
            wd[k] = v
            out.append((k, v))
        return out

    def _record(self, ev, reads, writes):
        for r in reads:
            self.reads.setdefault(r, []).append(ev)
        for w in writes:
            self.last_w[w] = ev
            self.reads[w] = []

    def op(self, eng, fn, reads=(), writes=()):
        reads = tuple(reads); writes = tuple(writes)
        waits = self._deps(eng, reads, writes)
        k = self._semkey(("E", eng))
        self.cnt[k] += 1
        v = self.cnt[k]
        self.q[eng].append((waits, fn, k, 1))
        self._record((k, v), reads, writes)
        self.nops += 1

    def dma(self, eng, fn, reads=(), writes=(), sem=None):
        reads = tuple(reads); writes = tuple(writes)
        waits = self._deps(eng, reads, writes)
        k = self._semkey(("D", sem if sem is not None else writes[0]))
        if sem == "misc":
            self.shared.add(k)
        self.cnt[k] += 16
        v = self.cnt[k]
        self.q[eng].append((waits, fn, k, 16))
        self._record((k, v), reads, writes)
        self.nops += 1

    def wait_all(self, eng, keys):
        waits = self._deps(eng, tuple(keys), ())
        self.q[eng].append((waits, None, None, 0))

    def build(self):
        nc = self.nc
        for i, k in enumerate(self.semkeys):
            self.sems[k] = self.stack.enter_context(nc.semaphore("s%d" % i))
        block = self.stack.enter_context(nc.Block())
        sems = self.sems

        def run(e, items):
            for waits, fn, k, inc in items:
                for wk, wv in waits:
                    e.wait_ge(sems[wk], wv)
                if fn is not None:
                    fn(e).then_inc(sems[k], inc)

        @block.sync
        def _(e):
            run(e, self.q["sync"])

        @block.scalar
        def _(e):
            run(e, self.q["scalar"])

        @block.vector
        def _(e):
            run(e, self.q["vector"])

        @block.gpsimd
        def _(e):
            run(e, self.q["gpsimd"])

        @block.tensor
        def _(e):
            run(e, self.q["tensor"])

    def close(self):
        self.stack.close()


def _barrier(P):
    allk = [(k, P.cnt[k]) for k in P.semkeys if P.cnt[k] > 0]
    for e in ENGS:
        waits = []
        for k, v in allk:
            if P.waited[e].get(k, 0) < v:
                P.waited[e][k] = v
                waits.append((k, v))
        if waits:
            P.q[e].append((waits, None, None, 0))


Prog.barrier = _barrier


class Arena:
    def __init__(self, P, nbytes):
        self.words = nbytes // 4
        self.t = P.sb("arena", [128, self.words], F32)
        self.off = 0
        self.n = 0

    def alloc(self, shape, dt=F32):
        n = 1
        for s in shape:
            n *= s
        esz = 2 if dt in (BF16, U16) else 4
        words = (n * esz + 3) // 4
        words = (words + 7) // 8 * 8
        assert self.off + words <= self.words, ("arena overflow", self.off, words)
        ap = self.t[:, self.off:self.off + words]
        self.off += words
        if dt != F32:
            ap = ap.bitcast(dt)
        ap = ap[:, 0:n]
        if len(shape) > 1:
            names = "abcd"[:len(shape)]
            pat = "p (%s) -> p %s" % (" ".join(names), " ".join(names))
            ap = ap.rearrange(pat, **{names[i]: shape[i] for i in range(len(shape))})
        return ap


D = 1024
NT = 34
EPS = 1e-6
NEG = -1.0e30


class KB:
    def __init__(self, dbg=None):
        self.dbg = dbg or {}
        nc = self.nc = bass.Bass("TRN2", target_bir_lowering=False)
        P = self.P = Prog(nc)
        self.A = Arena(P, 172000)
        self.psum = P.ps("psum", [128, 8, 512], F32)
        di = lambda n, s, dt=F32: nc.dram_tensor(n, list(s), dt, kind="ExternalInput")
        self.x = di("x", [4096, D]); self.ctx = di("ctx", [256, D])
        self.condT = di("condT", [128, 16])
        self.consts = di("consts", [128, 1024])
        self.ada_w = di("ada_w", [2, D, 6 * D]); self.ada_bT = di("ada_bT", [2, 128, 48])
        self.nmixT = di("nmixT", [2, 128, 8]); self.nffnT = di("nffnT", [2, 128, 8])
        self.nf_row = di("nf_row", [128, D])
        self.peer_wq = di("peer_wq", [2, D, 2048]); self.keysT = di("keysT", [2, 128, 2048])
        self.peer_u = [di("peer_u%d" % l, [16384, D]) for l in range(2)]; self.peer_v = [di("peer_v%d" % l, [16384, D]) for l in range(2)]
        self.out = nc.dram_tensor("out", [4096, D], F32, kind="ExternalOutput")
        self.x1 = nc.dram_tensor("x1", [NT * 128, D], F32, kind=self.dbg.get("x1", "Internal"))
        self.x2 = nc.dram_tensor("x2", [NT * 128, D], F32, kind=self.dbg.get("x2", "Internal"))
        self.x3 = nc.dram_tensor("x3", [NT * 128, D], F32, kind=self.dbg.get("x3", "Internal"))
        self.uid = 0
        self.tab_bf = {}
        for l in range(2):
            self.tab_bf[("u", l)] = nc.dram_tensor("ub%d" % l, [16384, D], BF16, kind="Internal")
            self.tab_bf[("v", l)] = nc.dram_tensor("vb%d" % l, [16384, D], BF16, kind="Internal")

    def key(self, s):
        self.uid += 1
        return "%s#%d" % (s, self.uid)

    def bank(self, b, n=512, off=0):
        return self.psum[:, b, off:off + n]

    def src_tile(self, i):
        if i < 2:
            return self.ctx[i * 128:(i + 1) * 128, :]
        return self.x[(i - 2) * 128:(i - 1) * 128, :]

    def setup_consts(self):
        P, A = self.P, self.A
        self.cst = A.alloc([1024]);
        P.dma("sync", lambda e: e.dma_start(out=self.cst, in_=self.consts.ap()), writes=["cst"], sem="misc")
        self.ident = self.cst[:, 0:128]; self.ones = self.cst[:, 128:256]
        self.cond = A.alloc([16])
        P.dma("sync", lambda e: e.dma_start(out=self.cond, in_=self.condT.ap()), writes=["cond"], sem="misc")
        self.condS = A.alloc([8, 2])
        P.op("scalar", lambda e: e.activation(out=self.condS, in_=self.cond.rearrange("p (a b) -> p a b", b=2), func=AF.Silu),
             reads=["cond"], writes=["condS"])
        self.modsFM = [A.alloc([48, 2]) for _ in range(2)]
        self.identb = A.alloc([128], BF16)
        P.op("vector", lambda e: e.tensor_copy(out=self.identb, in_=self.ident), reads=["cst"], writes=["identb"])
        P.barrier()

    def mods_phase(self, l):
        P, A = self.P, self.A
        mark = A.off
        ws = [A.alloc([8, 768]) for _ in range(2)]
        abT = A.alloc([48])
        P.dma("sync", lambda e: e.dma_start(out=abT, in_=self.ada_bT[l]), writes=["abT"], sem="misc")
        pm = self.psum[:, 0, 0:96]
        awv = self.ada_w[l].rearrange("(k p) n -> p k n", p=128)
        for g in range(8):
            s = g % 2
            P.dma("sync" if s == 0 else "scalar", lambda e, g=g, s=s: e.dma_start(out=ws[s], in_=awv[:, :, g * 768:(g + 1) * 768]),
                  writes=[("ws", s)])
            for jj in range(6):
                j = g * 6 + jj
                for k in range(8):
                    P.op("tensor", lambda e, k=k, s=s, j=j, jj=jj: e.matmul(pm[:, 2 * j:2 * j + 2], lhsT=ws[s][:, k, jj * 128:(jj + 1) * 128],
                                                                           rhs=self.condS[:, k, :], start=(k == 0), stop=(k == 7)),
                         reads=[("ws", s), "condS"], writes=[("ps", 0)])
        m = self.modsFM[l]
        P.op("vector", lambda e: e.tensor_tensor(out=m, in0=pm.rearrange("p (a b) -> p a b", b=2),
                                                 in1=abT.unsqueeze(2).to_broadcast([128, 48, 2]), op=ALU.add),
             reads=[("ps", 0), "abT"], writes=[("mods", l)])
        P.barrier()
        A.off = mark

    def make_row(self, dst, col, dstkey, colkeys, banks=(6, 7)):
        P = self.P
        dg = self.diag
        for jj in range(8):
            P.op("vector", lambda e, jj=jj: e.tensor_scalar(out=dg, in0=self.ident, scalar1=col[:, jj:jj + 1], scalar2=None, op0=ALU.mult),
                 reads=["cst"] + list(colkeys), writes=["diag"])
            b = banks[jj // 4]
            P.op("tensor", lambda e, jj=jj, b=b: e.matmul(self.bank(b, 128, (jj % 4) * 128), lhsT=self.ones, rhs=dg, start=True, stop=True),
                 reads=["diag", "cst"], writes=[("ps", b)])
        for h in range(2):
            P.op("scalar", lambda e, h=h: e.copy(out=dst[:, h * 512:(h + 1) * 512], in_=self.bank(banks[h])),
                 reads=[("ps", banks[h])], writes=[dstkey])

    def conv_tables(self):
        P, A = self.P, self.A
        mark = A.off
        RB = 4
        ib = [A.alloc([RB, D]) for _ in range(2)]
        ob = [A.alloc([RB, D], BF16) for _ in range(3)]
        n = 0
        for l in range(2):
            for nm, src in (("u", self.peer_u[l]), ("v", self.peer_v[l])):
                sv = src.ap().rearrange("(p r) d -> p r d", p=128)
                dv = self.tab_bf[(nm, l)].ap().rearrange("(p r) d -> p r d", p=128)
                for c in range(128 // RB):
                    si = n % 2; so = n % 3; n += 1
                    P.dma("sync", lambda e, sv=sv, c=c, si=si: e.dma_start(out=ib[si], in_=sv[:, c * RB:(c + 1) * RB, :]), writes=[("tib", si)])
                    eng = ("vector", "gpsimd", "scalar")[so]
                    if eng == "scalar":
                        P.op(eng, lambda e, si=si, so=so: e.copy(out=ob[so], in_=ib[si]), reads=[("tib", si)], writes=[("tob", so)])
                    else:
                        P.op(eng, lambda e, si=si, so=so: e.tensor_copy(out=ob[so], in_=ib[si]), reads=[("tib", si)], writes=[("tob", so)])
                    P.dma("scalar" if so != 2 else "gpsimd", lambda e, dv=dv, c=c, so=so: e.dma_start(out=dv[:, c * RB:(c + 1) * RB, :], in_=ob[so]),
                          reads=[("tob", so)], writes=[self.key("tbf")], sem=("tob_st", so))
        P.barrier()
        A.off = mark

    def peer_phase(self, l, src_fn, dst, tiles, final_norm=False):
        P, A = self.P, self.A
        mark = A.off
        V = lambda fn, r, w: P.op("vector", fn, r, w)
        S = lambda fn, r, w: P.op("scalar", fn, r, w)
        T = lambda fn, r, w: P.op("tensor", fn, r, w)
        m = self.modsFM[l]
        mk = [("mods", l)]
        wq = A.alloc([8, 2048])
        for k in range(8):
            P.dma("sync" if k % 2 == 0 else "scalar",
                  lambda e, k=k: e.dma_start(out=wq[:, k, :], in_=self.peer_wq[l, k * 128:(k + 1) * 128, :]), writes=["wq"], sem="misc")
        kT = A.alloc([16, 128])
        P.dma("sync", lambda e: e.dma_start(out=kT, in_=self.keysT[l].rearrange("p (a b) -> p a b", b=128)), writes=["kT"], sem="misc")
        nf = A.alloc([8])
        P.dma("sync", lambda e: e.dma_start(out=nf, in_=self.nffnT[l]), writes=["nf"], sem="misc")
        Arow = A.alloc([D]); Brow = A.alloc([D]); Grow = A.alloc([D])
        acol = A.alloc([8])
        xt = A.alloc([D]); h2 = A.alloc([D]); hT = A.alloc([8, 128]); xn = h2
        qT = A.alloc([16, 128]); sub = A.alloc([16, 128]); sub2 = sub
        cand = sub.rearrange("p a b -> p (a b)").rearrange("p (a b) -> p a b", b=256)
        candi = qT.rearrange("p a b -> p (a b)").rearrange("p (a b) -> p a b", b=256)
        st = A.alloc([16, 16]); it = A.alloc([16, 16], U32); itf = A.alloc([16, 16])
        bs = A.alloc([8, 16]); idxf = A.alloc([128]); idx = A.alloc([128], U32)
        gate = A.alloc([8, 16]); sm = A.alloc([16]); act = A.alloc([128]); araw = A.alloc([128])
        jbig = A.alloc([2 * D]); junk = jbig[:, 0:D]; acc1 = jbig[:, D:2 * D]; acc = A.alloc([D])
        cand2 = jbig.rearrange("p (a b) -> p a b", b=256)
        NS = 6
        gs = [A.alloc([D], BF16) for _ in range(NS)]
        gcount = [0]
        if final_norm:
            nfr = A.alloc([D])
            P.dma("sync", lambda e: e.dma_start(out=nfr, in_=self.nf_row.ap()), writes=["nfr"], sem="misc")
        utab = self.tab_bf[("u", l)].ap(); vtab = self.tab_bf[("v", l)].ap()
        cur_c = [None]

        def set_cond(c):
            if cur_c[0] == c:
                return
            cur_c[0] = c
            V(lambda e: e.scalar_tensor_tensor(out=acol, in0=m[:, 32:40, c], scalar=1.0, in1=nf, op0=ALU.add, op1=ALU.mult),
              mk + ["nf"], ["acol"])
            self.make_row(Arow, acol, "Arow", ["acol"])
            self.make_row(Brow, m[:, 24:32, c], "Brow", mk)
            self.make_row(Grow, m[:, 40:48, c], "Grow", mk)

        for i in tiles:
            set_cond(0 if i >= 2 else 1)
            P.dma("sync", lambda e, i=i: e.dma_start(out=xt, in_=src_fn(i)), writes=["xt"])
            S(lambda e: e.activation(out=junk, in_=xt, func=AF.Square, accum_out=sm[:, 0:1]), ["xt"], ["junk", "sm0"])
            S(lambda e: e.activation(out=sm[:, 1:2], in_=sm[:, 0:1], func=AF.Sqrt, scale=1.0 / D, bias=EPS), ["sm0"], ["sm1"])
            V(lambda e: e.reciprocal(out=sm[:, 2:3], in_=sm[:, 1:2]), ["sm1"], ["sm2"])
            V(lambda e: e.tensor_scalar(out=xn, in0=xt, scalar1=sm[:, 2:3], scalar2=None, op0=ALU.mult), ["xt", "sm2"], ["h2"])
            V(lambda e: e.tensor_tensor(out=xn, in0=xn, in1=Arow, op=ALU.mult), ["h2", "Arow"], ["h2"])
            V(lambda e: e.tensor_tensor(out=h2, in0=xn, in1=Brow, op=ALU.add), ["h2", "Brow"], ["h2"])
            for k in range(8):
                T(lambda e, k=k: e.transpose(self.bank(k // 4, 128, (k % 4) * 128), h2[:, k * 128:(k + 1) * 128], self.ident),
                  ["h2", "cst"], [("ps", k // 4)])
            S(lambda e: e.copy(out=hT.rearrange("p a b -> p (a b)").rearrange("p (a b) -> p a b", b=512), in_=self.psum[:, 0:2, :]),
              [("ps", 0), ("ps", 1)], ["hT"])
            for j in range(16):
                for k in range(8):
                    T(lambda e, j=j, k=k: e.matmul(self.bank(2 + j // 4, 128, (j % 4) * 128), lhsT=wq[:, k, j * 128:(j + 1) * 128],
                                                  rhs=hT[:, k, :], start=(k == 0), stop=(k == 7)),
                      ["wq", "hT"], [("ps", 2 + j // 4)])
            S(lambda e: e.copy(out=qT.rearrange("p a b -> p (a b)").rearrange("p (a b) -> p a b", b=512), in_=self.psum[:, 2:6, :]),
              [("ps", b) for b in (2, 3, 4, 5)], ["qT"])
            sb_banks = (0, 1, 6, 7)
            for j in range(16):
                b = sb_banks[j // 4]
                T(lambda e, j=j, b=b: e.matmul(self.bank(b, 128, (j % 4) * 128), lhsT=qT[:, j, :], rhs=kT[:, j, :], start=True, stop=True),
                  ["qT", "kT"], [("ps", b)])
            for q4 in range(4):
                b = sb_banks[q4]
                S(lambda e, q4=q4, b=b: e.copy(out=sub[:, q4 * 4:(q4 + 1) * 4, :].rearrange("p a b -> p (a b)"), in_=self.bank(b)),
                  [("ps", b)], ["sub"])
            for j in range(16):
                V(lambda e, j=j: e.max(out=st[:, j, 0:8], in_=sub[:, j, :]), ["sub"], [("st0", j)])
            for j in range(16):
                V(lambda e, j=j: e.max_index(out=it[:, j, 0:8], in_max=st[:, j, 0:8], in_values=sub[:, j, :]), ["sub", ("st0", j)], [("it0", j)])
            for j in range(16):
                V(lambda e, j=j: e.match_replace(out=sub[:, j, :], in_to_replace=st[:, j, 0:8], in_values=sub[:, j, :], imm_value=NEG),
                  ["sub", ("st0", j), ("it0", j)], [("sub2", j)])
            for j in range(16):
                V(lambda e, j=j: e.max(out=st[:, j, 8:16], in_=sub[:, j, :]), [("sub2", j)], [("st1", j)])
            for j in range(16):
                V(lambda e, j=j: e.max_index(out=it[:, j, 8:16], in_max=st[:, j, 8:16], in_values=sub[:, j, :]), [("sub2", j), ("st1", j)], [("it1", j)])
            V(lambda e: e.tensor_copy(out=itf, in_=it), [("it0", j) for j in range(16)] + [("it1", j) for j in range(16)], ["itf"])
            st4 = st.rearrange("p (h a) k -> p h a k", a=2)
            if4 = itf.rearrange("p (h a) k -> p h a k", a=2)
            V(lambda e: e.tensor_scalar(out=if4[:, :, 0, :], in0=if4[:, :, 0, :], scalar1=128.0, scalar2=None, op0=ALU.mult), ["itf"], ["itf"])
            c4 = cand.rearrange("p h (a b) -> p h a b", b=16)
            ci4 = candi.rearrange("p h (a b) -> p h a b", b=16)
            V(lambda e: e.tensor_tensor(out=c4, in0=st4[:, :, 0, :].unsqueeze(3).to_broadcast([128, 8, 16, 16]),
                                        in1=st4[:, :, 1, :].unsqueeze(2).to_broadcast([128, 8, 16, 16]), op=ALU.add), [("st0", j) for j in range(16)] + [("st1", j) for j in range(16)] + [("it0", j) for j in range(16)] + [("it1", j) for j in range(16)] + [("sub2", j) for j in range(16)], ["sub"])
            V(lambda e: e.tensor_tensor(out=ci4, in0=if4[:, :, 0, :].unsqueeze(3).to_broadcast([128, 8, 16, 16]),
                                        in1=if4[:, :, 1, :].unsqueeze(2).to_broadcast([128, 8, 16, 16]), op=ALU.add), ["itf"], ["qT"])
            for h in range(8):
                V(lambda e, h=h: e.max(out=bs[:, h, 0:8], in_=cand[:, h, :]), ["sub"], [("bs0", h)])
            for h in range(8):
                V(lambda e, h=h: e.match_replace(out=cand2[:, h, :], in_to_replace=bs[:, h, 0:8], in_values=cand[:, h, :], imm_value=NEG),
                  ["sub", ("bs0", h), "junk", "acc1"], [("cand2", h)])
            for h in range(8):
                V(lambda e, h=h: e.max(out=bs[:, h, 8:16], in_=cand2[:, h, :]), [("cand2", h)], [("bs1", h)])
            for h in range(8):
                for k in range(16):
                    V(lambda e, h=h, k=k: e.scalar_tensor_tensor(out=junk[:, 0:256], in0=cand[:, h, :], scalar=bs[:, h, k:k + 1],
                                                               in1=candi[:, h, :], op0=ALU.is_equal, op1=ALU.mult,
                                                               accum_out=idxf[:, h * 16 + k:h * 16 + k + 1]),
                      ["sub", ("bs0", h), ("bs1", h), "qT"], [("idxf1", h * 16 + k)])
            V(lambda e: e.tensor_scalar(out=idxf, in0=idxf, scalar1=16383.0, scalar2=0.0, op0=ALU.min, op1=ALU.max), [("idxf1", q) for q in range(128)], ["idxf"])
            V(lambda e: e.tensor_copy(out=idx, in_=idxf), ["idxf"], ["idx"])
            V(lambda e: e.tensor_tensor(out=gate, in0=bs, in1=bs[:, :, 0:1].to_broadcast([128, 8, 16]), op=ALU.subtract), [("bs0", h) for h in range(8)] + [("bs1", h) for h in range(8)], ["gate"])
            S(lambda e: e.activation(out=gate, in_=gate, func=AF.Exp), ["gate"], ["gate"])
            V(lambda e: e.tensor_reduce(out=sm[:, 4:12], in_=gate, axis=AX.X, op=ALU.add), ["gate"], ["sm4"])
            V(lambda e: e.reciprocal(out=sm[:, 4:12], in_=sm[:, 4:12]), ["sm4"], ["sm4"])
            V(lambda e: e.tensor_tensor(out=gate, in0=gate, in1=sm[:, 4:12].unsqueeze(2).to_broadcast([128, 8, 16]), op=ALU.mult),
              ["gate", "sm4"], ["gate"])
            for k in range(128):
                s = gcount[0] % NS; gcount[0] += 1
                P.dma("gpsimd", lambda e, k=k, s=s: e.indirect_dma_start(
                    out=gs[s], out_offset=None, in_=utab,
                    in_offset=bass.IndirectOffsetOnAxis(ap=idx[:, k:k + 1], axis=0)), reads=["idx"], writes=[("gs", s)],
                    sem=("gs", l, s, gcount[0] // (NS * 250)))
                V(lambda e, k=k, s=s: e.scalar_tensor_tensor(out=junk, in0=gs[s], scalar=1.0, in1=h2, op0=ALU.mult, op1=ALU.mult,
                                                           accum_out=araw[:, k:k + 1]), [("gs", s), "h2"], [("araw", k)])
            S(lambda e: e.activation(out=act, in_=araw, func=AF.Gelu), [("araw", k) for k in range(128)], ["act"])
            V(lambda e: e.tensor_tensor(out=act, in0=act, in1=gate.rearrange("p a b -> p (a b)"), op=ALU.mult), ["act", "gate"], ["act"])
            for k in range(128):
                s = gcount[0] % NS; gcount[0] += 1
                P.dma("gpsimd", lambda e, k=k, s=s: e.indirect_dma_start(
                    out=gs[s], out_offset=None, in_=vtab,
                    in_offset=bass.IndirectOffsetOnAxis(ap=idx[:, k:k + 1], axis=0)), reads=["idx"], writes=[("gs", s)],
                    sem=("gs", l, s, gcount[0] // (NS * 250)))
                ab = acc if k % 2 == 0 else acc1
                akey = "junk" if k % 2 == 0 else "acc1"
                if k < 2:
                    V(lambda e, k=k, s=s, ab=ab: e.tensor_scalar(out=ab, in0=gs[s], scalar1=act[:, k:k + 1], scalar2=None, op0=ALU.mult),
                      [("gs", s), "act"], [akey])
                else:
                    V(lambda e, k=k, s=s, ab=ab: e.scalar_tensor_tensor(out=ab, in0=gs[s], scalar=act[:, k:k + 1], in1=ab,
                                                                      op0=ALU.mult, op1=ALU.add), [("gs", s), "act", akey], [akey])
            V(lambda e: e.tensor_tensor(out=acc, in0=acc, in1=acc1, op=ALU.add), ["junk", "acc1"], ["junk"])
            V(lambda e: e.tensor_tensor(out=acc, in0=acc, in1=Grow, op=ALU.mult), ["junk", "Grow"], ["junk"])
            V(lambda e: e.tensor_tensor(out=acc, in0=acc, in1=xt, op=ALU.add), ["junk", "xt"], ["junk"])
            if final_norm:
                S(lambda e: e.activation(out=h2, in_=acc, func=AF.Square, accum_out=sm[:, 12:13]), ["junk"], ["h2", "sm12"])
                S(lambda e: e.activation(out=sm[:, 13:14], in_=sm[:, 12:13], func=AF.Sqrt, scale=1.0 / D, bias=EPS), ["sm12"], ["sm13"])
                V(lambda e: e.reciprocal(out=sm[:, 14:15], in_=sm[:, 13:14]), ["sm13"], ["sm14"])
                V(lambda e: e.scalar_tensor_tensor(out=acc, in0=acc, scalar=sm[:, 14:15], in1=nfr, op0=ALU.mult, op1=ALU.mult),
                  ["junk", "sm14", "nfr"], ["junk"])
            P.dma("scalar", lambda e, i=i: e.dma_start(out=dst(i), in_=acc), reads=["junk"], writes=[self.key("dst")], sem="acc_st")
        P.barrier()
        A.off = mark


def fm(v, nch):
    return np.ascontiguousarray(np.asarray(v, np.float32).reshape(nch, 128).T)


def make_consts():
    c = np.zeros((128, 1024), np.float32)
    k = np.arange(128)
    c[:, 0:128] = np.eye(128, dtype=np.float32)
    c[:, 128:256] = 1.0
    c[:, 256:384] = (k[:, None] <= k[None, :])
    c[:, 384:512] = (k[:, None] > k[None, :])
    c[:, 512:640] = (k[:, None] >= k[None, :])
    c[:, 640:768] = (k[:, None] < k[None, :])
    return c


def prep_shared(inp):
    g = {}
    f32 = lambda a: np.ascontiguousarray(np.asarray(a, np.float32))
    g["consts"] = make_consts()
    g["ada_w"] = f32(inp["ada_w"])
    g["ada_bT"] = np.stack([fm(inp["ada_b"][l], 48) for l in range(2)])
    g["nmixT"] = np.stack([fm(inp["norm_mix_w"][l], 8) for l in range(2)])
    g["nffnT"] = np.stack([fm(inp["norm_ffn_w"][l], 8) for l in range(2)])
    g["nf_row"] = np.ascontiguousarray(np.broadcast_to(f32(inp["norm_f_w"])[None, :], (128, D)))
    g["ev_w_in"] = f32(inp["ev_w_in"][0]); g["ev_w_out"] = f32(inp["ev_w_out"][0])
    cwf = lambda w, nj: np.ascontiguousarray(f32(w).reshape(3, nj, 128).transpose(2, 1, 0).reshape(128, nj * 3))
    g["ssd_cwT"] = cwf(inp["ssd_conv_w"][0], 16); g["sc_cwT"] = cwf(inp["sc_conv_w"][0], 8)
    g["ssd_cbT"] = fm(inp["ssd_conv_b"][0], 16)
    brow = lambda v: np.ascontiguousarray(np.broadcast_to(f32(v).reshape(1, -1), (128, f32(v).size)))
    g["dtb_row"] = brow(inp["ssd_dt_bias"][0]); g["alog_row"] = brow(inp["ssd_a_log"][0])
    g["dsk_row"] = brow(inp["ssd_d"][0]); g["ssdnw_row"] = brow(inp["ssd_norm_w"][0])
    g["od_w_in"] = f32(inp["od_w_in"][0]); g["od_w_out"] = f32(inp["od_w_out"][0])
    lg = f32(inp["hg_lb_logits"])
    g["lblT"] = np.ascontiguousarray(lg.reshape(2, 2, 8, 128).transpose(3, 0, 1, 2).reshape(128, 32))
    g["hgnw_row"] = brow(inp["hg_norm_w"][0])
    smk = np.ones((128, 8, 2, 64), np.float32); smk[:, :, :, 0] = 0.0
    g["scanmask"] = smk.reshape(128, 1024)
    g["peer_wq"] = f32(inp["peer_wq"])
    pk = f32(inp["peer_keys"])
    g["keysT"] = np.ascontiguousarray(pk.transpose(0, 4, 1, 2, 3).reshape(2, 128, 2048))
    for l in range(2):
        g["peer_u%d" % l] = f32(inp["peer_u"][l]); g["peer_v%d" % l] = f32(inp["peer_v"][l])
    return g


def prep_core(inp, b):
    m = {}
    m["x"] = np.ascontiguousarray(np.asarray(inp["x"][b], np.float32))
    m["ctx"] = np.ascontiguousarray(np.asarray(inp["ctx"][b], np.float32))
    ct = np.zeros((128, 8, 2), np.float32)
    ct[:, :, 0] = fm(inp["c"][b], 8); ct[:, :, 1] = fm(inp["c_ctx"], 8)
    m["condT"] = ct.reshape(128, 16)
    return m


def build_full():
    kb = KB()
    kb.ssd_inputs()
    kb.hg_inputs()
    kb.setup_consts()
    kb.diag = kb.A.alloc([128])
    tiles = list(range(NT))
    xtile = lambda t: (lambda i: t[i * 128:(i + 1) * 128, :])
    kb.conv_tables()
    kb.mods_phase(0)
    src0 = kb.src_tile
    if hasattr(kb, "mixer0"):
        kb.mixer0(src0, xtile(kb.x1))
        src0 = xtile(kb.x1)
    kb.peer_phase(0, src0, xtile(kb.x2), tiles)
    kb.mods_phase(1)
    src1 = xtile(kb.x2)
    if hasattr(kb, "mixer1"):
        kb.mixer1(src1, xtile(kb.x3))
        src1 = xtile(kb.x3)
    kb.peer_phase(1, src1, lambda i: kb.out[(i - 2) * 128:(i - 1) * 128, :], tiles[2:], final_norm=True)
    kb.P.wait_all("sync", [])
    kb.P.build()
    return kb


def kernel(**inputs):
    inp = {k: np.asarray(v) for k, v in inputs.items()}
    kb = build_full()
    names = set()
    for a in kb.nc.allocations:
        if getattr(a, "kind", None) == "ExternalInput" and a.memorylocations:
            names.add(a.memorylocations[0].name)
    g = prep_shared(inp)
    in_maps = []
    for b in range(8):
        m = prep_core(inp, b)
        m.update(g)
        in_maps.append({k: v for k, v in m.items() if k in names})
    res = run_bass_kernel_spmd(kb.nc, in_maps, core_ids=list(range(8)))
    out = np.stack([np.asarray(r["out"], np.float32) for r in res.results], axis=0)
    return out


def _mix_helpers(self):
    P = self.P
    self.V = lambda fn, r, w: P.op("vector", fn, r, w)
    self.S = lambda fn, r, w: P.op("scalar", fn, r, w)
    self.T = lambda fn, r, w: P.op("tensor", fn, r, w)
    self.G = lambda fn, r, w: P.op("gpsimd", fn, r, w)


def load_w_bf16(self, dst, src, col0, ncols, key, stage):
    P = self.P
    piece = stage[0].shape[-1]
    n = 0
    for k in range(8):
        c = 0
        while c < ncols:
            w = min(piece, ncols - c)
            s = n % 2; n += 1
            P.dma("sync" if s == 0 else "scalar",
                  lambda e, k=k, c=c, w=w, s=s: e.dma_start(out=stage[s][:, 0:w], in_=src[k * 128:(k + 1) * 128, col0 + c:col0 + c + w]),
                  writes=[("stg", s)])
            P.op("gpsimd", lambda e, k=k, c=c, w=w, s=s: e.tensor_copy(out=dst[:, k, c:c + w], in_=stage[s][:, 0:w]),
                 reads=[("stg", s)], writes=[key])
            c += w


def norm_T(self, src_ap, Acol, Bcol, ckeys, dst, dkey, bufs):
    P, V, S, T = self.P, self.V, self.S, self.T
    xt, xnb, sm = bufs
    P.dma("sync", lambda e: e.dma_start(out=xt, in_=src_ap), writes=["n_xt"])
    S(lambda e: e.activation(out=xnb, in_=xt, func=AF.Square, accum_out=sm[:, 0:1]), ["n_xt"], ["n_xnb", "n_sm0"])
    S(lambda e: e.activation(out=sm[:, 1:2], in_=sm[:, 0:1], func=AF.Sqrt, scale=1.0 / D, bias=EPS), ["n_sm0"], ["n_sm1"])
    V(lambda e: e.reciprocal(out=sm[:, 2:3], in_=sm[:, 1:2]), ["n_sm1"], ["n_sm2"])
    V(lambda e: e.tensor_scalar(out=xnb, in0=xt, scalar1=sm[:, 2:3], scalar2=None, op0=ALU.mult), ["n_xt", "n_sm2"], ["n_xnb"])
    pb = self.psum[:, 7, :].bitcast(BF16)
    for k in range(8):
        T(lambda e, k=k: e.transpose(pb[:, k * 128:(k + 1) * 128], xnb[:, k * 128:(k + 1) * 128], self.identb),
          ["n_xnb", "identb"], [("ps", 7)])
    for k in range(8):
        S(lambda e, k=k: e.activation(out=dst[:, k, :], in_=pb[:, k * 128:(k + 1) * 128], func=AF.Identity,
                                      scale=Acol[:, k:k + 1], bias=Bcol[:, k:k + 1]), [("ps", 7)] + list(ckeys), [dkey])


def proj_fm(self, dst, dkey, w, wkey, col0, nj, hT, hkey, W, func=None):
    for jj in range(nj):
        b = 6 + (jj % 2)
        for k in range(8):
            self.T(lambda e, jj=jj, k=k, b=b: e.matmul(self.bank(b, W), lhsT=w[:, k, col0 + jj * 128:col0 + (jj + 1) * 128], rhs=hT[:, k, 0:W],
                                                       start=(k == 0), stop=(k == 7)), [wkey, hkey], [("ps", b)])
        if func is None:
            self.S(lambda e, jj=jj, b=b: e.copy(out=dst[:, jj, 0:W], in_=self.bank(b, W)), [("ps", b)], [(dkey, jj)])
        else:
            self.S(lambda e, jj=jj, b=b: e.activation(out=dst[:, jj, 0:W], in_=self.bank(b, W), func=func), [("ps", b)], [(dkey, jj)])


def conv3(self, dst, dkey, src, skeys, cw, cwkey, nj, W, rows):
    L = W // rows
    d3 = lambda j: dst[:, j, 0:W].rearrange("p (r l) -> p r l", l=L)
    s3 = lambda j: src[:, j, 0:W].rearrange("p (r l) -> p r l", l=L)
    for j in range(nj):
        self.V(lambda e, j=j: e.tensor_scalar(out=dst[:, j, 0:W], in0=src[:, j, 0:W], scalar1=cw[:, j, 1:2], scalar2=None, op0=ALU.mult),
               skeys(j) + [cwkey], [(dkey, j)])
    for j in range(nj):
        self.V(lambda e, j=j: e.scalar_tensor_tensor(out=d3(j)[:, :, 1:L], in0=s3(j)[:, :, 0:L - 1], scalar=cw[:, j, 0:1],
                                                     in1=d3(j)[:, :, 1:L], op0=ALU.mult, op1=ALU.add), skeys(j) + [cwkey, (dkey, j)], [(dkey, j)])
    for j in range(nj):
        self.V(lambda e, j=j: e.scalar_tensor_tensor(out=d3(j)[:, :, 0:L - 1], in0=s3(j)[:, :, 1:L], scalar=cw[:, j, 2:3],
                                                     in1=d3(j)[:, :, 0:L - 1], op0=ALU.mult, op1=ALU.add), skeys(j) + [cwkey, (dkey, j)], [(dkey, j)])


KB._mix_helpers = _mix_helpers
KB.load_w_bf16 = load_w_bf16
KB.norm_T = norm_T
KB.proj_fm = proj_fm
KB.conv3 = conv3


def ssd_inputs(self):
    nc = self.nc
    di = lambda n, s, dt=F32: nc.dram_tensor(n, list(s), dt, kind="ExternalInput")
    self.ev_w_in = di("ev_w_in", [D, 6176]); self.ev_w_out = di("ev_w_out", [2048, D])
    self.ssd_cwT = di("ssd_cwT", [128, 48]); self.ssd_cbT = di("ssd_cbT", [128, 16]); self.sc_cwT = di("sc_cwT", [128, 24])
    self.dtb_row = di("dtb_row", [128, 32]); self.alog_row = di("alog_row", [128, 32])
    self.dsk_row = di("dsk_row", [128, 16]); self.ssdnw_row = di("ssdnw_row", [128, D])
    self.yscr = nc.dram_tensor("yscr", [NT * 128, D], F32, kind=self.dbg.get("yscr", "Internal"))
    self.xsscr = nc.dram_tensor("xsscr", [NT * 128, D], BF16, kind="Internal")


def mix_cols(self, l, c, nT, Acol, key):
    m = self.modsFM[l]
    self.V(lambda e: e.scalar_tensor_tensor(out=Acol, in0=m[:, 8:16, c], scalar=1.0, in1=nT, op0=ALU.add, op1=ALU.mult),
           [("mods", l), "nT"], [key])
    return m[:, 0:8, c]


def ssd_scan(self, d, src_fn):
    P, A, V, S, T = self.P, self.A, self.V, self.S, self.T
    mark = A.off
    l = 0
    wx = A.alloc([8, 2080], BF16)
    stage = [A.alloc([2080]) for _ in range(2)]
    self.load_w_bf16(wx, self.ev_w_in, 4096, 2080, "wx", stage)
    cw = A.alloc([16, 3]); cb = A.alloc([16]); nT = A.alloc([8])
    dtb = A.alloc([32]); arow = A.alloc([32])
    for dst_, src_, k_ in ((cw, self.ssd_cwT.ap().rearrange("p (a b) -> p a b", b=3), "cw"), (cb, self.ssd_cbT.ap(), "cb"),
                           (nT, self.nmixT[l], "nT"), (dtb, self.dtb_row.ap(), "dtb"), (arow, self.alog_row.ap(), "arow")):
        P.dma("sync", lambda e, dst_=dst_, src_=src_: e.dma_start(out=dst_, in_=src_), writes=[k_], sem="misc")
    S(lambda e: e.activation(out=arow, in_=arow, func=AF.Exp), ["arow"], ["arow"])
    V(lambda e: e.tensor_scalar(out=arow, in0=arow, scalar1=-1.0, scalar2=None, op0=ALU.mult), ["arow"], ["arow"])
    Acol = [A.alloc([8]) for _ in range(2)]
    Bcol = [self.mix_cols(l, c, nT, Acol[c], ("Acol", c)) for c in range(2)]
    xt = A.alloc([D]); xnb = A.alloc([D], BF16); sm = A.alloc([4])
    hTu = A.alloc([8, 256], BF16)
    praw = A.alloc([16, 256]); cv = A.alloc([16, 256]); xbcT = A.alloc([16, 256], BF16)
    dt = A.alloc([2, 16]); dta = A.alloc([2, 16]); e48 = A.alloc([48]); wend = A.alloc([16])
    R = A.alloc([16, 128]); cbm = A.alloc([4, 128]); Eq = A.alloc([4, 128]); attT = A.alloc([16, 128], BF16)
    xs_tm = A.alloc([D], BF16); bm_tm = A.alloc([512], BF16); xw = A.alloc([D], BF16)
    ych = A.alloc([D]); ypv = A.alloc([D]); St = A.alloc([D]); Sb = A.alloc([D], BF16)
    V(lambda e: e.memset(St, 0.0), [], ["St"])
    V(lambda e: e.memset(Sb, 0.0), [], ["Sb"])
    cst = self.cst
    if d == 0:
        Bm = cst[:, 256:384]; Am = cst[:, 384:512]; Mk = cst[:, 256:384]
    else:
        Bm = cst[:, 512:640]; Am = cst[:, 640:768]; Mk = cst[:, 512:640]
    units = [(0, 256, 1)] + [(i, 128, 0) for i in range(2, NT)]
    if d == 1:
        units = [units[0]] + units[1:][::-1]
    pbf = lambda b: self.psum[:, b, :].bitcast(BF16)
    def unit_body(t0, W, c):
        nsub = W // 128
        for sub in range(nsub):
            self.norm_T(src_fn(t0 + sub), Acol[c], Bcol[c], [("Acol", c), ("mods", l)], hTu[:, :, sub * 128:(sub + 1) * 128], "hTu", (xt, xnb, sm))
        self.proj_fm(praw, "praw", wx, "wx", 0, 16, hTu, "hTu", W)
        self.conv3(cv, "cv", praw, lambda j: [("praw", j)], cw, "cw", 16, W, 1 if c == 1 else W // 64)
        for j in range(16):
            S(lambda e, j=j: e.activation(out=xbcT[:, j, 0:W], in_=cv[:, j, 0:W], func=AF.Silu, bias=cb[:, j:j + 1]), [("cv", j), "cb"], ["xbcT"])
        for sub in range(nsub):
            for k in range(8):
                T(lambda e, k=k, sub=sub: e.matmul(self.bank(6, 32), lhsT=hTu[:, k, sub * 128:(sub + 1) * 128], rhs=wx[:, k, 2048:2080],
                                                   start=(k == 0), stop=(k == 7)), ["hTu", "wx"], [("ps", 6)])
            V(lambda e, sub=sub: e.tensor_tensor(out=dt[:, sub, :], in0=self.bank(6, 16, d * 16), in1=dtb[:, d * 16:(d + 1) * 16], op=ALU.add),
              [("ps", 6), "dtb"], ["dt"])
        S(lambda e: e.activation(out=dt[:, 0:nsub, :], in_=dt[:, 0:nsub, :], func=AF.Exp), ["dt"], ["dt"])
        S(lambda e: e.activation(out=dt[:, 0:nsub, :], in_=dt[:, 0:nsub, :], func=AF.Ln, bias=1.0), ["dt"], ["dt"])
        V(lambda e: e.tensor_tensor(out=dta[:, 0:nsub, :], in0=dt[:, 0:nsub, :],
                                    in1=arow[:, d * 16:(d + 1) * 16].unsqueeze(1).to_broadcast([128, nsub, 16]), op=ALU.mult),
          ["dt", "arow"], ["dta"])
        chunks = list(range(nsub))
        if d == 1:
            chunks = chunks[::-1]
        def chunk_body(ch):
            tile = t0 + ch
            c0 = ch * 128
            cols = slice(c0, c0 + 128)
            for j in range(8):
                T(lambda e, j=j: e.transpose(pbf(7)[:, j * 128:(j + 1) * 128], xbcT[:, j, cols], self.identb), ["xbcT", "identb"], [("ps", 7)])
            S(lambda e: e.copy(out=xs_tm, in_=pbf(7)), [("ps", 7)], ["xs_tm"])
            for j in range(4):
                T(lambda e, j=j: e.transpose(pbf(6)[:, j * 128:(j + 1) * 128], xbcT[:, 8 + j, cols], self.identb), ["xbcT", "identb"], [("ps", 6)])
            S(lambda e: e.copy(out=bm_tm, in_=pbf(6)[:, 0:512]), [("ps", 6)], ["bm_tm"])
            for g in range(4):
                T(lambda e, g=g: e.matmul(self.bank(7, 128, g * 128), lhsT=xbcT[:, 8 + g, cols], rhs=xbcT[:, 12 + g, cols], start=True, stop=True),
                  ["xbcT"], [("ps", 7)])
            V(lambda e: e.tensor_tensor(out=cbm, in0=self.bank(7).rearrange("p (g t) -> p g t", t=128),
                                        in1=Mk.unsqueeze(1).to_broadcast([128, 4, 128]), op=ALU.mult), [("ps", 7), "cst"], ["cbm"])
            dta_c = dta[:, ch, :]; dt_c = dt[:, ch, :]
            for n_, lh in enumerate((Bm, Am, self.ones)):
                T(lambda e, n_=n_, lh=lh: e.matmul(self.bank(6, 16, n_ * 16), lhsT=lh, rhs=dta_c, start=True, stop=True), ["dta", "cst"], [("ps", 6)])
            S(lambda e: e.activation(out=e48, in_=self.bank(6, 48), func=AF.Exp), [("ps", 6)], ["e48"])
            V(lambda e: e.tensor_tensor(out=wend, in0=e48[:, 16:32], in1=dt_c, op=ALU.mult), ["e48", "dt"], ["wend"])
            V(lambda e: e.tensor_tensor(out=R, in0=dta_c.unsqueeze(2).to_broadcast([128, 16, 128]),
                                        in1=Bm.unsqueeze(1).to_broadcast([128, 16, 128]), op=ALU.mult), ["dta", "cst"], ["R"])
            for q in range(4):
                b = q % 2
                T(lambda e, q=q, b=b: e.matmul(self.bank(b), lhsT=Am, rhs=R[:, q * 4:(q + 1) * 4, :].rearrange("p a b -> p (a b)"), start=True, stop=True),
                  ["R", "cst"], [("ps", b)])
                S(lambda e, b=b: e.activation(out=Eq.rearrange("p a b -> p (a b)"), in_=self.bank(b), func=AF.Exp), [("ps", b)], ["Eq"])
                V(lambda e, q=q: e.tensor_tensor(out=Eq, in0=Eq, in1=cbm[:, q:q + 1, :].to_broadcast([128, 4, 128]), op=ALU.mult), ["Eq", "cbm"], ["Eq"])
                V(lambda e, q=q: e.tensor_tensor(out=attT[:, q * 4:(q + 1) * 4, :], in0=Eq,
                                                 in1=dt_c[:, q * 4:(q + 1) * 4].unsqueeze(2).to_broadcast([128, 4, 128]), op=ALU.mult),
                  ["Eq", "dt"], ["attT"])
            for h in range(16):
                T(lambda e, h=h: e.matmul(self.bank(2 + h // 8, 64, (h % 8) * 64), lhsT=attT[:, h, :], rhs=xs_tm[:, h * 64:(h + 1) * 64], start=True, stop=True),
                  ["attT", "xs_tm"], [("ps", 2 + h // 8)])
            for g in range(4):
                T(lambda e, g=g: e.matmul(self.bank(4 + g // 2, 256, (g % 2) * 256), lhsT=xbcT[:, 12 + g, cols], rhs=Sb[:, g * 256:(g + 1) * 256], start=True, stop=True),
                  ["xbcT", "Sb"], [("ps", 4 + g // 2)])
            V(lambda e: e.tensor_tensor(out=ych.rearrange("p (h q) -> p h q", q=64), in0=self.psum[:, 4:6, :].rearrange("p a (h q) -> p (a h) q", q=64),
                                        in1=e48[:, 0:16].unsqueeze(2).to_broadcast([128, 16, 64]), op=ALU.mult), [("ps", 4), ("ps", 5), "e48"], ["ych"])
            V(lambda e: e.tensor_tensor(out=ych.rearrange("p (a b) -> p a b", b=512), in0=ych.rearrange("p (a b) -> p a b", b=512), in1=self.psum[:, 2:4, :], op=ALU.add),
              ["ych", ("ps", 2), ("ps", 3)], ["ych"])
            ydst = self.yscr[tile * 128:(tile + 1) * 128, :]
            if d == 1:
                P.dma("sync", lambda e, ydst=ydst: e.dma_start(out=ypv, in_=ydst), reads=[("yscr", tile)], writes=["ypv"])
                V(lambda e: e.tensor_tensor(out=ych, in0=ych, in1=ypv, op=ALU.add), ["ych", "ypv"], ["ych"])
                P.dma("scalar", lambda e, tile=tile: e.dma_start(out=self.xsscr[tile * 128:(tile + 1) * 128, :], in_=xs_tm), reads=["xs_tm"],
                      writes=[("xsscr", tile)], sem="xs_st")
            P.dma("scalar", lambda e, ydst=ydst: e.dma_start(out=ydst, in_=ych), reads=["ych"], writes=[("yscr", tile)], sem="y_st")
            V(lambda e: e.tensor_tensor(out=xw.rearrange("p (h q) -> p h q", q=64), in0=xs_tm.rearrange("p (h q) -> p h q", q=64),
                                        in1=wend.unsqueeze(2).to_broadcast([128, 16, 64]), op=ALU.mult), ["xs_tm", "wend"], ["xw"])
            for g in range(4):
                T(lambda e, g=g: e.matmul(self.bank(4 + g // 2, 256, (g % 2) * 256), lhsT=bm_tm[:, g * 128:(g + 1) * 128], rhs=xw[:, g * 256:(g + 1) * 256], start=True, stop=True),
                  ["bm_tm", "xw"], [("ps", 4 + g // 2)])
            V(lambda e: e.tensor_tensor(out=St.rearrange("p (h q) -> p h q", q=64), in0=St.rearrange("p (h q) -> p h q", q=64),
                                        in1=e48[:, 32:48].unsqueeze(2).to_broadcast([128, 16, 64]), op=ALU.mult), ["St", "e48"], ["St"])
            V(lambda e: e.tensor_tensor(out=St.rearrange("p (a b) -> p a b", b=512), in0=St.rearrange("p (a b) -> p a b", b=512), in1=self.psum[:, 4:6, :], op=ALU.add),
              ["St", ("ps", 4), ("ps", 5)], ["St"])
            S(lambda e: e.copy(out=Sb, in_=St), ["St"], ["Sb"])
        for ch in chunks:
            chunk_body(ch)
    for u in units:
        unit_body(*u)
    P.barrier()
    A.off = mark


KB.ssd_inputs = ssd_inputs
KB.mix_cols = mix_cols
KB.ssd_scan = ssd_scan


def ssd_finish(self, src_fn, dst_fn):
    P, A, V, S, T = self.P, self.A, self.V, self.S, self.T
    mark = A.off
    l = 0
    wc = A.alloc([8, 4096], BF16)
    wo = A.alloc([16, D], BF16)
    stage = [A.alloc([2048]) for _ in range(2)]
    self.load_w_bf16(wc, self.ev_w_in, 0, 4096, "wc", stage)
    n = 0
    for k in range(16):
        s = n % 2; n += 1
        P.dma("sync" if s == 0 else "scalar", lambda e, k=k, s=s: e.dma_start(out=stage[s][:, 0:D], in_=self.ev_w_out[k * 128:(k + 1) * 128, :]),
              writes=[("stg", s)])
        P.op("gpsimd", lambda e, k=k, s=s: e.tensor_copy(out=wo[:, k, :], in_=stage[s][:, 0:D]), reads=[("stg", s)], writes=["wo"])
    A.off -= 2 * 2048
    P.barrier()
    cw = A.alloc([8, 3]); nT = A.alloc([8]); dsk = A.alloc([16]); nwr = A.alloc([D])
    for dst_, src_, k_ in ((cw, self.sc_cwT.ap().rearrange("p (a b) -> p a b", b=3), "cw"), (nT, self.nmixT[l], "nT"),
                           (dsk, self.dsk_row.ap(), "dsk"), (nwr, self.ssdnw_row.ap(), "nwr")):
        P.dma("sync", lambda e, dst_=dst_, src_=src_: e.dma_start(out=dst_, in_=src_), writes=[k_], sem="misc")
    Acol = [A.alloc([8]) for _ in range(2)]
    Bcol = [self.mix_cols(l, c, nT, Acol[c], ("Acol", c)) for c in range(2)]
    Grow = A.alloc([D])
    xt = A.alloc([D]); xnb = A.alloc([D], BF16); sm = A.alloc([16])
    hTu = A.alloc([8, 256], BF16)
    praw = A.alloc([16, 256]); cv = A.alloc([8, 256]); yaT = A.alloc([8, 256], BF16)
    zs = A.alloc([D]); ysum = A.alloc([D]); xsb = A.alloc([D], BF16); yb = A.alloc([D]); ybn = A.alloc([D], BF16)
    ybT = A.alloc([8, 128], BF16); xo = zs
    pbf = lambda b: self.psum[:, b, :].bitcast(BF16)
    units = [(0, 256, 1)] + [(i, 128, 0) for i in range(2, NT)]
    cur_c = [None]
    def unit_body(t0, W, c):
        nsub = W // 128
        if c != cur_c[0]:
            cur_c[0] = c
            self.make_row(Grow, self.modsFM[l][:, 16:24, c], "Grow", [("mods", l)], banks=(4, 5))
        for sub in range(nsub):
            self.norm_T(src_fn(t0 + sub), Acol[c], Bcol[c], [("Acol", c), ("mods", l)], hTu[:, :, sub * 128:(sub + 1) * 128], "hTu", (xt, xnb, sm))
        self.proj_fm(praw, "praw", wc, "wc", 0, 8, hTu, "hTu", W)
        self.proj_fm(praw[:, 8:16, :], "praw2", wc, "wc", 2048, 8, hTu, "hTu", W)
        pk1 = [("praw", j) for j in range(8)]; pk2 = [("praw2", j) for j in range(8)]
        V(lambda e: e.tensor_tensor(out=praw[:, 0:8, 0:W], in0=praw[:, 0:8, 0:W], in1=praw[:, 8:16, 0:W], op=ALU.mult), pk1 + pk2, ["prawm"])
        self.conv3(cv, "cv", praw, lambda j: ["prawm", ("praw", j)], cw, "cw", 8, W, 1 if c == 1 else W // 64)
        self.proj_fm(praw[:, 8:16, :], "praw2", wc, "wc", 1024, 8, hTu, "hTu", W)
        V(lambda e: e.tensor_tensor(out=yaT[:, :, 0:W], in0=cv[:, :, 0:W], in1=praw[:, 8:16, 0:W], op=ALU.mult), [("cv", j) for j in range(8)] + pk2, ["yaT"])
        def sub_body(sub):
            tile = t0 + sub
            cols = slice(sub * 128, (sub + 1) * 128)
            for n2 in range(2):
                for k in range(8):
                    T(lambda e, n2=n2, k=k: e.matmul(self.bank(n2), lhsT=hTu[:, k, cols], rhs=wc[:, k, 3072 + n2 * 512:3072 + (n2 + 1) * 512],
                                                     start=(k == 0), stop=(k == 7)), ["hTu", "wc"], [("ps", n2)])
            S(lambda e: e.activation(out=zs.rearrange("p (a b) -> p a b", b=512), in_=self.psum[:, 0:2, :], func=AF.Silu), [("ps", 0), ("ps", 1)], ["zs"])
            P.dma("sync", lambda e, tile=tile: e.dma_start(out=ysum, in_=self.yscr[tile * 128:(tile + 1) * 128, :]), reads=[("yscr", tile)], writes=["ysum"])
            P.dma("sync", lambda e, tile=tile: e.dma_start(out=xsb, in_=self.xsscr[tile * 128:(tile + 1) * 128, :]), reads=[("xsscr", tile)], writes=["xsb"])
            V(lambda e: e.tensor_tensor(out=yb.rearrange("p (h q) -> p h q", q=64), in0=xsb.rearrange("p (h q) -> p h q", q=64),
                                        in1=dsk.unsqueeze(2).to_broadcast([128, 16, 64]), op=ALU.mult), ["xsb", "dsk"], ["yb"])
            V(lambda e: e.tensor_tensor(out=yb, in0=yb, in1=ysum, op=ALU.add), ["yb", "ysum"], ["yb"])
            V(lambda e: e.tensor_tensor(out=yb, in0=yb, in1=zs, op=ALU.mult), ["yb", "zs"], ["yb"])
            for g in range(4):
                S(lambda e, g=g: e.activation(out=ysum[:, g * 256:(g + 1) * 256], in_=yb[:, g * 256:(g + 1) * 256], func=AF.Square, accum_out=sm[:, 4 + g:5 + g]),
                  ["yb"], ["ysum", "sm4"])
            S(lambda e: e.activation(out=sm[:, 8:12], in_=sm[:, 4:8], func=AF.Sqrt, scale=1.0 / 256, bias=EPS), ["sm4"], ["sm8"])
            V(lambda e: e.reciprocal(out=sm[:, 8:12], in_=sm[:, 8:12]), ["sm8"], ["sm8"])
            V(lambda e: e.tensor_tensor(out=yb.rearrange("p (g q) -> p g q", q=256), in0=yb.rearrange("p (g q) -> p g q", q=256),
                                        in1=sm[:, 8:12].unsqueeze(2).to_broadcast([128, 4, 256]), op=ALU.mult), ["yb", "sm8"], ["yb"])
            V(lambda e: e.tensor_tensor(out=ybn, in0=yb, in1=nwr, op=ALU.mult), ["yb", "nwr"], ["ybn"])
            for k in range(8):
                T(lambda e, k=k: e.transpose(pbf(7)[:, k * 128:(k + 1) * 128], ybn[:, k * 128:(k + 1) * 128], self.identb), ["ybn", "identb"], [("ps", 7)])
            S(lambda e: e.copy(out=ybT.rearrange("p a b -> p (a b)"), in_=pbf(7)), [("ps", 7)], ["ybT"])
            for n2 in range(2):
                for kc in range(16):
                    lh = yaT[:, kc, cols] if kc < 8 else ybT[:, kc - 8, :]
                    T(lambda e, n2=n2, kc=kc, lh=lh: e.matmul(self.bank(2 + n2), lhsT=lh, rhs=wo[:, kc, n2 * 512:(n2 + 1) * 512],
                                                              start=(kc == 0), stop=(kc == 15)), ["yaT", "ybT", "wo"], [("ps", 2 + n2)])
            V(lambda e: e.tensor_tensor(out=xo.rearrange("p (a b) -> p a b", b=512), in0=self.psum[:, 2:4, :], in1=Grow.rearrange("p (a b) -> p a b", b=512), op=ALU.mult),
              [("ps", 2), ("ps", 3), "Grow"], ["zs"])
            P.dma("sync", lambda e, tile=tile: e.dma_start(out=ysum, in_=src_fn(tile)), writes=["ysum"])
            V(lambda e: e.tensor_tensor(out=xo, in0=xo, in1=ysum, op=ALU.add), ["zs", "ysum"], ["zs"])
            P.dma("scalar", lambda e, tile=tile: e.dma_start(out=dst_fn(tile), in_=xo), reads=["zs"], writes=[self.key("x1")], sem="xo_st")
        for sub in range(nsub):
            sub_body(sub)
    for u in units:
        unit_body(*u)
    P.barrier()
    A.off = mark


def mixer0(self, src_fn, dst_fn):
    self._mix_helpers()
    self.ssd_scan(0, src_fn)
    self.ssd_scan(1, src_fn)
    self.ssd_finish(src_fn, dst_fn)


KB.ssd_finish = ssd_finish
KB.mixer0 = mixer0


def hg_inputs(self):
    nc = self.nc
    di = lambda n, s, dt=F32: nc.dram_tensor(n, list(s), dt, kind="ExternalInput")
    self.od_w_in = di("od_w_in", [D, 5120]); self.od_w_out = di("od_w_out", [D, D])
    self.lblT = di("lblT", [128, 32])
    self.hgnw_row = di("hgnw_row", [128, D])
    self.scanmask = di("scanmask", [128, 1024])


def hg_pass(self, d, src_fn, dst_fn):
    P, A, V, S, T = self.P, self.A, self.V, self.S, self.T
    mark = A.off
    l = 1
    fin = (d == 1)
    ncol = 5 if fin else 3
    w = A.alloc([8, ncol * 1024], BF16)
    stage = [A.alloc([1024]) for _ in range(2)]
    colsrc = [0, 1024 + d * 1024, 3072, 4096]
    for n_, c0 in enumerate(colsrc[:4 if fin else 3]):
        self.load_w_bf16(w[:, :, n_ * 1024:(n_ + 1) * 1024], self.od_w_in, c0, 1024, "w", stage)
    if fin:
        n = 0
        for k in range(8):
            s = n % 2; n += 1
            P.dma("sync" if s == 0 else "scalar", lambda e, k=k, s=s: e.dma_start(out=stage[s], in_=self.od_w_out[k * 128:(k + 1) * 128, :]), writes=[("stg", s)])
            P.op("gpsimd", lambda e, k=k, s=s: e.tensor_copy(out=w[:, k, 4096:5120], in_=stage[s]), reads=[("stg", s)], writes=["w"])
    P.barrier()
    A.off -= 2 * 1024
    nT = A.alloc([8]); lbl = A.alloc([32]); lb = A.alloc([8]); oml = A.alloc([8]); smask = A.alloc([D])
    nwr = A.alloc([D])
    for dst_, src_, k_ in ((nT, self.nmixT[l], "nT"), (lbl, self.lblT.ap(), "lbl"), (smask, self.scanmask.ap(), "smask"), (nwr, self.hgnw_row.ap(), "nwr")):
        P.dma("sync", lambda e, dst_=dst_, src_=src_: e.dma_start(out=dst_, in_=src_), writes=[k_], sem="misc")
    V(lambda e: e.tensor_tensor(out=lb, in0=lbl[:, 16 + d * 8:24 + d * 8], in1=lbl[:, d * 8:d * 8 + 8], op=ALU.subtract), ["lbl"], ["lb"])
    S(lambda e: e.activation(out=lb, in_=lb, func=AF.Sigmoid), ["lb"], ["lb"])
    V(lambda e: e.tensor_scalar(out=oml, in0=lb, scalar1=-1.0, scalar2=1.0, op0=ALU.mult, op1=ALU.add), ["lb"], ["oml"])
    Acol = [A.alloc([8]) for _ in range(2)]
    Bcol = [self.mix_cols(l, c, nT, Acol[c], ("Acol", c)) for c in range(2)]
    xt = A.alloc([D]); xnb = A.alloc([D], BF16); sm = A.alloc([32])
    hT = A.alloc([8, 128], BF16)
    praw = A.alloc([8, 128]); qF = A.alloc([8, 128]); fF = A.alloc([8, 128]); gl = A.alloc([8, 128]); GC = A.alloc([8, 128])
    ex = A.alloc([8, 128])
    qg = A.alloc([8, 128], BF16); kg = A.alloc([8, 128], BF16); kend = A.alloc([8, 128], BF16); iF = A.alloc([8, 128], BF16)
    eGE = A.alloc([8, 2]); GE = A.alloc([8, 2])
    v_tm = A.alloc([D], BF16); kend_tm = A.alloc([D], BF16); attm = A.alloc([8, 64], BF16)
    och = A.alloc([D]); St = A.alloc([8, 128]); Sb = A.alloc([8, 128], BF16)
    V(lambda e: e.memset(St, 0.0), [], ["St"])
    V(lambda e: e.memset(Sb, 0.0), [], ["Sb"])
    if fin:
        opv = A.alloc([D]); gs = A.alloc([D]); onb = A.alloc([D], BF16); onT = A.alloc([8, 64], BF16)
        Grow = A.alloc([D])
        self.make_row(Grow, self.modsFM[l][:, 16:24, 0], "Grow", [("mods", l)], banks=(4, 5))
    cst = self.cst
    Mk = cst[0:64, 256:320] if d == 0 else cst[0:64, 512:576]
    pbf = lambda b: self.psum[:, b, :].bitcast(BF16)
    tiles = [0, 1] + list(range(2, NT))
    if d == 1:
        tiles = [1, 0] + list(range(NT - 1, 1, -1))
    flat = lambda a: a.rearrange("p a b -> p (a b)")
    bc8 = lambda col: col.unsqueeze(2).to_broadcast([128, 8, 128])

    def tile_body(i):
        c = 1 if i < 2 else 0
        need_out = (i >= 2)
        self.norm_T(src_fn(i), Acol[c], Bcol[c], [("Acol", c), ("mods", l)], hT, "hT", (xt, xnb, sm))
        if need_out:
            self.proj_fm(qF, "qFj", w, "w", 0, 8, hT, "hT", 128, func=AF.Silu)
        self.proj_fm(fF, "fFj", w, "w", 1024, 8, hT, "hT", 128, func=AF.Sigmoid)
        V(lambda e: e.tensor_tensor(out=fF, in0=fF, in1=bc8(oml), op=ALU.mult), [("fFj", j) for j in range(8)] + ["fF", "oml"], ["fF"] + [("fFj", j) for j in range(8)])
        V(lambda e: e.tensor_tensor(out=fF, in0=fF, in1=bc8(lb), op=ALU.add), ["fF", "lb"], ["fF"])
        S(lambda e: e.activation(out=gl, in_=fF, func=AF.Ln), ["fF"], ["gl"])
        V(lambda e: e.tensor_scalar(out=fF, in0=fF, scalar1=-1.0, scalar2=1.0, op0=ALU.mult, op1=ALU.add), ["fF"], ["fF"])
        V(lambda e: e.tensor_tensor_scan(out=flat(GC), data0=smask, data1=flat(gl), initial=0.0, op0=ALU.mult, op1=ALU.add), ["smask", "gl"], ["GC"])
        GC4 = GC.rearrange("p j (c t) -> p j c t", t=64)
        V(lambda e: e.tensor_copy(out=GE, in_=GC4[:, :, :, 63]), ["GC"], ["GE"])
        if d == 1:
            V(lambda e: e.tensor_tensor(out=GC, in0=gl, in1=GC, op=ALU.subtract), ["gl", "GC"], ["GC"])
            V(lambda e: e.tensor_tensor(out=GC4, in0=GC4, in1=GE.unsqueeze(3).to_broadcast([128, 8, 2, 64]), op=ALU.add), ["GC", "GE"], ["GC"])
        S(lambda e: e.activation(out=eGE, in_=GE, func=AF.Exp), ["GE"], ["eGE"])
        if need_out:
            S(lambda e: e.activation(out=ex, in_=GC, func=AF.Exp), ["GC"], ["ex"])
            V(lambda e: e.scalar_tensor_tensor(out=flat(qg), in0=flat(qF), scalar=float(128 ** -0.5), in1=flat(ex), op0=ALU.mult, op1=ALU.mult), [("qFj", j) for j in range(8)] + ["ex"], ["qg"])
            S(lambda e: e.activation(out=ex, in_=GC, func=AF.Exp, scale=-1.0), ["GC"], ["ex"])
            V(lambda e: e.tensor_tensor(out=kg, in0=fF, in1=ex, op=ALU.mult), ["fF", "ex"], ["kg"])
        V(lambda e: e.tensor_tensor(out=GC4, in0=GE.unsqueeze(3).to_broadcast([128, 8, 2, 64]), in1=GC4, op=ALU.subtract), ["GC", "GE"], ["GC"])
        S(lambda e: e.activation(out=ex, in_=GC, func=AF.Exp), ["GC"], ["ex"])
        V(lambda e: e.tensor_tensor(out=kend, in0=fF, in1=ex, op=ALU.mult), ["fF", "ex"], ["kend"])
        self.proj_fm(iF, "iF", w, "w", 2048, 8, hT, "hT", 128)
        subs = [0, 1] if d == 0 else [1, 0]

        def sub_body(sc):
            cols = slice(sc * 64, sc * 64 + 64)
            rows = slice(i * 128 + sc * 64, i * 128 + sc * 64 + 64)
            for j in range(8):
                T(lambda e, j=j: e.transpose(pbf(7)[0:64, j * 128:(j + 1) * 128], iF[:, j, cols], self.identb), [("iF", j), "identb"], [("ps", 7)])
            S(lambda e: e.copy(out=v_tm[0:64, :], in_=pbf(7)[0:64, :]), [("ps", 7)], ["v_tm"])
            for j in range(8):
                T(lambda e, j=j: e.transpose(pbf(6)[0:64, j * 128:(j + 1) * 128], kend[:, j, cols], self.identb), ["kend", "identb"], [("ps", 6)])
            S(lambda e: e.copy(out=kend_tm[0:64, :], in_=pbf(6)[0:64, :]), [("ps", 6)], ["kend_tm"])
            if need_out:
                for j in range(8):
                    T(lambda e, j=j: e.matmul(self.psum[0:64, 0, j * 64:(j + 1) * 64], lhsT=kg[:, j, cols], rhs=qg[:, j, cols], start=True, stop=True),
                      ["kg", "qg"], [("ps", 0)])
                V(lambda e: e.tensor_tensor(out=attm[0:64], in0=self.psum[0:64, 0, :].rearrange("p (j t) -> p j t", t=64),
                                            in1=Mk.unsqueeze(1).to_broadcast([64, 8, 64]), op=ALU.mult), [("ps", 0), "cst"], ["attm"])
                for j in range(8):
                    ob = self.psum[0:64, 2 + j // 4, (j % 4) * 128:(j % 4 + 1) * 128]
                    T(lambda e, j=j, ob=ob: e.matmul(ob, lhsT=attm[0:64, j, :], rhs=v_tm[0:64, j * 128:(j + 1) * 128], start=True, stop=False),
                      ["attm", "v_tm"], [("ps", 2 + j // 4)])
                    T(lambda e, j=j, ob=ob: e.matmul(ob, lhsT=qg[:, j, cols], rhs=Sb[:, j, :], start=False, stop=True),
                      ["qg", "Sb"], [("ps", 2 + j // 4)])
                ydst = self.yscr[rows, :]
                if not fin:
                    S(lambda e: e.copy(out=och[0:64].rearrange("p (a b) -> p a b", b=512), in_=self.psum[0:64, 2:4, :]), [("ps", 2), ("ps", 3)], ["och"])
                    P.dma("scalar", lambda e: e.dma_start(out=ydst, in_=och[0:64]), reads=["och"], writes=[("yscr", i, sc)], sem="y_st")
                else:
                    P.dma("sync", lambda e: e.dma_start(out=opv[0:64], in_=ydst), reads=[("yscr", i, sc)], writes=["opv"])
                    V(lambda e: e.tensor_tensor(out=och[0:64].rearrange("p (a b) -> p a b", b=512), in0=self.psum[0:64, 2:4, :],
                                                in1=opv[0:64].rearrange("p (a b) -> p a b", b=512), op=ALU.add), [("ps", 2), ("ps", 3), "opv"], ["och"])
            for j in range(8):
                T(lambda e, j=j: e.matmul(self.psum[:, 4 + j // 4, (j % 4) * 128:(j % 4 + 1) * 128], lhsT=kend_tm[0:64, j * 128:(j + 1) * 128],
                                          rhs=v_tm[0:64, j * 128:(j + 1) * 128], start=True, stop=True), ["kend_tm", "v_tm"], [("ps", 4 + j // 4)])
            V(lambda e: e.tensor_tensor(out=St, in0=St, in1=eGE[:, :, sc:sc + 1].to_broadcast([128, 8, 128]), op=ALU.mult), ["St", "eGE"], ["St"])
            V(lambda e: e.tensor_tensor(out=flat(St).rearrange("p (a b) -> p a b", b=512), in0=flat(St).rearrange("p (a b) -> p a b", b=512),
                                        in1=self.psum[:, 4:6, :], op=ALU.add), ["St", ("ps", 4), ("ps", 5)], ["St"])
            S(lambda e: e.copy(out=Sb, in_=St), ["St"], ["Sb"])
            if need_out and fin:
                for j in range(8):
                    S(lambda e, j=j: e.activation(out=opv[0:64, j * 128:(j + 1) * 128], in_=och[0:64, j * 128:(j + 1) * 128], func=AF.Square,
                                                  accum_out=sm[0:64, 8 + j:9 + j]), ["och"], ["opv", "sm8"])
                S(lambda e: e.activation(out=sm[0:64, 16:24], in_=sm[0:64, 8:16], func=AF.Sqrt, scale=1.0 / 128, bias=EPS), ["sm8"], ["sm16"])
                V(lambda e: e.reciprocal(out=sm[0:64, 16:24], in_=sm[0:64, 16:24]), ["sm16"], ["sm16"])
                V(lambda e: e.tensor_tensor(out=och[0:64].rearrange("p (j q) -> p j q", q=128), in0=och[0:64].rearrange("p (j q) -> p j q", q=128),
                                            in1=sm[0:64, 16:24].unsqueeze(2).to_broadcast([64, 8, 128]), op=ALU.mult), ["och", "sm16"], ["och"])
                V(lambda e: e.tensor_tensor(out=och[0:64], in0=och[0:64], in1=nwr[0:64], op=ALU.mult), ["och", "nwr"], ["och"])
                for n2 in range(2):
                    for k in range(8):
                        T(lambda e, n2=n2, k=k: e.matmul(self.psum[0:64, n2, :], lhsT=hT[:, k, cols], rhs=w[:, k, 3072 + n2 * 512:3072 + (n2 + 1) * 512],
                                                         start=(k == 0), stop=(k == 7)), ["hT", "w"], [("ps", n2)])
                S(lambda e: e.activation(out=gs[0:64].rearrange("p (a b) -> p a b", b=512), in_=self.psum[0:64, 0:2, :], func=AF.Silu), [("ps", 0), ("ps", 1)], ["gs"])
                V(lambda e: e.tensor_tensor(out=onb[0:64], in0=och[0:64], in1=gs[0:64], op=ALU.mult), ["och", "gs"], ["onb"])
                for k in range(8):
                    T(lambda e, k=k: e.transpose(pbf(7)[:, k * 64:(k + 1) * 64], onb[0:64, k * 128:(k + 1) * 128], self.identb[0:64, 0:64]),
                      ["onb", "identb"], [("ps", 7)])
                S(lambda e: e.copy(out=flat(onT), in_=pbf(7)[:, 0:512]), [("ps", 7)], ["onT"])
                for n2 in range(2):
                    for k in range(8):
                        T(lambda e, n2=n2, k=k: e.matmul(self.psum[0:64, 2 + n2, :], lhsT=onT[:, k, :], rhs=w[:, k, 4096 + n2 * 512:4096 + (n2 + 1) * 512],
                                                         start=(k == 0), stop=(k == 7)), ["onT", "w"], [("ps", 2 + n2)])
                V(lambda e: e.tensor_tensor(out=gs[0:64].rearrange("p (a b) -> p a b", b=512), in0=self.psum[0:64, 2:4, :],
                                            in1=Grow[0:64].rearrange("p (a b) -> p a b", b=512), op=ALU.mult), [("ps", 2), ("ps", 3), "Grow"], ["gs"])
                P.dma("sync", lambda e: e.dma_start(out=opv[0:64], in_=src_fn(i)[sc * 64:sc * 64 + 64, :]), writes=["opv"])
                V(lambda e: e.tensor_tensor(out=gs[0:64], in0=gs[0:64], in1=opv[0:64], op=ALU.add), ["gs", "opv"], ["gs"])
                P.dma("scalar", lambda e: e.dma_start(out=dst_fn(i)[sc * 64:sc * 64 + 64, :], in_=gs[0:64]), reads=["gs"], writes=[self.key("x3")], sem="xo_st")
        for sc in subs:
            sub_body(sc)
    for i in tiles:
        tile_body(i)
    P.barrier()
    A.off = mark


def mixer1(self, src_fn, dst_fn):
    self._mix_helpers()
    self.hg_pass(0, src_fn, dst_fn)
    self.hg_pass(1, src_fn, dst_fn)


KB.hg_inputs = hg_inputs
KB.hg_pass = hg_pass
KB.mixer1 = mixer1
```
